# Optimizing a Trainium2 kernel written in Bass

```python
import math
import jax, jax.numpy as jnp
from jax import lax
import numpy as np

D_MODEL = 1024
BATCH = 4
SEQ = 4096
DEPTH = 1

GLA_HEADS = 4
GLA_DK = D_MODEL // 2 // GLA_HEADS
GLA_DV = D_MODEL // GLA_HEADS
GLA_LOWRANK = 16
GLA_TAU = 16.0
GLA_CHUNK = 64
FNET_GROUPS = 4
FNET_GW = D_MODEL // 8
MEM_LEN = 256
MEM_HEADS = 4
MEM_HD = D_MODEL // 8
N_BRANCH = 3
N_EXPERTS = 32
TOP_K = 4
D_FF = D_MODEL
SWIGLU_LIMIT = 7.0
SWIGLU_ALPHA = 1.702
MOE_BLOCK = 256
LN_EPS = 1e-5
RMS_EPS = 1e-6
DN_ALPHA = (2.0 * DEPTH) ** 0.25
DN_BETA = (8.0 * DEPTH) ** -0.25

QK_W = GLA_HEADS * GLA_DK
V_W = GLA_HEADS * GLA_DV
R_W = V_W
FN_W = FNET_GROUPS * FNET_GW
MQ_W = MEM_HEADS * MEM_HD
GATE_W = N_BRANCH * D_MODEL
IN_SIZES = (QK_W, QK_W, V_W, R_W, GLA_LOWRANK, GLA_LOWRANK, FN_W, MQ_W, GATE_W)
IN_WIDTH = sum(IN_SIZES)

kernel_name = "hybrid_gla_fnet_memxattn_moe_encoder"


def layer_norm(x, g, b):
    xf = x.astype(jnp.float32)
    mu = jnp.mean(xf, axis=-1, keepdims=True)
    var = jnp.mean(jnp.square(xf - mu), axis=-1, keepdims=True)
    return ((xf - mu) * lax.rsqrt(var + LN_EPS) * g + b).astype(x.dtype)


def gla_chunked(q, k, v, log_a):
    B, S, H, DK = q.shape
    DV = v.shape[-1]
    C = GLA_CHUNK
    N = S // C

    def chunk(t):
        return t.astype(jnp.float32).reshape(B, N, C, H, t.shape[-1]).transpose(1, 0, 3, 2, 4)

    qc, kc, vc, ac = chunk(q), chunk(k), chunk(v), chunk(log_a)
    bcum = jnp.cumsum(ac, axis=-2)
    blast = bcum[..., -1:, :]
    q_in = qc * jnp.exp(bcum)
    k_in = kc * jnp.exp(-bcum)
    k_st = kc * jnp.exp(blast - bcum)
    mask = jnp.tril(jnp.ones((C, C), dtype=bool))
    att = jnp.where(mask, jnp.einsum('nbhik,nbhjk->nbhij', q_in, k_in), 0.0)
    o_intra = jnp.einsum('nbhij,nbhjv->nbhiv', att, vc)

    def step(state, inp):
        q_i, k_s, v_i, dec = inp
        o = jnp.einsum('bhik,bhkv->bhiv', q_i, state)
        state = state * jnp.exp(dec[..., 0, :])[..., None] + jnp.einsum('bhjk,bhjv->bhkv', k_s, v_i)
        return state, o

    state0 = jnp.zeros((B, H, DK, DV), jnp.float32)
    _, o_inter = lax.scan(step, state0, (q_in, k_st, vc, blast))
    o = o_intra + o_inter
    return o.transpose(1, 0, 3, 2, 4).reshape(B, S, H, DV)


def mixer_branches(u, mem_n, w_in, b_in, w_decay_f, b_decay_f, w_decay_b, b_decay_b,
                   gla_norm_g, w_br_gla, w_br_fnet, w_br_mem, w_mem_kv, w_out, b_out):
    B, S, D = u.shape
    dt = u.dtype
    proj = u @ w_in + b_in
    splits = np.cumsum(IN_SIZES)[:-1].tolist()
    q, k, v, r, lr_f, lr_b, fn, mq, gates = jnp.split(proj, splits, axis=-1)

    def heads(t, d):
        return t.reshape(B, S, GLA_HEADS, d)
    qh = heads(q, GLA_DK) * (GLA_DK ** -0.5)
    kh = heads(k, GLA_DK)
    vh = heads(v, GLA_DV)
    la_f = heads(jax.nn.log_sigmoid((lr_f @ w_decay_f + b_decay_f).astype(jnp.float32)) / GLA_TAU, GLA_DK)
    la_b = heads(jax.nn.log_sigmoid((lr_b @ w_decay_b + b_decay_b).astype(jnp.float32)) / GLA_TAU, GLA_DK)
    flip = lambda t: jnp.flip(t, axis=1)
    o_f = gla_chunked(qh, kh, vh, la_f)
    o_b = flip(gla_chunked(flip(qh), flip(kh), flip(vh), flip(la_b)))
    o = o_f + o_b
    o = o * lax.rsqrt(jnp.mean(jnp.square(o), axis=-1, keepdims=True) + RMS_EPS) * gla_norm_g
    o = (o * jax.nn.silu(heads(r, GLA_DV).astype(jnp.float32))).astype(dt).reshape(B, S, V_W)
    y_gla = o @ w_br_gla

    f = fn.reshape(B, S, FNET_GROUPS, FNET_GW).astype(jnp.float32)
    f = jnp.fft.fft2(f, axes=(1, 3), norm='ortho').real
    y_fnet = f.astype(dt).reshape(B, S, FN_W) @ w_br_fnet

    kv = mem_n @ w_mem_kv
    mk, mv = jnp.split(kv, 2, axis=-1)
    M = mem_n.shape[1]
    qm = mq.reshape(B, S, MEM_HEADS, MEM_HD)
    mk = mk.reshape(B, M, MEM_HEADS, MEM_HD)
    mv = mv.reshape(B, M, MEM_HEADS, MEM_HD)
    s = jnp.einsum('bshd,bmhd->bhsm', qm, mk).astype(jnp.float32) * (MEM_HD ** -0.5)
    p = jax.nn.softmax(s, axis=-1).astype(dt)
    om = jnp.einsum('bhsm,bmhd->bshd', p, mv).reshape(B, S, MQ_W)
    y_mem = om @ w_br_mem

    g = jax.nn.sigmoid(gates.astype(jnp.float32)).reshape(B, S, N_BRANCH, D)
    merged = (g[:, :, 0] * y_gla.astype(jnp.float32)
              + g[:, :, 1] * y_fnet.astype(jnp.float32)
              + g[:, :, 2] * y_mem.astype(jnp.float32)).astype(dt)
    return merged @ w_out + b_out


def moe_ffn(h, w_router, b_router, w_gu, b_gu, w_down, b_down):
    B, S, D = h.shape
    T = B * S
    A = T * TOP_K
    hf = h.reshape(T, D)
    logits = (hf @ w_router + b_router).astype(jnp.float32)
    top_val, top_idx = lax.top_k(logits, TOP_K)
    top_w = jax.nn.softmax(top_val, axis=-1)
    flat_e = top_idx.reshape(A).astype(jnp.int32)
    flat_tok = jnp.repeat(jnp.arange(T, dtype=jnp.int32), TOP_K)
    flat_w = top_w.reshape(A)
    order = jnp.argsort(flat_e)
    sorted_e = flat_e[order]
    counts = jnp.bincount(flat_e, length=N_EXPERTS)
    start = jnp.cumsum(counts) - counts
    padded = (counts + MOE_BLOCK - 1) // MOE_BLOCK * MOE_BLOCK
    pad_end = jnp.cumsum(padded)
    pad_start = pad_end - padded
    dest = pad_start[sorted_e] + (jnp.arange(A, dtype=jnp.int32) - start[sorted_e])
    n_blocks = (A + N_EXPERTS * (MOE_BLOCK - 1) + MOE_BLOCK - 1) // MOE_BLOCK
    P = n_blocks * MOE_BLOCK
    buf_tok = jnp.zeros((P,), jnp.int32).at[dest].set(flat_tok[order])
    buf_w = jnp.zeros((P,), jnp.float32).at[dest].set(flat_w[order])
    block_e = jnp.minimum(
        jnp.searchsorted(pad_end, jnp.arange(n_blocks, dtype=pad_end.dtype) * MOE_BLOCK, side='right'),
        N_EXPERTS - 1)

    def block_fn(inp):
        tok, wgt, e = inp
        xb = hf[tok]
        gu = xb @ w_gu[e] + b_gu[e]
        gate, up = jnp.split(gu, 2, axis=-1)
        gate = jnp.minimum(gate, SWIGLU_LIMIT)
        up = jnp.clip(up, -SWIGLU_LIMIT, SWIGLU_LIMIT)
        act = (up + 1.0) * (gate * jax.nn.sigmoid(SWIGLU_ALPHA * gate))
        out = act @ w_down[e] + b_down[e]
        return out * wgt[:, None].astype(out.dtype)

    outs = lax.map(block_fn, (buf_tok.reshape(n_blocks, MOE_BLOCK),
                              buf_w.reshape(n_blocks, MOE_BLOCK), block_e))
    y = jnp.zeros((T, D), h.dtype).at[buf_tok].add(outs.reshape(P, D).astype(h.dtype))
    return y.reshape(B, S, D)


def setup_inputs(seed: int = 0) -> dict:
    key = jax.random.key(seed)
    keys = jax.random.split(key, 40)
    ctr = [0]

    def nrm(shape, scale):
        k_ = keys[ctr[0]]
        ctr[0] += 1
        return jax.random.normal(k_, shape, jnp.float32) * scale

    L, D = DEPTH, D_MODEL
    x = nrm((BATCH, SEQ, D), 1.0)
    mem = nrm((BATCH, MEM_LEN, D), 1.0)
    ln_in_g = 1.0 + nrm((D,), 0.02)
    ln_in_b = nrm((D,), 0.01)
    ln_mem_g = 1.0 + nrm((D,), 0.02)
    ln_mem_b = nrm((D,), 0.01)
    col_scale = jnp.concatenate([jnp.ones((2 * QK_W,), jnp.float32),
                                 jnp.full((V_W,), DN_BETA, jnp.float32),
                                 jnp.ones((IN_WIDTH - 2 * QK_W - V_W,), jnp.float32)])
    w_in = nrm((L, D, IN_WIDTH), D ** -0.5) * col_scale
    b_in = nrm((L, IN_WIDTH), 0.01)
    w_decay_f = nrm((L, GLA_LOWRANK, QK_W), GLA_LOWRANK ** -0.5)
    b_decay_f = nrm((L, QK_W), 0.1)
    w_decay_b = nrm((L, GLA_LOWRANK, QK_W), GLA_LOWRANK ** -0.5)
    b_decay_b = nrm((L, QK_W), 0.1)
    gla_norm_g = 1.0 + nrm((L, GLA_DV), 0.02)
    w_br_gla = nrm((L, V_W, D), V_W ** -0.5)
    w_br_fnet = nrm((L, FN_W, D), FN_W ** -0.5)
    w_br_mem = nrm((L, MQ_W, D), MQ_W ** -0.5)
    kv_scale = jnp.concatenate([jnp.ones((MQ_W,), jnp.float32), jnp.full((MQ_W,), DN_BETA, jnp.float32)])
    w_mem_kv = nrm((L, D, 2 * MQ_W), D ** -0.5) * kv_scale
    w_out = nrm((L, D, D), D ** -0.5) * DN_BETA
    b_out = nrm((L, D), 0.01)
    ln1_g = 1.0 + nrm((L, D), 0.02)
    ln1_b = nrm((L, D), 0.01)
    w_router = nrm((L, D, N_EXPERTS), D ** -0.5)
    b_router = nrm((L, N_EXPERTS), 0.01)
    w_gu = nrm((L, N_EXPERTS, D, 2 * D_FF), D ** -0.5)
    b_gu = nrm((L, N_EXPERTS, 2 * D_FF), 0.01)
    w_down = nrm((L, N_EXPERTS, D_FF, D), D_FF ** -0.5) * DN_BETA
    b_down = nrm((L, N_EXPERTS, D), 0.01)
    ln2_g = 1.0 + nrm((L, D), 0.02)
    ln2_b = nrm((L, D), 0.01)
    return {"x": x, "mem": mem, "ln_in_g": ln_in_g, "ln_in_b": ln_in_b,
            "ln_mem_g": ln_mem_g, "ln_mem_b": ln_mem_b, "w_in": w_in, "b_in": b_in,
            "w_decay_f": w_decay_f, "b_decay_f": b_decay_f, "w_decay_b": w_decay_b,
            "b_decay_b": b_decay_b, "gla_norm_g": gla_norm_g, "w_br_gla": w_br_gla,
            "w_br_fnet": w_br_fnet, "w_br_mem": w_br_mem, "w_mem_kv": w_mem_kv,
            "w_out": w_out, "b_out": b_out, "ln1_g": ln1_g, "ln1_b": ln1_b,
            "w_router": w_router, "b_router": b_router, "w_gu": w_gu, "b_gu": b_gu,
            "w_down": w_down, "b_down": b_down, "ln2_g": ln2_g, "ln2_b": ln2_b}


def reference(x, mem, ln_in_g, ln_in_b, ln_mem_g, ln_mem_b, w_in, b_in,
              w_decay_f, b_decay_f, w_decay_b, b_decay_b, gla_norm_g, w_br_gla,
              w_br_fnet, w_br_mem, w_mem_kv, w_out, b_out, ln1_g, ln1_b,
              w_router, b_router, w_gu, b_gu, w_down, b_down, ln2_g, ln2_b):
    h = layer_norm(x, ln_in_g, ln_in_b)
    mem_n = layer_norm(mem, ln_mem_g, ln_mem_b)
    for l in range(DEPTH):
        mix = mixer_branches(h, mem_n, w_in[l], b_in[l], w_decay_f[l], b_decay_f[l],
                             w_decay_b[l], b_decay_b[l], gla_norm_g[l], w_br_gla[l],
                             w_br_fnet[l], w_br_mem[l], w_mem_kv[l], w_out[l], b_out[l])
        h = layer_norm(DN_ALPHA * h + mix, ln1_g[l], ln1_b[l])
        ff = moe_ffn(h, w_router[l], b_router[l], w_gu[l], b_gu[l], w_down[l], b_down[l])
        h = layer_norm(DN_ALPHA * h + ff, ln2_g[l], ln2_b[l])
    return h
```

```python
import contextlib
import math
import numpy as np
import ml_dtypes
import concourse.bass as bass
import concourse.mybir as mybir
from concourse.bass_utils import run_bass_kernel_spmd

F32 = mybir.dt.float32
BF16 = mybir.dt.bfloat16
I32 = mybir.dt.int32
U32 = mybir.dt.uint32
AF = mybir.ActivationFunctionType
ALU = mybir.AluOpType
AX = mybir.AxisListType

D = 1024
S = 4096
NT = 2048
NTILE = 16
E = 32
CAP = 384
ALPHA = 2.0 ** 0.25
LN_EPS = 1e-5
RMS_EPS = 1e-6
OQ, OK_, OV, OR, OLF, OLB, OFN, OMQ, OG = 0, 512, 1024, 2048, 3072, 3088, 3104, 3616, 4128
C_LNG, C_LNB, C_MG, C_MB, C_BQ, C_BK, C_BR, C_BMQ, C_BG, C_GN, C_BLF, C_BLB, C_BGU = 0, 8, 16, 24, 32, 36, 40, 48, 52, 76, 78, 79, 80
NCOLS = 80 + 512
V_BK, V_BV, V_BFN, V_BOUT, V_LNG, V_LNB, V_L1G, V_L1B, V_L2G, V_L2B, V_BRT = 0, 512, 1536, 2048, 3072, 4096, 5120, 6144, 7168, 8192, 9216
NVEC = 9216 + 32
K_ID, K_TRIF, K_TRIB, K_TRIRF, K_TRIRB, K_MF, K_MB, K_ONES, K_STRI, K_ONE1, K_ECAP = 0, 128, 256, 384, 512, 640, 1152, 1664, 1792, 1920, 2048
NCST = 2048 + 32


class Reg:
    __slots__ = ("name", "w", "r", "pend")

    def __init__(self, name):
        self.name = name
        self.w = {}
        self.r = {}
        self.pend = None


class Eng:
    def __init__(self, name, sem):
        self.name = name
        self.sem = sem
        self.cnt = 0
        self.waited = {}
        self.ops = []
        self.pending = []
        self.dsems = []
        self.dval = {}
        self.di = 0


class KB:
    def __init__(self, nc, stack):
        self.nc = nc
        self.stack = stack
        self.E = {}
        for n in ("pe", "dve", "act", "pool", "sp"):
            self.E[n] = Eng(n, stack.enter_context(nc.semaphore("s_" + n)))
        for n, k in (("sp", 8), ("pool", 8), ("act", 4)):
            for i in range(k):
                s = stack.enter_context(nc.semaphore("d_%s%d" % (n, i)))
                self.E[n].dsems.append(s)
                self.E[n].dval[s] = 0
        self.nreg = 0
        self.dma_sems = set()
        for e_ in self.E.values():
            self.dma_sems.update(e_.dsems)
        self.eobj = {"pe": nc.tensor, "dve": nc.vector, "act": nc.scalar, "pool": nc.gpsimd, "sp": nc.sync}

    def reg(self, name=None):
        self.nreg += 1
        return Reg(name or ("r%d" % self.nreg))

    def _waits(self, E, r, w, skip_dma_w=False):
        waits = {}

        def need(sem, val):
            if E.waited.get(sem, 0) < val and waits.get(sem, 0) < val:
                waits[sem] = val

        for g in r:
            if g.pend is not None and g.pend != E.name:
                raise RuntimeError("region %s has pending updates from %s" % (g.name, g.pend))
            for sem, val in g.w.items():
                need(sem, val)
        for g in w:
            if g.pend is not None and g.pend != E.name:
                raise RuntimeError("region %s has pending updates from %s" % (g.name, g.pend))
            for sem, val in g.w.items():
                if skip_dma_w and sem in self.dma_sems:
                    continue
                need(sem, val)
            for sem, val in g.r.items():
                need(sem, val)
        if E.name == "pe":
            waits.pop(E.sem, None)
        for sem, val in waits.items():
            E.waited[sem] = val
        return list(waits.items())

    def op(self, en, fn, r=(), w=(), inc=True):
        E = self.E[en]
        wl = self._waits(E, r, w)
        if inc:
            E.cnt += 1
            tok = (E.sem, E.cnt)
            for rr, ww in E.pending + [(r, w)]:
                for g in ww:
                    g.w = {tok[0]: tok[1]}
                    g.r = {}
                    g.pend = None
                for g in rr:
                    if g not in ww:
                        g.r[E.sem] = E.cnt
                        g.pend = None
            E.pending = []
        else:
            E.pending.append((tuple(r), tuple(w)))
            for g in list(r) + list(w):
                g.pend = E.name
        self._emit(E, wl, fn, 1 if inc else 0, None)

    def _emit(self, E, wl, fn, inc, ds):
        eng = self.eobj[E.name]
        for sem, val in wl:
            eng.wait_ge(sem, val)
        if fn is None:
            return
        ins = fn(eng)
        if ds is not None:
            ins.then_inc(ds, 16)
        elif inc:
            ins.then_inc(E.sem, 1)

    def dma(self, en, fn, r=(), w=()):
        E = self.E[en]
        wl = self._waits(E, r, w, skip_dma_w=True)
        ds = E.dsems[E.di % len(E.dsems)]
        E.di += 1
        prev = E.dval[ds]
        if prev > 0 and E.waited.get(ds, 0) < prev:
            E.waited[ds] = prev
            wl.append((ds, prev))
        E.dval[ds] = prev + 16
        tok = (ds, prev + 16)
        for g in w:
            keep = {sm: v for sm, v in g.w.items() if sm in self.dma_sems}
            keep[ds] = prev + 16
            g.w = keep
            g.r = {}
        for g in r:
            if g not in w:
                g.r[ds] = prev + 16
        self._emit(E, wl, fn, 16, ds)

    def barrier(self):
        toks = []
        for E in self.E.values():
            if E.cnt > 0:
                toks.append((E.sem, E.cnt))
            for ds, v in E.dval.items():
                if v > 0:
                    toks.append((ds, v))
        for E in self.E.values():
            if E.pending:
                raise RuntimeError("pending at barrier on " + E.name)
            wl = []
            for sem, val in toks:
                if sem is E.sem and E.name == "pe":
                    continue
                if E.waited.get(sem, 0) < val:
                    E.waited[sem] = val
                    wl.append((sem, val))
            if wl:
                self._emit(E, wl, None, 0, None)

    def replay(self, en, eng):
        E = self.E[en]
        for wl, fn, inc, ds in E.ops:
            for sem, val in wl:
                eng.wait_ge(sem, val)
            if fn is None:
                continue
            ins = fn(eng)
            if ds is not None:
                ins.then_inc(ds, 16)
            elif inc:
                ins.then_inc(E.sem, 1)

    def mm(self, out, lhsT, rhs, start, stop, r=(), w=(), inc=None):
        if inc is None:
            inc = stop
        self.op("pe", lambda e: e.matmul(out, lhsT, rhs, start=start, stop=stop), r, w, inc)

    def tr(self, out, in_, ident, r=(), w=(), inc=True):
        self.op("pe", lambda e: e.transpose(out, in_, ident), r, w, inc)

    def act(self, out, in_, func, r=(), w=(), bias=0.0, scale=1.0, accum_out=None, en="act"):
        if accum_out is None:
            self.op(en, lambda e: e.activation(out, in_, func, bias=bias, scale=scale), r, w)
        else:
            self.op(en, lambda e: e.activation(out, in_, func, bias=bias, scale=scale, accum_out=accum_out), r, w)

    def tt(self, en, out, in0, in1, op, r=(), w=()):
        self.op(en, lambda e: e.tensor_tensor(out, in0, in1, op), r, w)

    def ts(self, en, out, in0, s1, s2, op0, op1=None, r=(), w=(), accum_out=None):
        if op1 is None:
            self.op(en, lambda e: e.tensor_scalar(out, in0, s1, None, op0), r, w)
        elif accum_out is not None:
            self.op(en, lambda e: e.tensor_scalar(out, in0, s1, s2, op0, op1, accum_out), r, w)
        else:
            self.op(en, lambda e: e.tensor_scalar(out, in0, s1, s2, op0, op1), r, w)

    def stt(self, en, out, in0, scalar, in1, op0, op1, r=(), w=()):
        self.op(en, lambda e: e.scalar_tensor_tensor(out, in0, scalar, in1, op0, op1), r, w)

    def copy(self, en, out, in_, r=(), w=()):
        if en == "act":
            self.op(en, lambda e: e.copy(out, in_), r, w)
        else:
            self.op(en, lambda e: e.tensor_copy(out, in_), r, w)

    def memset(self, en, ap, val, w=()):
        self.op(en, lambda e: e.memset(ap, val), (), w)

    def ld(self, out, in_, r=(), w=(), en="sp"):
        self.dma(en, lambda e: e.dma_start(out=out, in_=in_), r, w)


class T:
    def __init__(self, kb, stack, name, shape, dt):
        self.g = kb.reg(name)
        self.t = stack.enter_context(kb.nc.sbuf_tensor("sb_%s_%d" % (name, kb.nreg), list(shape), dt))

    def __getitem__(self, k):
        return self.t[k]


def host_constants():
    j = np.arange(128)[:, None]
    i = np.arange(128)[None, :]
    c = np.zeros((128, NCST), np.float32)
    c[:, K_ID:K_ID + 128] = (j == i)
    c[:, K_TRIF:K_TRIF + 128] = (j <= i) * (-1.0 / 16)
    c[:, K_TRIB:K_TRIB + 128] = (j >= i) * (-1.0 / 16)
    c[:, K_TRIRF:K_TRIRF + 128] = (j > i) * (-1.0 / 16)
    c[:, K_TRIRB:K_TRIRB + 128] = (j < i) * (-1.0 / 16)
    c[:, K_MF:K_MF + 512] = np.tile((j <= i).astype(np.float32), (1, 4))
    c[:, K_MB:K_MB + 512] = np.tile((j >= i).astype(np.float32), (1, 4))
    c[:, K_ONES:K_ONES + 128] = 1.0 / 256
    c[:, K_STRI:K_STRI + 128] = (j < i)
    c[:, K_ONE1:K_ONE1 + 128] = 1.0
    c[:, K_ECAP:K_ECAP + 32] = (np.arange(32) * CAP)[None, :]
    return c


def dft_mats(hf):
    tau = np.arange(S, dtype=np.int64)
    sg = tau if hf == 0 else (S - 1 - tau)
    prod = (sg[:, None] * sg[None, :NT]) % S
    th = prod.astype(np.float64) * (2 * np.pi / S)
    sc = 1.0 / math.sqrt(S)
    return ((np.cos(th) * sc).astype(ml_dtypes.bfloat16), (np.sin(th) * sc).astype(ml_dtypes.bfloat16))


def chan_dft():
    c = np.arange(128, dtype=np.int64)
    ph = ((c[:, None] * c[None, :]) % 128).astype(np.float64) * (2 * np.pi / 128)
    sc = 1.0 / math.sqrt(128)
    return np.concatenate([np.cos(ph) * sc, -np.sin(ph) * sc], axis=1).astype(ml_dtypes.bfloat16)


def build(stage=99, dbg=False):
    nc = bass.Bass("TRN2", target_bir_lowering=False)
    dr = lambda name, shape, dt, kind="ExternalInput": nc.dram_tensor(name, list(shape), dt, kind=kind).ap()
    x_own = dr("x_own", [NT, D], F32)
    x_oth = dr("x_oth", [NT, D], F32)
    mem = dr("mem", [256, D], F32)
    w_in = dr("w_in", [D, 7200], F32)
    w_lr = dr("w_lr", [D, 32], F32)
    wdec = dr("wdec", [2, 32, 512], F32)
    colsT_d = dr("colsT", [128, NCOLS], F32)
    vecs_d = dr("vecs", [1, NVEC], F32)
    cst_d = dr("cst", [128, NCST], F32)
    dftC = dr("dftC", [S, NT], BF16)
    dftS = dr("dftS", [S, NT], BF16)
    cdft_d = dr("cdft", [128, 256], BF16)
    w_br_gla = dr("w_br_gla", [1024, D], F32)
    w_br_fnet = dr("w_br_fnet", [512, D], F32)
    w_br_mem = dr("w_br_mem", [512, D], F32)
    w_mem_kv = dr("w_mem_kv", [D, 1024], F32)
    w_out = dr("w_out", [D, D], F32)
    w_router = dr("w_router", [D, E], F32)
    if stage >= 8:
        w_gu = dr("w_gu", [E, D, 2048], F32)
        w_down = dr("w_down", [E, D, D], F32)
        b_down = dr("b_down", [E, D], F32)
    out_d = dr("out", [NT, D], F32, "ExternalOutput")
    dbg_d = {}

    def dbg_out(name, shape, dt=F32):
        dbg_d[name] = dr("dbg_" + name, shape, dt, "ExternalOutput")
        return dbg_d[name]

    xbuf = dr("xbuf", [E * CAP + 1, D], BF16, "Internal")
    ybuf = dr("ybuf", [E * CAP + 1, D], F32, "Internal")
    h1_dram = dr("h1_dram", [NT, D], F32, "Internal")

    with contextlib.ExitStack() as top:
        kb = KB(nc, top)
        bcreg = nc.gpsimd.alloc_register("bcreg")
        nc.gpsimd.reg_mov(bcreg, E * CAP)
        banks = []
        for i in range(7):
            t = top.enter_context(nc.psum_tensor("pb%d" % i, [128, 512], F32))
            banks.append((t, kb.reg("pb%d" % i)))
        pbt = top.enter_context(nc.psum_tensor("pbt", [128, 1024], BF16))
        pbt_g = kb.reg("pbt")
        bstate = {"i": 0}

        def bank():
            b = banks[bstate["i"] % 7]
            bstate["i"] += 1
            return b

        cst = T(kb, top, "cst", [128, NCST], F32)
        colsT = T(kb, top, "colsT", [128, NCOLS], F32)
        identb = T(kb, top, "identb", [128, 128], BF16)
        wts = T(kb, top, "wts", [128, 64], F32)
        desti = T(kb, top, "desti", [128, 64], I32)
        desti_g = [kb.reg("desti%d" % i) for i in range(16)]
        kb.ld(cst[:], cst_d, w=[cst.g])
        kb.ld(colsT[:], colsT_d, w=[colsT.g])
        kb.copy("dve", identb[:], cst[:, K_ID:K_ID + 128], r=[cst.g], w=[identb.g])
        ident = cst[:, K_ID:K_ID + 128]

        def col(c0, n=1):
            return colsT[:, c0:c0 + n]

        def bcast_load(tile_ap, off, n, g):
            kb.ld(tile_ap, vecs_d[:, off:off + n].partition_broadcast(128), w=[g])

        def ln_tile(xt, xg, stats, sg, out_bf, og):
            kb.op("dve", lambda e: e.bn_stats(stats[:, 4:10], xt[:, 0:512]), r=[xg], w=[sg])
            kb.op("dve", lambda e: e.bn_stats(stats[:, 10:16], xt[:, 512:1024]), r=[xg], w=[sg])
            kb.op("dve", lambda e: e.bn_aggr(stats[:, 0:2], stats[:, 4:16]), r=[sg], w=[sg])
            kb.act(stats[:, 3:4], stats[:, 1:2], AF.Sqrt, r=[sg], w=[sg], bias=LN_EPS)
            kb.op("dve", lambda e: e.reciprocal(stats[:, 2:3], stats[:, 3:4]), r=[sg], w=[sg])
            if out_bf is not None:
                kb.ts("dve", out_bf, xt[:, :], stats[:, 0:1], stats[:, 2:3], ALU.subtract, ALU.mult, r=[xg, sg], w=[og])

        def transpose_to_fm(src_bf, sg_, dstT, dg, tok0, gcol, bcol):
            for c in range(8):
                kb.tr(pbt[:, c * 128:(c + 1) * 128], src_bf[:, c * 128:(c + 1) * 128], identb[:],
                      r=[sg_, identb.g], w=[pbt_g], inc=(c == 7))
            for c in range(8):
                if c % 2 == 0:
                    kb.act(dstT[:, c, tok0:tok0 + 128], pbt[:, c * 128:(c + 1) * 128], AF.Identity,
                           r=[pbt_g, colsT.g], w=[dg], bias=col(bcol + c), scale=col(gcol + c))
                else:
                    kb.ts("dve", dstT[:, c, tok0:tok0 + 128], pbt[:, c * 128:(c + 1) * 128], col(gcol + c), col(bcol + c),
                          ALU.mult, ALU.add, r=[pbt_g, colsT.g], w=[dg])

        def wload(tile, g, src, ncols, kchunks=8, c0=0, r0=0, kstep=8):
            for k0 in range(0, kchunks, kstep):
                k1 = min(kchunks, k0 + kstep)
                srcv = src[r0 + k0 * 128:r0 + k1 * 128, c0:c0 + ncols].rearrange("(k p) f -> p k f", p=128)
                kb.ld(tile[:, k0:k1, 0:ncols], srcv, w=[g], en="pool")

        mkT = T(kb, top, "mkT", [128, 4, 256], BF16)
        mv = T(kb, top, "mv", [128, 2, 512], BF16)
        xg = kb.reg("xbuf")
        yg = kb.reg("ybuf")
        with contextlib.ExitStack() as ph:
            ztf = T(kb, ph, "ztf", [1, 1024], F32)
            kb.memset("pool", ztf[:], 0.0, w=[ztf.g])
            kb.ld(ybuf[E * CAP:E * CAP + 1, :], ztf[:], r=[ztf.g], w=[yg])
            wkv = T(kb, ph, "wkv", [128, 8, 1024], BF16)
            wload(wkv, wkv.g, w_mem_kv, 1024)
            memT = T(kb, ph, "memT", [128, 8, 256], BF16)
            for mt in range(2):
                xt = T(kb, ph, "memx%d" % mt, [128, 1024], F32)
                st = T(kb, ph, "memst%d" % mt, [128, 16], F32)
                xb = T(kb, ph, "memxb%d" % mt, [128, 1024], BF16)
                kb.ld(xt[:], mem[mt * 128:(mt + 1) * 128, :], w=[xt.g])
                ln_tile(xt, xt.g, st, st.g, xb[:], xb.g)
                transpose_to_fm(xb, xb.g, memT, memT.g, mt * 128, C_MG, C_MB)
            for h in range(4):
                pb, pg = bank()
                for k in range(8):
                    kb.mm(pb[:, 0:256], wkv[:, k, h * 128:(h + 1) * 128], memT[:, k, :], k == 0, k == 7,
                          r=[wkv.g, memT.g], w=[pg])
                kb.copy("dve", mkT[:, h, :], pb[:, 0:256], r=[pg], w=[mkT.g])
            for mt in range(2):
                pb, pg = bank()
                for k in range(8):
                    kb.mm(pb[:, :], memT[:, k, mt * 128:(mt + 1) * 128], wkv[:, k, 512:1024], k == 0, k == 7,
                          r=[wkv.g, memT.g], w=[pg])
                kb.copy("dve", mv[:, mt, :], pb[:, :], r=[pg], w=[mv.g])
            kb.barrier()
        if dbg and stage == 0:
            o1 = dbg_out("mkT", [128, 4 * 256], BF16)
            kb.ld(o1, mkT[:].rearrange("p a b -> p (a b)"), r=[mkT.g])
            o2 = dbg_out("mv", [128, 2 * 512], BF16)
            kb.ld(o2, mv[:].rearrange("p a b -> p (a b)"), r=[mv.g])


        mix = top.enter_context(contextlib.ExitStack())
        hT = T(kb, mix, "hT", [128, 8, NT], BF16)
        slots = T(kb, mix, "slots", [128, 16, 1024], BF16)
        slot_g = [kb.reg("slot%d" % i) for i in range(17)]
        FT = T(kb, mix, "FT", [128, 4, NT], BF16)
        mixA = top.enter_context(contextlib.ExitStack())
        stB = T(kb, mixA, "stB", [128, 1024], F32)
        stBb = T(kb, mixA, "stBb", [128, 1024], BF16)
        wk = T(kb, mixA, "wk", [128, 8, 512], BF16)
        wv = T(kb, mixA, "wv", [128, 8, 1024], BF16)
        wlr = T(kb, mixA, "wlr", [128, 8, 32], BF16)
        wdec_sb = T(kb, mixA, "wdec_sb", [32, 2, 512], F32)
        bkv = T(kb, mixA, "bkv", [128, 2048], F32)
        wload(wk, wk.g, w_in, 512, c0=OK_)
        wload(wv, wv.g, w_in, 1024, c0=OV)
        wload(wlr, wlr.g, w_lr, 32)
        kb.ld(wdec_sb[:, 0, :], wdec[0], w=[wdec_sb.g])
        kb.ld(wdec_sb[:, 1, :], wdec[1], w=[wdec_sb.g])
        bcast_load(bkv[:, 0:2048], V_BK, 2048, bkv.g)
        kb.memset("pool", stB[:], 0.0, w=[stB.g])

        def state_pass(ph, xsrc, own, dirn, f_tok, wfn, tmp, hTd):
            st = tmp["st"]
            pend = [None]
            blocks = list(range(4)) if own else list(range(3, -1, -1))
            hregs = [kb.reg("hblk%d" % i) for i in range(4)]
            fcnt = [0]
            zcnt = [0]

            def front_tile(blk, t):
                p = fcnt[0] % 2
                fcnt[0] += 1
                xt, stt_, xb = tmp["x"][p], tmp["stat"][p], tmp["xb"][p]
                r0 = blk * 512 + t * 128
                kb.ld(xt[:], xsrc[r0:r0 + 128, :], w=[xt.g])
                if own:
                    for _ in range(6):
                        zi = zcnt[0]
                        zcnt[0] += 1
                        kb.ld(xbuf[zi * 128:(zi + 1) * 128, :], slots[:, 0, :], r=[slot_g[0]], w=[xg], en="pool")
                ln_tile(xt, xt.g, stt_, stt_.g, xb[:], xb.g)
                transpose_to_fm(xb, xb.g, hTd, hregs[blk], r0, C_LNG, C_LNB)

            for t in range(4):
                front_tile(blocks[0], t)
            for bi, blk in enumerate(blocks):
                nxt_blk = blocks[bi + 1] if bi + 1 < 4 else None
                ftl = [0]
                hv = lambda k, a, b_, blk=blk: hTd[:, k, blk * 512 + a: blk * 512 + b_]
                hg = hregs[blk]
                lrT = tmp["lrT"]
                pb, pg = bank()
                for k in range(8):
                    kb.mm(pb[0:16, :], wlr[:, k, dirn * 16:(dirn + 1) * 16], hv(k, 0, 512), k == 0, k == 7,
                          r=[wlr.g, hg], w=[pg])
                kb.act(lrT[0:16, :], pb[0:16, :], AF.Identity, r=[pg, colsT.g], w=[lrT.g],
                       bias=colsT[0:16, C_BLF + dirn:C_BLF + dirn + 1])
                tiles = range(4) if own else range(3, -1, -1)

                def stageA(t, blk=blk, hv=hv, hg=hg):
                    p = t % 2
                    gt = blk * 4 + t
                    ktok, vtok, e1, L, kst, dec = (tmp[n][p] for n in ("ktok", "vtok", "e1", "L", "kst", "dec"))
                    pb, pg = bank()
                    for k in range(8):
                        kb.mm(pb[:, :], hv(k, t * 128, (t + 1) * 128), wk[:, k, :], k == 0, k == 7, r=[hg, wk.g], w=[pg])
                    kb.tt("dve", ktok[:], pb[:, :], bkv[:, 0:512], ALU.add, r=[pg, bkv.g], w=[ktok.g])
                    for hh in range(2):
                        pb, pg = bank()
                        for k in range(8):
                            kb.mm(pb[:, :], hv(k, t * 128, (t + 1) * 128), wv[:, k, hh * 512:(hh + 1) * 512], k == 0, k == 7,
                                  r=[hg, wv.g], w=[pg])
                        kb.tt("dve", vtok[:, hh * 512:(hh + 1) * 512], pb[:, :], bkv[:, 512 + hh * 512:1024 + hh * 512], ALU.add,
                              r=[pg, bkv.g], w=[vtok.g])
                    pb, pg = bank()
                    for k in range(8):
                        kb.mm(pb[:, :], hv(k, t * 128, (t + 1) * 128), wfn[:, k, :], k == 0, k == 7, r=[hg, wfn.g], w=[pg])
                    fidx = gt if own else 16 + gt
                    kb.tt("dve", f_tok[:, fidx, :], pb[:, :], bkv[:, 1536:2048], ALU.add, r=[pg, bkv.g], w=[f_tok.g])
                    pb, pg = bank()
                    kb.mm(pb[:, :], lrT[0:32, t * 128:(t + 1) * 128], wdec_sb[0:32, dirn, :], True, True,
                          r=[lrT.g, wdec_sb.g], w=[pg])
                    kb.act(e1[:], pb[:, :], AF.Exp, r=[pg], w=[e1.g], scale=-1.0)
                    kb.act(L[:], e1[:], AF.Ln, r=[e1.g], w=[L.g], bias=1.0)

                def stageB(t, blk=blk):
                    p = t % 2
                    gt = blk * 4 + t
                    ktok, vtok, e1, L, kst, dec = (tmp[n][p] for n in ("ktok", "vtok", "e1", "L", "kst", "dec"))
                    pb, pg = bank()
                    tri = cst[:, K_TRIRF:K_TRIRF + 128] if dirn == 0 else cst[:, K_TRIRB:K_TRIRB + 128]
                    kb.mm(pb[:, :], tri, L[:], True, True, r=[cst.g, L.g], w=[pg])
                    kb.act(e1[:], pb[:, :], AF.Exp, r=[pg], w=[e1.g])
                    kb.tt("pool", kst[:], ktok[:], e1[:], ALU.mult, r=[ktok.g, e1.g], w=[kst.g])
                    pb, pg = bank()
                    for h in range(4):
                        kb.mm(pb[:, h:h + 1], L[:, h * 128:(h + 1) * 128], cst[:, K_TRIF + 127:K_TRIF + 128], True, True,
                              r=[L.g, cst.g], w=[pg], inc=(h == 3))
                    kb.act(dec[:], pb[:, 0:4], AF.Exp, r=[pg], w=[dec.g])
                    for hp in range(2):
                        pb, pg = bank()
                        for h2 in range(2):
                            h = hp * 2 + h2
                            kb.mm(pb[:, h2 * 256:(h2 + 1) * 256], kst[:, h * 128:(h + 1) * 128], vtok[:, h * 256:(h + 1) * 256],
                                  True, True, r=[kst.g, vtok.g], w=[pg], inc=(h2 == 1))
                        for h2 in range(2):
                            h = hp * 2 + h2
                            kb.stt("dve", st[:, h * 256:(h + 1) * 256], st[:, h * 256:(h + 1) * 256], dec[:, h:h + 1],
                                   pb[:, h2 * 256:(h2 + 1) * 256], ALU.mult, ALU.add, r=[pg, dec.g], w=[st.g])
                    if own and gt < 15:
                        kb.copy("act", slots[:, gt + 1, :], st[:], r=[st.g], w=[slot_g[gt + 1]])

                for t in tiles:
                    stageA(t)
                    if nxt_blk is not None:
                        front_tile(nxt_blk, ftl[0])
                        ftl[0] += 1
                    if pend[0] is not None:
                        pend[0]()
                    pend[0] = (lambda t=t, f=stageB: f(t))
            if pend[0] is not None:
                pend[0]()
                pend[0] = None

        with contextlib.ExitStack() as ph:
            f_tok = T(kb, ph, "f_tok", [128, 32, 512], BF16)
            with contextlib.ExitStack() as ph2:
                wfn = T(kb, ph2, "wfn", [128, 8, 512], BF16)
                wload(wfn, wfn.g, w_in, 512, c0=OFN)
                tmp = {"st": stB}
                class V:
                    def __init__(self, ap, name):
                        self.ap = ap
                        self.g = kb.reg(name)

                    def __getitem__(self, k):
                        return self.ap[k]
                hTo = V(slots[:].rearrange("p a b -> p (a b)").rearrange("p (k f) -> p k f", k=8), "hTo")
                tmp["xb"] = [V(FT[:, 2, i * 1024:(i + 1) * 1024], "xb%d" % i) for i in range(2)]
                tmp["vtok"] = [V(FT[:, 3, i * 1024:(i + 1) * 1024], "vtok%d" % i) for i in range(2)]
                tmp["lrT"] = T(kb, ph2, "lrT", [32, 512], F32)
                kb.memset("pool", tmp["lrT"][:], 1.0, w=[tmp["lrT"].g])
                x1 = T(kb, ph2, "sp_x", [128, 1024], F32)
                tmp["x"] = [x1, V(FT[:, 0, :].bitcast(F32), "sp_x2")]
                for n, shp, dt in (("stat", [128, 16], F32),
                                   ("ktok", [128, 512], F32), ("e1", [128, 512], F32),
                                   ("L", [128, 512], F32), ("kst", [128, 512], BF16), ("dec", [128, 4], F32)):
                    tmp[n] = [T(kb, ph2, "sp_%s%d" % (n, i), shp, dt) for i in range(2)]
                state_pass(ph2, x_oth, False, 1, f_tok, wfn, tmp, hTo)
                kb.copy("act", stBb[:], stB[:], r=[stB.g], w=[stBb.g])
                kb.barrier()
                kb.memset("pool", slots[:, 0, :], 0.0, w=[slot_g[0]])
                kb.ld(xbuf[E * CAP:E * CAP + 1, :], slots[0:1, 0, :], r=[slot_g[0]], w=[xg])
                stF = T(kb, ph2, "stF", [128, 1024], F32)
                kb.memset("pool", stF[:], 0.0, w=[stF.g])
                tmp["st"] = stF
                state_pass(ph2, x_own, True, 0, f_tok, wfn, tmp, hT)
                if dbg and stage == 2:
                    kb.ld(dbg_out("stB", [128, 1024]), stB[:], r=[stB.g])
                    kb.ld(dbg_out("stF", [128, 1024]), stF[:], r=[stF.g])
                    kb.ld(dbg_out("ftok", [128, 32 * 512], BF16), f_tok[:].rearrange("p a b -> p (a b)"), r=[f_tok.g])
                    kb.ld(dbg_out("hT", [128, 8 * NT], BF16), hT[:].rearrange("p a b -> p (a b)"), r=[hT.g])
                kb.barrier()
            if stage >= 3:
                with contextlib.ExitStack() as ph3:
                    cd = T(kb, ph3, "cd", [128, 256], BF16)
                    kb.ld(cd[:], cdft_d, w=[cd.g])
                    ring = [T(kb, ph3, "dring%d" % i, [128, 2, 8, 256], BF16) for i in range(3)]
                    pq = [T(kb, ph3, "pq%d" % i, [128, 4, 512], BF16) for i in range(2)]
                    ri = 0
                    for blk in range(8):
                        accs = [bank() for _ in range(4)]
                        for k0 in range(0, 32, 8):
                            rt = ring[ri % 3]
                            ri += 1
                            for ci, src in enumerate((dftC, dftS)):
                                kb.ld(rt[:, ci, :, :], src[k0 * 128:(k0 + 8) * 128, blk * 256:(blk + 1) * 256].rearrange("(k p) f -> p k f", p=128),
                                      w=[rt.g])
                            for kk in range(8):
                                kc = k0 + kk
                                for g in range(4):
                                    pb, pg = accs[g]
                                    last = (kc == 31)
                                    kb.mm(pb[:, :], f_tok[:, kc, g * 128:(g + 1) * 128], rt[:, :, kk, :], kc == 0, last,
                                          r=[f_tok.g, rt.g], w=[pg], inc=(last or (kk == 7 and g == 3)))
                        pqt = pq[blk % 2]
                        for g in range(4):
                            pb, pg = accs[g]
                            kb.copy("act" if g % 2 else "dve", pqt[:, g, :], pb[:, :], r=[pg], w=[pqt.g])
                        for g in range(4):
                            pb, pg = bank()
                            kb.mm(pb[:, 0:256], cd[:, 0:128], pqt[:, g, 0:256], True, False, r=[cd.g, pqt.g], w=[pg], inc=False)
                            kb.mm(pb[:, 0:256], cd[:, 128:256], pqt[:, g, 256:512], False, True, r=[cd.g, pqt.g], w=[pg])
                            kb.copy("act" if g % 2 else "dve", FT[:, g, blk * 256:(blk + 1) * 256], pb[:, 0:256], r=[pg], w=[FT.g])
                    if dbg and stage == 3:
                        kb.ld(dbg_out("FT", [128, 4 * NT], BF16), FT[:].rearrange("p a b -> p (a b)"), r=[FT.g])
                    kb.barrier()


        if stage >= 4:
            with contextlib.ExitStack() as ph4:
                wq = T(kb, ph4, "wq", [128, 8, 512], BF16)
                wload(wq, wq.g, w_in, 512, c0=OQ)
                bqs = T(kb, ph4, "bqs", [128, 4], F32)
                QS = 128.0 ** -0.5
                kb.ts("dve", bqs[:], colsT[:, C_BQ:C_BQ + 4], QS, None, ALU.mult, r=[colsT.g], w=[bqs.g])
                wrc = [T(kb, ph4, "wrc%d" % i, [128, 8, 128], BF16) for i in range(4)]
                BL = 256
                qT = T(kb, ph4, "qT", [128, 4, BL], F32)
                kT = T(kb, ph4, "kT", [128, 4, BL], F32)
                ktok = T(kb, ph4, "ktok4", [128, 2, 512], F32)
                vtok = T(kb, ph4, "vtok4", [128, 2, 1024], BF16)
                rs = T(kb, ph4, "rs", [128, 8, BL], BF16)
                lrT2 = [T(kb, ph4, "lrT4_%d" % i, [32, BL], F32) for i in range(2)]
                for i in range(2):
                    kb.memset("pool", lrT2[i][:], 1.0, w=[lrT2[i].g])
                e1 = T(kb, ph4, "e1_4", [128, 512], F32)
                Ls = [T(kb, ph4, "L4_%d" % i, [128, 512], F32) for i in range(2)]
                Eq = T(kb, ph4, "Eq", [128, 512], F32)
                Ek = T(kb, ph4, "Ek", [128, 512], F32)
                Eqs = [Eq, T(kb, ph4, "Eq2", [128, 512], F32)]
                Eks = [Ek, T(kb, ph4, "Ek2", [128, 512], F32)]
                e1s = [e1, T(kb, ph4, "e1_4b", [128, 512], F32)]
                decB = T(kb, ph4, "decB", [128, 4], F32)
                qin = [T(kb, ph4, "qin%d" % i, [128, 4, 128], BF16) for i in range(2)]
                kin = [T(kb, ph4, "kin%d" % i, [128, 4, 128], BF16) for i in range(2)]
                kst = T(kb, ph4, "kst4", [128, 512], BF16)
                ta = T(kb, ph4, "ta", [128, 512], F32)
                tb = T(kb, ph4, "tb", [128, 512], F32)
                attb = T(kb, ph4, "attb", [128, 4, 128], BF16)
                sq = [ta, tb]
                rstd = T(kb, ph4, "rstd", [128, 512], F32)
                ton = T(kb, ph4, "ton", [128, 256], F32)
                wri = 0
                wreq = [0]
                for blk in range(NT // BL - 1, -1, -1):
                    tok0 = blk * BL
                    for h in range(4):
                        pb, pg = bank()
                        for k in range(8):
                            kb.mm(pb[:, 0:BL], wq[:, k, h * 128:(h + 1) * 128], hT[:, k, tok0:tok0 + BL], k == 0, k == 7, r=[wq.g, hT.g], w=[pg])
                        kb.act(qT[:, h, :], pb[:, 0:BL], AF.Identity, r=[pg, bqs.g], w=[qT.g], bias=bqs[:, h:h + 1], scale=QS)
                        pb, pg = bank()
                        for k in range(8):
                            kb.mm(pb[:, 0:BL], wk[:, k, h * 128:(h + 1) * 128], hT[:, k, tok0:tok0 + BL], k == 0, k == 7, r=[wk.g, hT.g], w=[pg])
                        kb.act(kT[:, h, :], pb[:, 0:BL], AF.Identity, r=[pg, colsT.g], w=[kT.g], bias=col(C_BK + h))
                    for hc in range(8):
                        while wreq[0] < min(wri + 4, 8 * (NT // BL)):
                            wt2 = wrc[wreq[0] % 4]
                            wload(wt2, wt2.g, w_in, 128, c0=OR + (wreq[0] % 8) * 128, kstep=8)
                            wreq[0] += 1
                        wt = wrc[wri % 4]
                        wri += 1
                        pb, pg = bank()
                        for k in range(8):
                            kb.mm(pb[:, 0:BL], wt[:, k, :], hT[:, k, tok0:tok0 + BL], k == 0, k == 7, r=[wt.g, hT.g], w=[pg])
                        kb.act(rs[:, hc, :], pb[:, 0:BL], AF.Silu, r=[pg, colsT.g], w=[rs.g], bias=col(C_BR + hc))
                    for dirn in range(2):
                        pb, pg = bank()
                        for k in range(8):
                            kb.mm(pb[0:16, 0:BL], wlr[:, k, dirn * 16:(dirn + 1) * 16], hT[:, k, tok0:tok0 + BL], k == 0, k == 7,
                                  r=[wlr.g, hT.g], w=[pg])
                        kb.act(lrT2[dirn][0:16, :], pb[0:16, 0:BL], AF.Identity, r=[pg, colsT.g], w=[lrT2[dirn].g],
                               bias=colsT[0:16, C_BLF + dirn:C_BLF + dirn + 1])
                    for t in range(BL // 128):
                        pb, pg = bank()
                        for k in range(8):
                            kb.mm(pb[:, :], hT[:, k, tok0 + t * 128:tok0 + (t + 1) * 128], wk[:, k, :], k == 0, k == 7, r=[hT.g, wk.g], w=[pg])
                        kb.tt("dve", ktok[:, t, :], pb[:, :], bkv[:, 0:512], ALU.add, r=[pg, bkv.g], w=[ktok.g])
                        for hh in range(2):
                            pb, pg = bank()
                            for k in range(8):
                                kb.mm(pb[:, :], hT[:, k, tok0 + t * 128:tok0 + (t + 1) * 128], wv[:, k, hh * 512:(hh + 1) * 512], k == 0, k == 7,
                                      r=[hT.g, wv.g], w=[pg])
                            kb.tt("dve", vtok[:, t, hh * 512:(hh + 1) * 512], pb[:, :], bkv[:, 512 + hh * 512:1024 + hh * 512], ALU.add,
                                  r=[pg, bkv.g], w=[vtok.g])
                    for t in range(BL // 128 - 1, -1, -1):
                        gt = blk * (BL // 128) + t
                        i0 = t * 128
                        zbs = []
                        for dirn in range(2):
                            pb, pg = bank()
                            kb.mm(pb[:, :], lrT2[dirn][0:32, i0:i0 + 128], wdec_sb[0:32, dirn, :], True, True,
                                  r=[lrT2[dirn].g, wdec_sb.g], w=[pg])
                            zbs.append((pb, pg))
                        for dirn in range(2):
                            pb, pg = zbs[dirn]
                            kb.act(e1s[dirn][:], pb[:, :], AF.Exp, r=[pg], w=[e1s[dirn].g], scale=-1.0)
                        for dirn in range(2):
                            kb.act(Ls[dirn][:], e1s[dirn][:], AF.Ln, r=[e1s[dirn].g], w=[Ls[dirn].g], bias=1.0)
                        abs_ = []
                        for dirn in range(2):
                            pb, pg = bank()
                            tri = cst[:, K_TRIF:K_TRIF + 128] if dirn == 0 else cst[:, K_TRIB:K_TRIB + 128]
                            for h in range(4):
                                kb.mm(pb[:, h * 128:(h + 1) * 128], Ls[dirn][:, h * 128:(h + 1) * 128], tri, True, True,
                                      r=[Ls[dirn].g, cst.g], w=[pg], inc=(h == 3))
                            abs_.append((pb, pg))
                        pbr, pgr = bank()
                        kb.mm(pbr[:, :], cst[:, K_TRIRB:K_TRIRB + 128], Ls[1][:], True, True, r=[cst.g, Ls[1].g], w=[pgr])
                        for dirn in range(2):
                            pb, pg = abs_[dirn]
                            kb.act(Eqs[dirn][:], pb[:, :], AF.Exp, r=[pg], w=[Eqs[dirn].g])
                            kb.act(Eks[dirn][:], pb[:, :], AF.Exp, r=[pg], w=[Eks[dirn].g], scale=-1.0)
                        kb.act(e1s[0][:], pbr[:, :], AF.Exp, r=[pgr], w=[e1s[0].g])
                        kb.copy("pool", decB[:], Eqs[1][:].rearrange("p (h i) -> p h i", h=4)[:, :, 0], r=[Eqs[1].g], w=[decB.g])
                        for dirn in range(2):
                            kb.tt("pool", qin[dirn][:], qT[:, :, i0:i0 + 128], Eqs[dirn][:].rearrange("p (h i) -> p h i", h=4), ALU.mult,
                                  r=[qT.g, Eqs[dirn].g], w=[qin[dirn].g])
                            kb.tt("dve", kin[dirn][:], kT[:, :, i0:i0 + 128], Eks[dirn][:].rearrange("p (h i) -> p h i", h=4), ALU.mult,
                                  r=[kT.g, Eks[dirn].g], w=[kin[dirn].g])
                        kb.tt("pool", kst[:], ktok[:, t, :], e1s[0][:], ALU.mult, r=[ktok.g, e1s[0].g], w=[kst.g])
                        pbF, pgF = bank()
                        pbB, pgB = bank()
                        for h in range(4):
                            kb.mm(pbF[:, h * 128:(h + 1) * 128], kin[0][:, h, :], qin[0][:, h, :], True, True,
                                  r=[kin[0].g, qin[0].g], w=[pgF], inc=(h == 3))
                        for h in range(4):
                            kb.mm(pbB[:, h * 128:(h + 1) * 128], kin[1][:, h, :], qin[1][:, h, :], True, True,
                                  r=[kin[1].g, qin[1].g], w=[pgB], inc=(h == 3))
                        kb.tt("dve", ta[:], pbF[:, :], cst[:, K_MF:K_MF + 512], ALU.mult, r=[pgF, cst.g], w=[ta.g])
                        kb.tt("dve", tb[:], pbB[:, :], cst[:, K_MB:K_MB + 512], ALU.mult, r=[pgB, cst.g], w=[tb.g])
                        kb.tt("pool", attb[:].rearrange("p h i -> p (h i)"), ta[:], tb[:], ALU.add, r=[ta.g, tb.g], w=[attb.g])
                        obanks = [bank(), bank()]
                        for hp in range(2):
                            pb, pg = obanks[hp]
                            for h2 in range(2):
                                h = hp * 2 + h2
                                for c in range(2):
                                    vs = h * 256 + c * 128
                                    oc = pb[:, (h2 * 2 + c) * 128:(h2 * 2 + c + 1) * 128]
                                    kb.mm(oc, vtok[:, t, vs:vs + 128], attb[:, h, :], True, False, r=[vtok.g, attb.g], w=[pg], inc=False)
                                    kb.mm(oc, slots[:, gt, vs:vs + 128], qin[0][:, h, :], False, False, r=[slot_g[gt], qin[0].g], w=[pg], inc=False)
                                    kb.mm(oc, stBb[:, vs:vs + 128], qin[1][:, h, :], False, True, r=[stBb.g, qin[1].g], w=[pg],
                                          inc=(h2 == 1 and c == 1))
                        msb, msg = bank()
                        for hp in range(2):
                            pb, pg = obanks[hp]
                            kb.act(sq[hp][:], pb[:, :], AF.Square, r=[pg], w=[sq[hp].g])
                        for h in range(4):
                            hp, h2 = h // 2, h % 2
                            kb.mm(msb[:, h * 128:(h + 1) * 128], cst[:, K_ONES:K_ONES + 128], sq[hp][:, (h2 * 2) * 128:(h2 * 2 + 1) * 128],
                                  True, False, r=[cst.g, sq[hp].g], w=[msg], inc=False)
                            kb.mm(msb[:, h * 128:(h + 1) * 128], cst[:, K_ONES:K_ONES + 128], sq[hp][:, (h2 * 2 + 1) * 128:(h2 * 2 + 2) * 128],
                                  False, True, r=[cst.g, sq[hp].g], w=[msg], inc=(h == 3))
                        kb.act(e1[:], msb[:, :], AF.Sqrt, r=[msg], w=[e1.g], bias=RMS_EPS)
                        kb.op("dve", lambda e, a=rstd[:], b_=e1[:]: e.reciprocal(a, b_), r=[e1.g], w=[rstd.g])
                        for hp in range(2):
                            pb, pg = obanks[hp]
                            ov = pb[:, :].rearrange("p (h c i) -> p h c i", h=2, c=2)
                            rv = rstd[:].rearrange("p (h i) -> p h i", h=4)[:, hp * 2:hp * 2 + 2, :]
                            sv = slots[:, gt, :].rearrange("p (h c i) -> p h c i", h=4, c=2)
                            rsv = rs[:].rearrange("p (h c) i -> p h c i", c=2)
                            for c in range(2):
                                tv = ton[:].rearrange("p (h i) -> p h i", h=2)
                                kb.stt("dve", tv, ov[:, :, c, :], col(C_GN + c), rv, ALU.mult, ALU.mult, r=[pg, rstd.g, colsT.g], w=[ton.g])
                                kb.tt("pool", sv[:, hp * 2:hp * 2 + 2, c, :], tv, rsv[:, hp * 2:hp * 2 + 2, c, i0:i0 + 128], ALU.mult,
                                      r=[ton.g, rs.g], w=[slot_g[gt]])
                        for hp in range(2):
                            pb, pg = bank()
                            for h2 in range(2):
                                h = hp * 2 + h2
                                kb.mm(pb[:, h2 * 256:(h2 + 1) * 256], kst[:, h * 128:(h + 1) * 128], vtok[:, t, h * 256:(h + 1) * 256],
                                      True, True, r=[kst.g, vtok.g], w=[pg], inc=(h2 == 1))
                            for h2 in range(2):
                                h = hp * 2 + h2
                                kb.stt("dve", stB[:, h * 256:(h + 1) * 256], stB[:, h * 256:(h + 1) * 256], decB[:, h:h + 1],
                                       pb[:, h2 * 256:(h2 + 1) * 256], ALU.mult, ALU.add, r=[pg, decB.g], w=[stB.g])
                        kb.copy("act", stBb[:], stB[:], r=[stB.g], w=[stBb.g])
                if dbg and stage == 4:
                    kb.ld(dbg_out("og", [128, 16 * 1024], BF16), slots[:].rearrange("p a b -> p (a b)"), r=slot_g)
                    kb.ld(dbg_out("FT", [128, 4 * NT], BF16), FT[:].rearrange("p a b -> p (a b)"), r=[FT.g])
                kb.barrier()


        if stage >= 5:
            mixA.close()
            omT = T(kb, mix, "omT", [128, 4, NT], BF16)
            onesb = T(kb, mix, "onesb", [128, 128], BF16)
            kb.copy("dve", onesb[:], cst[:, K_ONE1:K_ONE1 + 128], r=[cst.g], w=[onesb.g])
            with contextlib.ExitStack() as ph5:
                wmq = T(kb, ph5, "wmq", [128, 8, 512], BF16)
                wload(wmq, wmq.g, w_in, 512, c0=OMQ)
                MS = 128.0 ** -0.5
                bms = T(kb, ph5, "bms", [128, 4], F32)
                kb.ts("dve", bms[:], colsT[:, C_BMQ:C_BMQ + 4], MS, None, ALU.mult, r=[colsT.g], w=[bms.g])
                mqT = T(kb, ph5, "mqT", [128, 4, 512], BF16)
                eT = [T(kb, ph5, "eT%d" % i, [128, 2, 512], BF16) for i in range(2)]
                rden = [T(kb, ph5, "rden%d" % i, [128, 512], F32) for i in range(2)]
                for blk in range(4):
                    tok0 = blk * 512
                    for h in range(4):
                        pb, pg = bank()
                        for k in range(8):
                            kb.mm(pb[:, :], wmq[:, k, h * 128:(h + 1) * 128], hT[:, k, tok0:tok0 + 512], k == 0, k == 7, r=[wmq.g, hT.g], w=[pg])
                        kb.act(mqT[:, h, :], pb[:, :], AF.Identity, r=[pg, bms.g], w=[mqT.g], bias=bms[:, h:h + 1], scale=MS)
                    for h in range(4):
                        et = eT[h % 2]
                        for mc in range(2):
                            pb, pg = bank()
                            kb.mm(pb[:, :], mkT[:, h, mc * 128:(mc + 1) * 128], mqT[:, h, :], True, True, r=[mkT.g, mqT.g], w=[pg])
                            kb.act(et[:, mc, :], pb[:, :], AF.Exp, r=[pg], w=[et.g])
                        pbo, pgo = bank()
                        pbd, pgd = bank()
                        for mc in range(2):
                            kb.mm(pbo[:, :], mv[:, mc, h * 128:(h + 1) * 128], et[:, mc, :], mc == 0, mc == 1, r=[mv.g, et.g], w=[pgo])
                        for mc in range(2):
                            kb.mm(pbd[:, :], onesb[:], et[:, mc, :], mc == 0, mc == 1, r=[onesb.g, et.g], w=[pgd])
                        rd = rden[h % 2]
                        kb.op("dve", lambda e, a=rd[:], b_=pbd[:, :]: e.reciprocal(a, b_), r=[pgd], w=[rd.g])
                        kb.tt("dve", omT[:, h, tok0:tok0 + 512], pbo[:, :], rd[:], ALU.mult, r=[pgo, rd.g], w=[omT.g])
                if dbg and stage == 5:
                    kb.ld(dbg_out("omT", [128, 4 * NT], BF16), omT[:].rearrange("p a b -> p (a b)"), r=[omT.g])
                kb.barrier()

        if stage >= 6:
            p7 = top.enter_context(contextlib.ExitStack())
            sprev = T(kb, p7, "sprev", [128, 32], F32)
            kb.memset("pool", sprev[:], 0.0, w=[sprev.g])
            wout = T(kb, p7, "wout", [128, 8, 1024], BF16)
            wload(wout, wout.g, w_out, 1024)
            wrt = T(kb, p7, "wrt", [128, 8, 32], F32)
            kb.ld(wrt[:], w_router.rearrange("(k p) e -> p k e", p=128), w=[wrt.g])
            bc = T(kb, p7, "bc", [128, 4, 1024], F32)
            brt = T(kb, p7, "brt", [128, 32], F32)
            bcast_load(brt[:], V_BRT, 32, brt.g)
            with contextlib.ExitStack() as pz:
                tmpb = T(kb, pz, "tmpb", [128, 2, 1024], F32)
                bcast_load(bc[:, 0, :], V_LNG, 1024, bc.g)
                bcast_load(tmpb[:, 0, :], V_LNB, 1024, tmpb.g)
                bcast_load(tmpb[:, 1, :], V_BOUT, 1024, tmpb.g)
                bcast_load(bc[:, 2, :], V_L1G, 1024, bc.g)
                bcast_load(bc[:, 3, :], V_L1B, 1024, bc.g)
                kb.ts("dve", bc[:, 0, :], bc[:, 0, :], ALPHA, None, ALU.mult, r=[bc.g], w=[bc.g])
                kb.stt("dve", bc[:, 1, :], tmpb[:, 0, :], ALPHA, tmpb[:, 1, :], ALU.mult, ALU.add, r=[tmpb.g], w=[bc.g])
                kb.barrier()
            h1g = kb.reg("h1_dram")
            for half in range(2):
                mergedT = T(kb, p7, "mergedT%d" % half, [128, 8, 1024], BF16) if half == 0 else mergedT
                with contextlib.ExitStack() as p6:
                    wsets = []
                    for i in range(2):
                        wsets.append({"g": [T(kb, p6, "wg%d_%d" % (i, j), [128, 8, 128], BF16) for j in range(3)],
                                      "bg": T(kb, p6, "wbg%d" % i, [128, 8, 128], BF16),
                                      "bf": T(kb, p6, "wbf%d" % i, [128, 4, 128], BF16),
                                      "bm": T(kb, p6, "wbm%d" % i, [128, 4, 128], BF16)})
                    sig = [T(kb, p6, "sig%d" % i, [128, 3, 512], F32) for i in range(2)]
                    t0s = [T(kb, p6, "t0s%d" % i, [128, 512], F32) for i in range(2)]
                    t1s = [T(kb, p6, "t1s%d" % i, [128, 512], F32) for i in range(2)]
                    it = 0

                    def load_chunk(c2):
                        ws2 = wsets[c2 % 2]
                        for j2 in range(3):
                            wload(ws2["g"][j2], ws2["g"][j2].g, w_in, 128, c0=OG + j2 * 1024 + c2 * 128)
                        wload(ws2["bg"], ws2["bg"].g, w_br_gla, 128, c0=c2 * 128)
                        wload(ws2["bf"], ws2["bf"].g, w_br_fnet, 128, kchunks=4, c0=c2 * 128)
                        wload(ws2["bm"], ws2["bm"].g, w_br_mem, 128, kchunks=4, c0=c2 * 128)

                    load_chunk(0)
                    for c in range(8):
                        ws = wsets[c % 2]
                        if c + 1 < 8:
                            load_chunk(c + 1)
                        for b2 in range(2):
                            tok0 = half * 1024 + b2 * 512
                            tl0 = tok0 // 128
                            sg_, t0, t1 = sig[it % 2], t0s[it % 2], t1s[it % 2]
                            it += 1
                            for j in range(3):
                                pb, pg = bank()
                                for k in range(8):
                                    kb.mm(pb[:, :], ws["g"][j][:, k, :], hT[:, k, tok0:tok0 + 512], k == 0, k == 7, r=[ws["g"][j].g, hT.g], w=[pg])
                                kb.act(sg_[:, j, :], pb[:, :], AF.Sigmoid, r=[pg, colsT.g], w=[sg_.g], bias=col(C_BG + j * 8 + c))
                            pbg, pgg = bank()
                            for hc in range(8):
                                kb.mm(pbg[:, :], ws["bg"][:, hc, :], slots[:, tl0:tl0 + 4, hc * 128:(hc + 1) * 128], hc == 0, hc == 7,
                                      r=[ws["bg"].g] + slot_g[tl0:tl0 + 4], w=[pgg])
                            pbf, pgf = bank()
                            for g in range(4):
                                kb.mm(pbf[:, :], ws["bf"][:, g, :], FT[:, g, tok0:tok0 + 512], g == 0, g == 3, r=[ws["bf"].g, FT.g], w=[pgf])
                            pbm, pgm = bank()
                            for h in range(4):
                                kb.mm(pbm[:, :], ws["bm"][:, h, :], omT[:, h, tok0:tok0 + 512], h == 0, h == 3, r=[ws["bm"].g, omT.g], w=[pgm])
                            kb.tt("dve", t0[:], pbg[:, :], sg_[:, 0, :], ALU.mult, r=[pgg, sg_.g], w=[t0.g])
                            kb.tt("dve", t1[:], pbf[:, :], sg_[:, 1, :], ALU.mult, r=[pgf, sg_.g], w=[t1.g])
                            kb.tt("dve", t0[:], t0[:], t1[:], ALU.add, r=[t1.g], w=[t0.g])
                            kb.tt("dve", t1[:], pbm[:, :], sg_[:, 2, :], ALU.mult, r=[pgm, sg_.g], w=[t1.g])
                            kb.tt("dve", mergedT[:, c, b2 * 512:(b2 + 1) * 512], t0[:], t1[:], ALU.add, r=[t0.g, t1.g], w=[mergedT.g])
                    if dbg and stage == 6 and half == 0:
                        kb.ld(dbg_out("mergedT", [128, 8 * 1024], BF16), mergedT[:].rearrange("p a b -> p (a b)"), r=[mergedT.g])
                    kb.barrier()
                if stage < 7:
                    continue
                with contextlib.ExitStack() as q7:
                    xt7 = [T(kb, q7, "xt7_%d" % i, [128, 1024], F32) for i in range(2)]
                    st7 = [T(kb, q7, "st7_%d" % i, [128, 16], F32) for i in range(2)]
                    zt7 = [T(kb, q7, "zt7_%d" % i, [128, 1024], F32) for i in range(2)]
                    sz7 = [T(kb, q7, "sz7_%d" % i, [128, 16], F32) for i in range(2)]
                    h1f = [T(kb, q7, "h1f_%d" % i, [128, 1024], F32) for i in range(2)]
                    h1b = [T(kb, q7, "h1b_%d" % i, [128, 1024], BF16) for i in range(4)]
                    h1T = T(kb, q7, "h1T", [128, 8, 128], F32)
                    lg = T(kb, q7, "lg", [128, 32], F32)
                    m8 = T(kb, q7, "m8", [128, 8], F32)
                    msk = T(kb, q7, "msk", [128, 32], F32)
                    ex4 = T(kb, q7, "ex4", [128, 8], F32)
                    posE = T(kb, q7, "posE", [128, 32], F32)
                    posS = T(kb, q7, "posS", [128, 32], F32)
                    prod4 = T(kb, q7, "prod4", [128, 4, 32], F32)
                    ovf = T(kb, q7, "ovf", [128, 32], F32)
                    prod = T(kb, q7, "prod", [128, 32], F32)
                    destf = T(kb, q7, "destf", [128, 4], F32)
                    lgs = [lg, T(kb, q7, "lg2", [128, 32], F32)]

                    obk = {}

                    def p7A0(tl):
                        obk[tl] = []
                        for h2 in range(2):
                            pb, pg = bank()
                            for c in range(8):
                                kb.mm(pb[:, :], mergedT[:, c, tl * 128:(tl + 1) * 128], wout[:, c, h2 * 512:(h2 + 1) * 512], c == 0, c == 7,
                                      r=[mergedT.g, wout.g], w=[pg])
                            obk[tl].append((pb, pg))

                    def p7A(tl):
                        lg = lgs[tl % 2]
                        gt = half * 8 + tl
                        p = tl % 2
                        xt, stt_, z, sz, hf_, hb_ = xt7[p], st7[p], zt7[p], sz7[p], h1f[p], h1b[tl % 4]
                        kb.ld(xt[:], x_own[gt * 128:(gt + 1) * 128, :], w=[xt.g])
                        ln_tile(xt, xt.g, stt_, stt_.g, None, None)
                        kb.stt("dve", z[:], xt[:], stt_[:, 0:1], bc[:, 0, :], ALU.subtract, ALU.mult, r=[xt.g, stt_.g, bc.g], w=[z.g])
                        kb.stt("dve", z[:], z[:], stt_[:, 2:3], bc[:, 1, :], ALU.mult, ALU.add, r=[stt_.g, bc.g], w=[z.g])
                        for h2 in range(2):
                            pb, pg = obk[tl][h2]
                            kb.tt("dve", z[:, h2 * 512:(h2 + 1) * 512], z[:, h2 * 512:(h2 + 1) * 512], pb[:, :], ALU.add, r=[pg], w=[z.g])
                        ln_tile(z, z.g, sz, sz.g, None, None)
                        kb.stt("dve", hf_[:], z[:], sz[:, 0:1], bc[:, 2, :], ALU.subtract, ALU.mult, r=[z.g, sz.g, bc.g], w=[hf_.g])
                        kb.stt("dve", hf_[:], hf_[:], sz[:, 2:3], bc[:, 3, :], ALU.mult, ALU.add, r=[sz.g, bc.g], w=[hf_.g])
                        kb.ld(h1_dram[gt * 128:(gt + 1) * 128, :], hf_[:], r=[hf_.g], w=[h1g])
                        kb.copy("act", hb_[:], hf_[:], r=[hf_.g], w=[hb_.g])

                    def p7A2(tl):
                        lg = lgs[tl % 2]
                        gt = half * 8 + tl
                        p = tl % 2
                        hf_ = h1f[p]
                        for h2 in range(2):
                            pb, pg = bank()
                            for c4 in range(4):
                                c = h2 * 4 + c4
                                kb.tr(pb[:, c4 * 128:(c4 + 1) * 128], hf_[:, c * 128:(c + 1) * 128], ident, r=[hf_.g, cst.g], w=[pg], inc=(c4 == 3))
                            kb.copy("act", h1T[:, h2 * 4:(h2 + 1) * 4, :].rearrange("p a b -> p (a b)"), pb[:, :], r=[pg], w=[h1T.g])
                        pb, pg = bank()
                        for k in range(8):
                            kb.mm(pb[:, 0:32], h1T[:, k, :], wrt[:, k, :], k == 0, k == 7, r=[h1T.g, wrt.g], w=[pg])
                        rbk[tl] = (pb, pg)

                    rbk = {}

                    def p7A2b(tl):
                        lg = lgs[tl % 2]
                        pb, pg = rbk[tl]
                        kb.tt("dve", lg[:], pb[:, 0:32], brt[:], ALU.add, r=[pg, brt.g], w=[lg.g])

                    def p7Be(tl):
                        lg = lgs[tl % 2]
                        gt = half * 8 + tl
                        kb.op("dve", lambda e, a=m8[:], b_=lg[:]: e.max(a, b_), r=[lg.g], w=[m8.g])
                        kb.ts("dve", ex4[:, 4:5], m8[:, 0:1], -1.0, None, ALU.mult, r=[m8.g], w=[ex4.g])
                        kb.act(ex4[:, 0:4], m8[:, 0:4], AF.Exp, r=[m8.g, ex4.g], w=[ex4.g], bias=ex4[:, 4:5], accum_out=ex4[:, 5:6])
                        kb.op("dve", lambda e, a=ex4[:, 6:7], b_=ex4[:, 5:6]: e.reciprocal(a, b_), r=[ex4.g], w=[ex4.g])
                        kb.ts("dve", wts[:, gt * 4:gt * 4 + 4], ex4[:, 0:4], ex4[:, 6:7], None, ALU.mult, r=[ex4.g], w=[wts.g])
                        kb.ts("pool", msk[:], lg[:], m8[:, 3:4], None, ALU.is_ge, r=[lg.g, m8.g], w=[msk.g])

                    def p7Bl(tl):
                        lg = lgs[tl % 2]
                        gt = half * 8 + tl
                        pb, pg = bank()
                        kb.mm(pb[:, 0:32], cst[:, K_STRI:K_STRI + 128], msk[:], True, False, r=[cst.g, msk.g], w=[pg], inc=False)
                        kb.mm(pb[:, 0:32], cst[:, K_ONE1:K_ONE1 + 128], sprev[:], False, True, r=[cst.g, sprev.g], w=[pg])
                        kb.copy("act", posS[:], pb[:, 0:32], r=[pg], w=[posS.g])

                    def p7Bl1(tl):
                        lg = lgs[tl % 2]
                        gt = half * 8 + tl
                        kb.ts("pool", ovf[:], posS[:], float(CAP), None, ALU.is_ge, r=[posS.g], w=[ovf.g])
                        kb.tt("pool", posE[:], posS[:], cst[:, K_ECAP:K_ECAP + 32], ALU.add, r=[posS.g, cst.g], w=[posE.g])
                        kb.ts("pool", prod[:], posE[:], -1.0, float(E * CAP), ALU.mult, ALU.add, r=[posE.g], w=[prod.g])
                        kb.tt("pool", prod[:], prod[:], ovf[:], ALU.mult, r=[ovf.g], w=[prod.g])
                        kb.tt("pool", posE[:], posE[:], prod[:], ALU.add, r=[prod.g], w=[posE.g])
                        kb.tt("pool", sprev[:], sprev[:], msk[:], ALU.add, r=[msk.g], w=[sprev.g])
                        for k in range(4):
                            kb.ts("pool", prod[:], lg[:], m8[:, k:k + 1], None, ALU.is_equal, r=[lg.g, m8.g], w=[prod.g])
                            kb.tt("pool", prod4[:, k, :], prod[:], posE[:], ALU.mult, r=[prod.g, posE.g], w=[prod4.g])

                    def p7C(tl):
                        gt = half * 8 + tl
                        hb_ = h1b[tl % 4]
                        kb.op("dve", lambda e, a=destf[:, 0:4], b_=prod4[:, :, :]: e.reduce_sum(a, b_, AX.X), r=[prod4.g], w=[destf.g])
                        kb.copy("dve", desti[:, gt * 4:gt * 4 + 4], destf[:], r=[destf.g], w=[desti_g[gt]])
                        for k in range(4):
                            kb.dma("pool", lambda e, idx=desti[:, gt * 4 + k:gt * 4 + k + 1], src=hb_[:, :]: e.indirect_dma_start(
                                out=xbuf, out_offset=bass.IndirectOffsetOnAxis(ap=idx, axis=0), in_=src, in_offset=None,
                                bounds_check=bcreg, oob_is_err=False), r=[hb_.g, desti_g[gt]], w=[xg])

                    for st_ in range(11):
                        if 0 <= st_ - 2 < 8:
                            p7Be(st_ - 2)
                        if st_ < 8:
                            p7A0(st_)
                        if 0 <= st_ - 1 < 8:
                            p7A2(st_ - 1)
                        if st_ < 8:
                            p7A(st_)
                        if 0 <= st_ - 1 < 8:
                            p7A2b(st_ - 1)
                        if 0 <= st_ - 2 < 8:
                            p7Bl(st_ - 2)
                        if 0 <= st_ - 3 < 8:
                            p7C(st_ - 3)
                        if 0 <= st_ - 2 < 8:
                            p7Bl1(st_ - 2)
                    kb.barrier()
            if dbg and stage == 7:
                kb.ld(dbg_out("wts", [128, 64]), wts[:], r=[wts.g])
                kb.ld(dbg_out("desti", [128, 64], I32), desti[:], r=desti_g)
                kb.ld(dbg_out("h1", [NT, D]), h1_dram, r=[h1g])


        if stage >= 8:
            p7.close()
            mix.close()
            with contextlib.ExitStack() as p8:
                NS = 16
                wslot = [T(kb, p8, "wslot%d" % i, [128, 8, 512], BF16) for i in range(NS)]
                xtok = [T(kb, p8, "xtok%d" % i, [128, 3, 1024], BF16) for i in range(2)]
                xT = [T(kb, p8, "xT%d" % i, [128, 8, CAP], BF16) for i in range(2)]
                actT = T(kb, p8, "actT", [128, 8, CAP], BF16)
                bdn = [T(kb, p8, "bdn%d" % i, [128, 1024], F32) for i in range(2)]
                gg = [T(kb, p8, "gg%d" % i, [128, CAP], F32) for i in range(2)]
                sg8 = [T(kb, p8, "sg8%d" % i, [128, CAP], F32) for i in range(2)]
                uu = [T(kb, p8, "uu%d" % i, [128, CAP], F32) for i in range(2)]
                yo = [T(kb, p8, "yo%d" % i, [128, 1024], F32) for i in range(2)]
                it = 0

                def load_gu(e2):
                    ps_ = []
                    for pc in range(4):
                        wsl = wslot[(e2 * 6 + pc) % NS]
                        wload(wsl, wsl.g, w_gu[e2], 512, c0=pc * 512, kstep=8)
                        ps_.append(wsl)
                    return ps_

                def load_dn(e2):
                    ps_ = []
                    for pc in range(2):
                        wsl = wslot[(e2 * 6 + 4 + pc) % NS]
                        wload(wsl, wsl.g, w_down[e2], 512, c0=pc * 512, kstep=8)
                        ps_.append(wsl)
                    return ps_

                def prep_expert(e2):
                    xk2, xt2, bd2 = xtok[e2 % 2], xT[e2 % 2], bdn[e2 % 2]
                    kb.ld(xk2[:], xbuf[e2 * CAP:(e2 + 1) * CAP, :].rearrange("(c p) d -> p c d", p=128), r=[xg], w=[xk2.g])
                    kb.ld(bd2[:], b_down[e2:e2 + 1, :].partition_broadcast(128), w=[bd2.g])
                    for sc in range(3):
                        for c in range(8):
                            kb.tr(pbt[:, c * 128:(c + 1) * 128], xk2[:, sc, c * 128:(c + 1) * 128], identb[:], r=[xk2.g, identb.g], w=[pbt_g], inc=(c == 7))
                        kb.copy("act" if sc % 2 else "dve", xt2[:, :, sc * 128:(sc + 1) * 128], pbt[:, :].rearrange("p (c i) -> p c i", c=8),
                                r=[pbt_g], w=[xt2.g])

                loaded = {0: load_gu(0) + load_dn(0), 1: load_gu(1) + load_dn(1)}
                for e_ in range(E):
                    pieces = loaded.pop(e_)
                    if e_ + 2 < E:
                        loaded[e_ + 2] = load_gu(e_ + 2)
                    xk, xt_ = xtok[e_ % 2], xT[e_ % 2]
                    bd = bdn[e_ % 2]
                    if e_ == 0:
                        prep_expert(0)
                    for j in range(8):
                        g_, s_, u_ = gg[it % 2], sg8[it % 2], uu[it % 2]
                        it += 1
                        pg_t, pg_g = bank()
                        wg_ = pieces[j // 4]
                        for k in range(8):
                            kb.mm(pg_t[:, 0:CAP], wg_[:, k, (j % 4) * 128:(j % 4 + 1) * 128], xt_[:, k, :], k == 0, k == 7, r=[wg_.g, xt_.g], w=[pg_g])
                        pu_t, pu_g = bank()
                        wu_ = pieces[2 + j // 4]
                        for k in range(8):
                            kb.mm(pu_t[:, 0:CAP], wu_[:, k, (j % 4) * 128:(j % 4 + 1) * 128], xt_[:, k, :], k == 0, k == 7, r=[wu_.g, xt_.g], w=[pu_g])
                        bgc = col(C_BGU + e_ * 16 + j)
                        buc = col(C_BGU + e_ * 16 + 8 + j)
                        kb.ts("dve", g_[:], pg_t[:, 0:CAP], bgc, 7.0, ALU.add, ALU.min, r=[pg_g, colsT.g], w=[g_.g])
                        kb.act(s_[:], g_[:], AF.Sigmoid, r=[g_.g], w=[s_.g], scale=1.702)
                        kb.act(u_[:], pu_t[:, 0:CAP], AF.Identity, r=[pu_g, colsT.g], w=[u_.g], bias=buc)
                        kb.ts("dve", u_[:], u_[:], 7.0, -7.0, ALU.min, ALU.max, r=[], w=[u_.g])
                        kb.tt("dve", g_[:], g_[:], s_[:], ALU.mult, r=[s_.g], w=[g_.g])
                        kb.stt("dve", actT[:, j, :], u_[:], 1.0, g_[:], ALU.add, ALU.mult, r=[g_.g, u_.g], w=[actT.g])
                    if e_ + 2 < E:
                        loaded[e_ + 2] = loaded[e_ + 2] + load_dn(e_ + 2)
                    if e_ + 1 < E:
                        prep_expert(e_ + 1)
                    for sc in range(3):
                        y_ = yo[sc % 2]
                        for h2 in range(2):
                            pb, pg = bank()
                            wd_ = pieces[4 + h2]
                            for j in range(8):
                                kb.mm(pb[:, :], actT[:, j, sc * 128:(sc + 1) * 128], wd_[:, j, :], j == 0, j == 7, r=[actT.g, wd_.g], w=[pg])
                            kb.tt("dve", y_[:, h2 * 512:(h2 + 1) * 512], pb[:, :], bd[:, h2 * 512:(h2 + 1) * 512], ALU.add, r=[pg, bd.g], w=[y_.g])
                        r0 = e_ * CAP + sc * 128
                        kb.ld(ybuf[r0:r0 + 128, :], y_[:], r=[y_.g], w=[yg])
                kb.barrier()
            with contextlib.ExitStack() as p9:
                l2 = T(kb, p9, "l2", [128, 2, 1024], F32)
                bcast_load(l2[:, 0, :], V_L2G, 1024, l2.g)
                bcast_load(l2[:, 1, :], V_L2B, 1024, l2.g)
                ygt = [T(kb, p9, "ygt%d" % i, [128, 4, 1024], F32) for i in range(4)]
                h1r = [T(kb, p9, "h1r%d" % i, [128, 1024], F32) for i in range(4)]
                acc = [T(kb, p9, "acc%d" % i, [128, 1024], F32) for i in range(4)]
                s9 = [T(kb, p9, "s9_%d" % i, [128, 16], F32) for i in range(4)]
                og_ = kb.reg("out")
                def combine_steps(gt):
                    p = gt % 4
                    y4, hr, ac, st9 = ygt[p], h1r[p], acc[p], s9[p]
                    for k in range(4):
                        kb.dma("pool", lambda e, idx=desti[:, gt * 4 + k:gt * 4 + k + 1], dst=y4[:, k, :]: e.indirect_dma_start(
                            out=dst, out_offset=None, in_=ybuf, in_offset=bass.IndirectOffsetOnAxis(ap=idx, axis=0),
                            bounds_check=bcreg, oob_is_err=False), r=[yg, desti_g[gt]], w=[y4.g])
                    kb.ld(hr[:], h1_dram[gt * 128:(gt + 1) * 128, :], r=[h1g], w=[hr.g])
                    yield
                    kb.stt("dve", ac[:], y4[:, 0, :], wts[:, gt * 4:gt * 4 + 1], hr[:], ALU.mult, ALU.add, r=[y4.g, wts.g, hr.g], w=[ac.g])
                    yield
                    kb.stt("dve", ac[:], hr[:], ALPHA - 1.0, ac[:], ALU.mult, ALU.add, r=[hr.g], w=[ac.g])
                    yield
                    for k in range(1, 4):
                        kb.stt("dve", ac[:], y4[:, k, :], wts[:, gt * 4 + k:gt * 4 + k + 1], ac[:], ALU.mult, ALU.add, r=[y4.g, wts.g], w=[ac.g])
                        yield
                    ln_tile(ac, ac.g, st9, st9.g, None, None)
                    yield
                    kb.stt("dve", hr[:], ac[:], st9[:, 0:1], l2[:, 0, :], ALU.subtract, ALU.mult, r=[ac.g, st9.g, l2.g], w=[hr.g])
                    yield
                    kb.stt("dve", hr[:], hr[:], st9[:, 2:3], l2[:, 1, :], ALU.mult, ALU.add, r=[st9.g, l2.g], w=[hr.g])
                    yield
                    kb.ld(out_d[gt * 128:(gt + 1) * 128, :], hr[:], r=[hr.g], w=[og_])

                for g0 in range(0, 16, 2):
                    gens = [combine_steps(g0), combine_steps(g0 + 1)]
                    while gens:
                        for g_ in list(gens):
                            try:
                                next(g_)
                            except StopIteration:
                                gens.remove(g_)
                kb.barrier()

        kb.barrier()
    return nc, dbg_d


def prep_inputs(inp):
    x = np.asarray(inp["x"], np.float32)
    mem = np.asarray(inp["mem"], np.float32)
    w_in = np.ascontiguousarray(np.asarray(inp["w_in"], np.float32)[0])
    b_in = np.asarray(inp["b_in"], np.float32)[0]
    cst = host_constants()
    cd = chan_dft()
    dfts = [dft_mats(0), dft_mats(1)]
    g1 = lambda k: np.asarray(inp[k], np.float32).reshape(-1)
    vecs = np.concatenate([b_in[OK_:OK_ + 512], b_in[OV:OV + 1024], b_in[OFN:OFN + 512], g1("b_out"),
                           g1("ln_in_g"), g1("ln_in_b"), g1("ln1_g"), g1("ln1_b"), g1("ln2_g"), g1("ln2_b"),
                           g1("b_router")]).astype(np.float32)[None, :]
    assert vecs.shape[1] == NVEC
    maps = []
    for c in range(8):
        b, hf = c // 2, c % 2
        if hf == 0:
            xo, xt = x[b, :NT], x[b, NT:]
            lf, lb, df, db = OLF, OLB, "f", "b"
        else:
            xo, xt = x[b, ::-1][:NT], x[b, ::-1][NT:]
            lf, lb, df, db = OLB, OLF, "b", "f"
        w_lr = np.concatenate([w_in[:, lf:lf + 16], w_in[:, lb:lb + 16]], axis=1)
        wdec = np.zeros((2, 32, 512), np.float32)
        for i, dd in enumerate((df, db)):
            wdec[i, :16] = np.asarray(inp["w_decay_" + dd], np.float32)[0]
            wdec[i, 16] = np.asarray(inp["b_decay_" + dd], np.float32)[0]
        colsT = np.zeros((128, NCOLS), np.float32)

        def put(c0, vec):
            v = np.asarray(vec, np.float32).reshape(-1, 128)
            colsT[:, c0:c0 + v.shape[0]] = v.T

        put(C_LNG, g1("ln_in_g")); put(C_LNB, g1("ln_in_b")); put(C_MG, g1("ln_mem_g")); put(C_MB, g1("ln_mem_b"))
        put(C_BQ, b_in[OQ:OQ + 512]); put(C_BK, b_in[OK_:OK_ + 512]); put(C_BR, b_in[OR:OR + 1024])
        put(C_BMQ, b_in[OMQ:OMQ + 512]); put(C_BG, b_in[OG:OG + 3072]); put(C_GN, g1("gla_norm_g"))
        colsT[:16, C_BLF] = b_in[lf:lf + 16]
        colsT[:16, C_BLB] = b_in[lb:lb + 16]
        put(C_BGU, np.asarray(inp["b_gu"], np.float32)[0].reshape(-1))
        maps.append({
            "x_own": np.ascontiguousarray(xo), "x_oth": np.ascontiguousarray(xt), "mem": np.ascontiguousarray(mem[b]),
            "w_in": w_in, "w_lr": np.ascontiguousarray(w_lr), "wdec": wdec, "colsT": colsT, "vecs": vecs, "cst": cst,
            "dftC": dfts[hf][0], "dftS": dfts[hf][1], "cdft": cd,
            "w_br_gla": np.asarray(inp["w_br_gla"], np.float32)[0], "w_br_fnet": np.asarray(inp["w_br_fnet"], np.float32)[0],
            "w_br_mem": np.asarray(inp["w_br_mem"], np.float32)[0], "w_mem_kv": np.asarray(inp["w_mem_kv"], np.float32)[0],
            "w_out": np.asarray(inp["w_out"], np.float32)[0], "w_router": np.asarray(inp["w_router"], np.float32)[0],
            "w_gu": np.asarray(inp["w_gu"], np.float32)[0], "w_down": np.asarray(inp["w_down"], np.float32)[0],
            "b_down": np.asarray(inp["b_down"], np.float32)[0],
        })
    return maps


def kernel(**inputs):
    nc, _ = build()
    maps = prep_inputs(inputs)
    res = run_bass_kernel_spmd(nc, maps, core_ids=list(range(8)))
    out = np.zeros((4, S, D), np.float32)
    for c in range(8):
        b, hf = c // 2, c % 2
        o = np.asarray(res.results[c]["out"], np.float32)
        if hf == 0:
            out[b, :NT] = o
        else:
            out[b, NT:] = o[::-1]
    return out
```

```python
import contextlib
import math
import numpy as np
import ml_dtypes
import concourse.bass as bass
import concourse.mybir as mybir
from concourse.bass_utils import run_bass_kernel_spmd

F32 = mybir.dt.float32
BF16 = mybir.dt.bfloat16
I32 = mybir.dt.int32
U32 = mybir.dt.uint32
AF = mybir.ActivationFunctionType
ALU = mybir.AluOpType
AX = mybir.AxisListType

D = 1024
S = 4096
NT = 2048
NTILE = 16
E = 32
CAP = 384
ALPHA = 2.0 ** 0.25
LN_EPS = 1e-5
RMS_EPS = 1e-6
OQ, OK_, OV, OR, OLF, OLB, OFN, OMQ, OG = 0, 512, 1024, 2048, 3072, 3088, 3104, 3616, 4128
C_LNG, C_LNB, C_MG, C_MB, C_BQ, C_BK, C_BR, C_BMQ, C_BG, C_GN, C_BLF, C_BLB, C_BGU = 0, 8, 16, 24, 32, 36, 40, 48, 52, 76, 78, 79, 80
NCOLS = 80 + 512
V_BK, V_BV, V_BFN, V_BOUT, V_LNG, V_LNB, V_L1G, V_L1B, V_L2G, V_L2B, V_BRT = 0, 512, 1536, 2048, 3072, 4096, 5120, 6144, 7168, 8192, 9216
NVEC = 9216 + 32
K_ID, K_TRIF, K_TRIB, K_TRIRF, K_TRIRB, K_MF, K_MB, K_ONES, K_STRI, K_ONE1, K_ECAP = 0, 128, 256, 384, 512, 640, 1152, 1664, 1792, 1920, 2048
NCST = 2048 + 32


class Reg:
    __slots__ = ("name", "w", "r", "pend")

    def __init__(self, name):
        self.name = name
        self.w = {}
        self.r = {}
        self.pend = None


class Eng:
    def __init__(self, name, sem):
        self.name = name
        self.sem = sem
        self.cnt = 0
        self.waited = {}
        self.ops = []
        self.pending = []
        self.dsems = []
        self.dval = {}
        self.di = 0


class KB:
    def __init__(self, nc, stack):
        self.nc = nc
        self.stack = stack
        self.E = {}
        for n in ("pe", "dve", "act", "pool", "sp"):
            self.E[n] = Eng(n, stack.enter_context(nc.semaphore("s_" + n)))
        for n, k in (("sp", 8), ("pool", 8), ("act", 4)):
            for i in range(k):
                s = stack.enter_context(nc.semaphore("d_%s%d" % (n, i)))
                self.E[n].dsems.append(s)
                self.E[n].dval[s] = 0
        self.nreg = 0
        self.dma_sems = set()
        for e_ in self.E.values():
            self.dma_sems.update(e_.dsems)
        self.eobj = {"pe": nc.tensor, "dve": nc.vector, "act": nc.scalar, "pool": nc.gpsimd, "sp": nc.sync}

    def reg(self, name=None):
        self.nreg += 1
        return Reg(name or ("r%d" % self.nreg))

    def _waits(self, E, r, w, skip_dma_w=False):
        waits = {}

        def need(sem, val):
            if E.waited.get(sem, 0) < val and waits.get(sem, 0) < val:
                waits[sem] = val

        for g in r:
            if g.pend is not None and g.pend != E.name:
                raise RuntimeError("region %s has pending updates from %s" % (g.name, g.pend))
            for sem, val in g.w.items():
                need(sem, val)
        for g in w:
            if g.pend is not None and g.pend != E.name:
                raise RuntimeError("region %s has pending updates from %s" % (g.name, g.pend))
            for sem, val in g.w.items():
                if skip_dma_w and sem in self.dma_sems:
                    continue
                need(sem, val)
            for sem, val in g.r.items():
                need(sem, val)
        if E.name == "pe":
            waits.pop(E.sem, None)
        for sem, val in waits.items():
            E.waited[sem] = val
        return list(waits.items())

    def op(self, en, fn, r=(), w=(), inc=True):
        E = self.E[en]
        wl = self._waits(E, r, w)
        if inc:
            E.cnt += 1
            tok = (E.sem, E.cnt)
            for rr, ww in E.pending + [(r, w)]:
                for g in ww:
                    g.w = {tok[0]: tok[1]}
                    g.r = {}
                    g.pend = None
                for g in rr:
                    if g not in ww:
                        g.r[E.sem] = E.cnt
                        g.pend = None
            E.pending = []
        else:
            E.pending.append((tuple(r), tuple(w)))
            for g in list(r) + list(w):
                g.pend = E.name
        self._emit(E, wl, fn, 1 if inc else 0, None)

    def _emit(self, E, wl, fn, inc, ds):
        eng = self.eobj[E.name]
        for sem, val in wl:
            eng.wait_ge(sem, val)
        if fn is None:
            return
        ins = fn(eng)
        if ds is not None:
            ins.then_inc(ds, 16)
        elif inc:
            ins.then_inc(E.sem, 1)

    def dma(self, en, fn, r=(), w=()):
        E = self.E[en]
        wl = self._waits(E, r, w, skip_dma_w=True)
        ds = E.dsems[E.di % len(E.dsems)]
        E.di += 1
        prev = E.dval[ds]
        if prev > 0 and E.waited.get(ds, 0) < prev:
            E.waited[ds] = prev
            wl.append((ds, prev))
        E.dval[ds] = prev + 16
        tok = (ds, prev + 16)
        for g in w:
            keep = {sm: v for sm, v in g.w.items() if sm in self.dma_sems}
            keep[ds] = prev + 16
            g.w = keep
            g.r = {}
        for g in r:
            if g not in w:
                g.r[ds] = prev + 16
        self._emit(E, wl, fn, 16, ds)

    def barrier(self):
        toks = []
        for E in self.E.values():
            if E.cnt > 0:
                toks.append((E.sem, E.cnt))
            for ds, v in E.dval.items():
                if v > 0:
                    toks.append((ds, v))
        for E in self.E.values():
            if E.pending:
                raise RuntimeError("pending at barrier on " + E.name)
            wl = []
            for sem, val in toks:
                if sem is E.sem and E.name == "pe":
                    continue
                if E.waited.get(sem, 0) < val:
                    E.waited[sem] = val
                    wl.append((sem, val))
            if wl:
                self._emit(E, wl, None, 0, None)

    def replay(self, en, eng):
        E = self.E[en]
        for wl, fn, inc, ds in E.ops:
            for sem, val in wl:
                eng.wait_ge(sem, val)
            if fn is None:
                continue
            ins = fn(eng)
            if ds is not None:
                ins.then_inc(ds, 16)
            elif inc:
                ins.then_inc(E.sem, 1)

    def mm(self, out, lhsT, rhs, start, stop, r=(), w=(), inc=None):
        if inc is None:
            inc = stop
        self.op("pe", lambda e: e.matmul(out, lhsT, rhs, start=start, stop=stop), r, w, inc)

    def tr(self, out, in_, ident, r=(), w=(), inc=True):
        self.op("pe", lambda e: e.transpose(out, in_, ident), r, w, inc)

    def act(self, out, in_, func, r=(), w=(), bias=0.0, scale=1.0, accum_out=None, en="act"):
        if accum_out is None:
            self.op(en, lambda e: e.activation(out, in_, func, bias=bias, scale=scale), r, w)
        else:
            self.op(en, lambda e: e.activation(out, in_, func, bias=bias, scale=scale, accum_out=accum_out), r, w)

    def tt(self, en, out, in0, in1, op, r=(), w=()):
        self.op(en, lambda e: e.tensor_tensor(out, in0, in1, op), r, w)

    def ts(self, en, out, in0, s1, s2, op0, op1=None, r=(), w=(), accum_out=None):
        if op1 is None:
            self.op(en, lambda e: e.tensor_scalar(out, in0, s1, None, op0), r, w)
        elif accum_out is not None:
            self.op(en, lambda e: e.tensor_scalar(out, in0, s1, s2, op0, op1, accum_out), r, w)
        else:
            self.op(en, lambda e: e.tensor_scalar(out, in0, s1, s2, op0, op1), r, w)

    def stt(self, en, out, in0, scalar, in1, op0, op1, r=(), w=()):
        self.op(en, lambda e: e.scalar_tensor_tensor(out, in0, scalar, in1, op0, op1), r, w)

    def copy(self, en, out, in_, r=(), w=()):
        if en == "act":
            self.op(en, lambda e: e.copy(out, in_), r, w)
        else:
            self.op(en, lambda e: e.tensor_copy(out, in_), r, w)

    def memset(self, en, ap, val, w=()):
        self.op(en, lambda e: e.memset(ap, val), (), w)

    def ld(self, out, in_, r=(), w=(), en="sp"):
        self.dma(en, lambda e: e.dma_start(out=out, in_=in_), r, w)


class T:
    def __init__(self, kb, stack, name, shape, dt):
        self.g = kb.reg(name)
        self.t = stack.enter_context(kb.nc.sbuf_tensor("sb_%s_%d" % (name, kb.nreg), list(shape), dt))

    def __getitem__(self, k):
        return self.t[k]


def host_constants():
    j = np.arange(128)[:, None]
    i = np.arange(128)[None, :]
    c = np.zeros((128, NCST), np.float32)
    c[:, K_ID:K_ID + 128] = (j == i)
    c[:, K_TRIF:K_TRIF + 128] = (j <= i) * (-1.0 / 16)
    c[:, K_TRIB:K_TRIB + 128] = (j >= i) * (-1.0 / 16)
    c[:, K_TRIRF:K_TRIRF + 128] = (j > i) * (-1.0 / 16)
    c[:, K_TRIRB:K_TRIRB + 128] = (j < i) * (-1.0 / 16)
    c[:, K_MF:K_MF + 512] = np.tile((j <= i).astype(np.float32), (1, 4))
    c[:, K_MB:K_MB + 512] = np.tile((j >= i).astype(np.float32), (1, 4))
    c[:, K_ONES:K_ONES + 128] = 1.0 / 256
    c[:, K_STRI:K_STRI + 128] = (j < i)
    c[:, K_ONE1:K_ONE1 + 128] = 1.0
    c[:, K_ECAP:K_ECAP + 32] = (np.arange(32) * CAP)[None, :]
    return c


def dft_mats(hf):
    tau = np.arange(S, dtype=np.int64)
    sg = tau if hf == 0 else (S - 1 - tau)
    prod = (sg[:, None] * sg[None, :NT]) % S
    th = prod.astype(np.float64) * (2 * np.pi / S)
    sc = 1.0 / math.sqrt(S)
    return ((np.cos(th) * sc).astype(ml_dtypes.bfloat16), (np.sin(th) * sc).astype(ml_dtypes.bfloat16))


def chan_dft():
    c = np.arange(128, dtype=np.int64)
    ph = ((c[:, None] * c[None, :]) % 128).astype(np.float64) * (2 * np.pi / 128)
    sc = 1.0 / math.sqrt(128)
    return np.concatenate([np.cos(ph) * sc, -np.sin(ph) * sc], axis=1).astype(ml_dtypes.bfloat16)


def build(stage=99, dbg=False):
    nc = bass.Bass("TRN2", target_bir_lowering=False)
    dr = lambda name, shape, dt, kind="ExternalInput": nc.dram_tensor(name, list(shape), dt, kind=kind).ap()
    x_own = dr("x_own", [NT, D], F32)
    x_oth = dr("x_oth", [NT, D], F32)
    mem = dr("mem", [256, D], F32)
    w_in = dr("w_in", [D, 7200], F32)
    w_lr = dr("w_lr", [D, 32], F32)
    wdec = dr("wdec", [2, 32, 512], F32)
    colsT_d = dr("colsT", [128, NCOLS], F32)
    vecs_d = dr("vecs", [1, NVEC], F32)
    cst_d = dr("cst", [128, NCST], F32)
    dftC = dr("dftC", [S, NT], BF16)
    dftS = dr("dftS", [S, NT], BF16)
    cdft_d = dr("cdft", [128, 256], BF16)
    w_br_gla = dr("w_br_gla", [1024, D], F32)
    w_br_fnet = dr("w_br_fnet", [512, D], F32)
    w_br_mem = dr("w_br_mem", [512, D], F32)
    w_mem_kv = dr("w_mem_kv", [D, 1024], F32)
    w_out = dr("w_out", [D, D], F32)
    w_router = dr("w_router", [D, E], F32)
    if stage >= 8:
        w_gu = dr("w_gu", [E, D, 2048], F32)
        w_down = dr("w_down", [E, D, D], F32)
        b_down = dr("b_down", [E, D], F32)
    out_d = dr("out", [NT, D], F32, "ExternalOutput")
    dbg_d = {}

    def dbg_out(name, shape, dt=F32):
        dbg_d[name] = dr("dbg_" + name, shape, dt, "ExternalOutput")
        return dbg_d[name]

    xbuf = dr("xbuf", [E * CAP + 1, D], BF16, "Internal")
    ybuf = dr("ybuf", [E * CAP + 1, D], F32, "Internal")
    h1_dram = dr("h1_dram", [NT, D], F32, "Internal")

    with contextlib.ExitStack() as top:
        kb = KB(nc, top)
        bcreg = nc.gpsimd.alloc_register("bcreg")
        nc.gpsimd.reg_mov(bcreg, E * CAP)
        banks = []
        for i in range(7):
            t = top.enter_context(nc.psum_tensor("pb%d" % i, [128, 512], F32))
            banks.append((t, kb.reg("pb%d" % i)))
        pbt = top.enter_context(nc.psum_tensor("pbt", [128, 1024], BF16))
        pbt_g = kb.reg("pbt")
        bstate = {"i": 0}

        def bank():
            b = banks[bstate["i"] % 7]
            bstate["i"] += 1
            return b

        cst = T(kb, top, "cst", [128, NCST], F32)
        colsT = T(kb, top, "colsT", [128, NCOLS], F32)
        identb = T(kb, top, "identb", [128, 128], BF16)
        wts = T(kb, top, "wts", [128, 64], F32)
        desti = T(kb, top, "desti", [128, 64], I32)
        desti_g = [kb.reg("desti%d" % i) for i in range(16)]
        kb.ld(cst[:], cst_d, w=[cst.g])
        kb.ld(colsT[:], colsT_d, w=[colsT.g])
        kb.copy("dve", identb[:], cst[:, K_ID:K_ID + 128], r=[cst.g], w=[identb.g])
        ident = cst[:, K_ID:K_ID + 128]

        def col(c0, n=1):
            return colsT[:, c0:c0 + n]

        def bcast_load(tile_ap, off, n, g):
            kb.ld(tile_ap, vecs_d[:, off:off + n].partition_broadcast(128), w=[g])

        def ln_tile(xt, xg, stats, sg, out_bf, og):
            kb.op("dve", lambda e: e.bn_stats(stats[:, 4:10], xt[:, 0:512]), r=[xg], w=[sg])
            kb.op("dve", lambda e: e.bn_stats(stats[:, 10:16], xt[:, 512:1024]), r=[xg], w=[sg])
            kb.op("dve", lambda e: e.bn_aggr(stats[:, 0:2], stats[:, 4:16]), r=[sg], w=[sg])
            kb.act(stats[:, 3:4], stats[:, 1:2], AF.Sqrt, r=[sg], w=[sg], bias=LN_EPS)
            kb.op("dve", lambda e: e.reciprocal(stats[:, 2:3], stats[:, 3:4]), r=[sg], w=[sg])
            if out_bf is not None:
                kb.ts("dve", out_bf, xt[:, :], stats[:, 0:1], stats[:, 2:3], ALU.subtract, ALU.mult, r=[xg, sg], w=[og])

        def transpose_to_fm(src_bf, sg_, dstT, dg, tok0, gcol, bcol):
            for c in range(8):
                kb.tr(pbt[:, c * 128:(c + 1) * 128], src_bf[:, c * 128:(c + 1) * 128], identb[:],
                      r=[sg_, identb.g], w=[pbt_g], inc=(c == 7))
            for c in range(8):
                if c % 2 == 0:
                    kb.act(dstT[:, c, tok0:tok0 + 128], pbt[:, c * 128:(c + 1) * 128], AF.Identity,
                           r=[pbt_g, colsT.g], w=[dg], bias=col(bcol + c), scale=col(gcol + c))
                else:
                    kb.ts("dve", dstT[:, c, tok0:tok0 + 128], pbt[:, c * 128:(c + 1) * 128], col(gcol + c), col(bcol + c),
                          ALU.mult, ALU.add, r=[pbt_g, colsT.g], w=[dg])

        def wload(tile, g, src, ncols, kchunks=8, c0=0, r0=0, kstep=8):
            for k0 in range(0, kchunks, kstep):
                k1 = min(kchunks, k0 + kstep)
                srcv = src[r0 + k0 * 128:r0 + k1 * 128, c0:c0 + ncols].rearrange("(k p) f -> p k f", p=128)
                kb.ld(tile[:, k0:k1, 0:ncols], srcv, w=[g], en="pool")

        mkT = T(kb, top, "mkT", [128, 4, 256], BF16)
        mv = T(kb, top, "mv", [128, 2, 512], BF16)
        xg = kb.reg("xbuf")
        yg = kb.reg("ybuf")
        with contextlib.ExitStack() as ph:
            ztf = T(kb, ph, "ztf", [1, 1024], F32)
            kb.memset("pool", ztf[:], 0.0, w=[ztf.g])
            kb.ld(ybuf[E * CAP:E * CAP + 1, :], ztf[:], r=[ztf.g], w=[yg])
            wkv = T(kb, ph, "wkv", [128, 8, 1024], BF16)
            wload(wkv, wkv.g, w_mem_kv, 1024)
            memT = T(kb, ph, "memT", [128, 8, 256], BF16)
            for mt in range(2):
                xt = T(kb, ph, "memx%d" % mt, [128, 1024], F32)
                st = T(kb, ph, "memst%d" % mt, [128, 16], F32)
                xb = T(kb, ph, "memxb%d" % mt, [128, 1024], BF16)
                kb.ld(xt[:], mem[mt * 128:(mt + 1) * 128, :], w=[xt.g])
                ln_tile(xt, xt.g, st, st.g, xb[:], xb.g)
                transpose_to_fm(xb, xb.g, memT, memT.g, mt * 128, C_MG, C_MB)
            for h in range(4):
                pb, pg = bank()
                for k in range(8):
                    kb.mm(pb[:, 0:256], wkv[:, k, h * 128:(h + 1) * 128], memT[:, k, :], k == 0, k == 7,
                          r=[wkv.g, memT.g], w=[pg])
                kb.copy("dve", mkT[:, h, :], pb[:, 0:256], r=[pg], w=[mkT.g])
            for mt in range(2):
                pb, pg = bank()
                for k in range(8):
                    kb.mm(pb[:, :], memT[:, k, mt * 128:(mt + 1) * 128], wkv[:, k, 512:1024], k == 0, k == 7,
                          r=[wkv.g, memT.g], w=[pg])
                kb.copy("dve", mv[:, mt, :], pb[:, :], r=[pg], w=[mv.g])
            kb.barrier()
        if dbg and stage == 0:
            o1 = dbg_out("mkT", [128, 4 * 256], BF16)
            kb.ld(o1, mkT[:].rearrange("p a b -> p (a b)"), r=[mkT.g])
            o2 = dbg_out("mv", [128, 2 * 512], BF16)
            kb.ld(o2, mv[:].rearrange("p a b -> p (a b)"), r=[mv.g])


        mix = top.enter_context(contextlib.ExitStack())
        hT = T(kb, mix, "hT", [128, 8, NT], BF16)
        slots = T(kb, mix, "slots", [128, 16, 1024], BF16)
        slot_g = [kb.reg("slot%d" % i) for i in range(17)]
        FT = T(kb, mix, "FT", [128, 4, NT], BF16)
        mixA = top.enter_context(contextlib.ExitStack())
        stB = T(kb, mixA, "stB", [128, 1024], F32)
        stBb = T(kb, mixA, "stBb", [128, 1024], BF16)
        wk = T(kb, mixA, "wk", [128, 8, 512], BF16)
        wv = T(kb, mixA, "wv", [128, 8, 1024], BF16)
        wlr = T(kb, mixA, "wlr", [128, 8, 32], BF16)
        wdec_sb = T(kb, mixA, "wdec_sb", [32, 2, 512], F32)
        bkv = T(kb, mixA, "bkv", [128, 2048], F32)
        wload(wk, wk.g, w_in, 512, c0=OK_)
        wload(wv, wv.g, w_in, 1024, c0=OV)
        wload(wlr, wlr.g, w_lr, 32)
        kb.ld(wdec_sb[:, 0, :], wdec[0], w=[wdec_sb.g])
        kb.ld(wdec_sb[:, 1, :], wdec[1], w=[wdec_sb.g])
        bcast_load(bkv[:, 0:2048], V_BK, 2048, bkv.g)
        kb.memset("pool", stB[:], 0.0, w=[stB.g])

        def state_pass(ph, xsrc, own, dirn, f_tok, wfn, tmp, hTd):
            st = tmp["st"]
            pend = [None]
            blocks = list(range(4)) if own else list(range(3, -1, -1))
            hregs = [kb.reg("hblk%d" % i) for i in range(4)]
            fcnt = [0]
            zcnt = [0]

            def front_tile(blk, t):
                p = fcnt[0] % 2
                fcnt[0] += 1
                xt, stt_, xb = tmp["x"][p], tmp["stat"][p], tmp["xb"][p]
                r0 = blk * 512 + t * 128
                kb.ld(xt[:], xsrc[r0:r0 + 128, :], w=[xt.g])
                if own:
                    for _ in range(6):
                        zi = zcnt[0]
                        zcnt[0] += 1
                        kb.ld(xbuf[zi * 128:(zi + 1) * 128, :], slots[:, 0, :], r=[slot_g[0]], w=[xg], en="pool")
                ln_tile(xt, xt.g, stt_, stt_.g, xb[:], xb.g)
                transpose_to_fm(xb, xb.g, hTd, hregs[blk], r0, C_LNG, C_LNB)

            for t in range(4):
                front_tile(blocks[0], t)
            for bi, blk in enumerate(blocks):
                nxt_blk = blocks[bi + 1] if bi + 1 < 4 else None
                ftl = [0]
                hv = lambda k, a, b_, blk=blk: hTd[:, k, blk * 512 + a: blk * 512 + b_]
                hg = hregs[blk]
                lrT = tmp["lrT"]
                pb, pg = bank()
                for k in range(8):
                    kb.mm(pb[0:16, :], wlr[:, k, dirn * 16:(dirn + 1) * 16], hv(k, 0, 512), k == 0, k == 7,
                          r=[wlr.g, hg], w=[pg])
                kb.act(lrT[0:16, :], pb[0:16, :], AF.Identity, r=[pg, colsT.g], w=[lrT.g],
                       bias=colsT[0:16, C_BLF + dirn:C_BLF + dirn + 1])
                tiles = range(4) if own else range(3, -1, -1)

                def stageA(t, blk=blk, hv=hv, hg=hg):
                    p = t % 2
                    gt = blk * 4 + t
                    ktok, vtok, e1, L, kst, dec = (tmp[n][p] for n in ("ktok", "vtok", "e1", "L", "kst", "dec"))
                    pb, pg = bank()
                    for k in range(8):
                        kb.mm(pb[:, :], hv(k, t * 128, (t + 1) * 128), wk[:, k, :], k == 0, k == 7, r=[hg, wk.g], w=[pg])
                    kb.tt("dve", ktok[:], pb[:, :], bkv[:, 0:512], ALU.add, r=[pg, bkv.g], w=[ktok.g])
                    for hh in range(2):
                        pb, pg = bank()
                        for k in range(8):
                            kb.mm(pb[:, :], hv(k, t * 128, (t + 1) * 128), wv[:, k, hh * 512:(hh + 1) * 512], k == 0, k == 7,
                                  r=[hg, wv.g], w=[pg])
                        kb.tt("dve", vtok[:, hh * 512:(hh + 1) * 512], pb[:, :], bkv[:, 512 + hh * 512:1024 + hh * 512], ALU.add,
                              r=[pg, bkv.g], w=[vtok.g])
                    pb, pg = bank()
                    for k in range(8):
                        kb.mm(pb[:, :], hv(k, t * 128, (t + 1) * 128), wfn[:, k, :], k == 0, k == 7, r=[hg, wfn.g], w=[pg])
                    fidx = gt if own else 16 + gt
                    kb.tt("dve", f_tok[:, fidx, :], pb[:, :], bkv[:, 1536:2048], ALU.add, r=[pg, bkv.g], w=[f_tok.g])
                    pb, pg = bank()
                    kb.mm(pb[:, :], lrT[0:32, t * 128:(t + 1) * 128], wdec_sb[0:32, dirn, :], True, True,
                          r=[lrT.g, wdec_sb.g], w=[pg])
                    kb.act(e1[:], pb[:, :], AF.Exp, r=[pg], w=[e1.g], scale=-1.0)
                    kb.act(L[:], e1[:], AF.Ln, r=[e1.g], w=[L.g], bias=1.0)

                def stageB(t, blk=blk):
                    p = t % 2
                    gt = blk * 4 + t
                    ktok, vtok, e1, L, kst, dec = (tmp[n][p] for n in ("ktok", "vtok", "e1", "L", "kst", "dec"))
                    pb, pg = bank()
                    tri = cst[:, K_TRIRF:K_TRIRF + 128] if dirn == 0 else cst[:, K_TRIRB:K_TRIRB + 128]
                    kb.mm(pb[:, :], tri, L[:], True, True, r=[cst.g, L.g], w=[pg])
                    kb.act(e1[:], pb[:, :], AF.Exp, r=[pg], w=[e1.g])
                    kb.tt("pool", kst[:], ktok[:], e1[:], ALU.mult, r=[ktok.g, e1.g], w=[kst.g])
                    pb, pg = bank()
                    for h in range(4):
                        kb.mm(pb[:, h:h + 1], L[:, h * 128:(h + 1) * 128], cst[:, K_TRIF + 127:K_TRIF + 128], True, True,
                              r=[L.g, cst.g], w=[pg], inc=(h == 3))
                    kb.act(dec[:], pb[:, 0:4], AF.Exp, r=[pg], w=[dec.g])
                    for hp in range(2):
                        pb, pg = bank()
                        for h2 in range(2):
                            h = hp * 2 + h2
                            kb.mm(pb[:, h2 * 256:(h2 + 1) * 256], kst[:, h * 128:(h + 1) * 128], vtok[:, h * 256:(h + 1) * 256],
                                  True, True, r=[kst.g, vtok.g], w=[pg], inc=(h2 == 1))
                        for h2 in range(2):
                            h = hp * 2 + h2
                            kb.stt("dve", st[:, h * 256:(h + 1) * 256], st[:, h * 256:(h + 1) * 256], dec[:, h:h + 1],
                                   pb[:, h2 * 256:(h2 + 1) * 256], ALU.mult, ALU.add, r=[pg, dec.g], w=[st.g])
                    if own and gt < 15:
                        kb.copy("act", slots[:, gt + 1, :], st[:], r=[st.g], w=[slot_g[gt + 1]])

                for t in tiles:
                    stageA(t)
                    if nxt_blk is not None:
                        front_tile(nxt_blk, ftl[0])
                        ftl[0] += 1
                    if pend[0] is not None:
                        pend[0]()
                    pend[0] = (lambda t=t, f=stageB: f(t))
            if pend[0] is not None:
                pend[0]()
                pend[0] = None

        with contextlib.ExitStack() as ph:
            f_tok = T(kb, ph, "f_tok", [128, 32, 512], BF16)
            with contextlib.ExitStack() as ph2:
                wfn = T(kb, ph2, "wfn", [128, 8, 512], BF16)
                wload(wfn, wfn.g, w_in, 512, c0=OFN)
                tmp = {"st": stB}
                class V:
                    def __init__(self, ap, name):
                        self.ap = ap
                        self.g = kb.reg(name)

                    def __getitem__(self, k):
                        return self.ap[k]
                hTo = V(slots[:].rearrange("p a b -> p (a b)").rearrange("p (k f) -> p k f", k=8), "hTo")
                tmp["xb"] = [V(FT[:, 2, i * 1024:(i + 1) * 1024], "xb%d" % i) for i in range(2)]
                tmp["vtok"] = [V(FT[:, 3, i * 1024:(i + 1) * 1024], "vtok%d" % i) for i in range(2)]
                tmp["lrT"] = T(kb, ph2, "lrT", [32, 512], F32)
                kb.memset("pool", tmp["lrT"][:], 1.0, w=[tmp["lrT"].g])
                x1 = T(kb, ph2, "sp_x", [128, 1024], F32)
                tmp["x"] = [x1, V(FT[:, 0, :].bitcast(F32), "sp_x2")]
                for n, shp, dt in (("stat", [128, 16], F32),
                                   ("ktok", [128, 512], F32), ("e1", [128, 512], F32),
                                   ("L", [128, 512], F32), ("kst", [128, 512], BF16), ("dec", [128, 4], F32)):
                    tmp[n] = [T(kb, ph2, "sp_%s%d" % (n, i), shp, dt) for i in range(2)]
                state_pass(ph2, x_oth, False, 1, f_tok, wfn, tmp, hTo)
                kb.copy("act", stBb[:], stB[:], r=[stB.g], w=[stBb.g])
                kb.barrier()
                kb.memset("pool", slots[:, 0, :], 0.0, w=[slot_g[0]])
                kb.ld(xbuf[E * CAP:E * CAP + 1, :], slots[0:1, 0, :], r=[slot_g[0]], w=[xg])
                stF = T(kb, ph2, "stF", [128, 1024], F32)
                kb.memset("pool", stF[:], 0.0, w=[stF.g])
                tmp["st"] = stF
                state_pass(ph2, x_own, True, 0, f_tok, wfn, tmp, hT)
                if dbg and stage == 2:
                    kb.ld(dbg_out("stB", [128, 1024]), stB[:], r=[stB.g])
                    kb.ld(dbg_out("stF", [128, 1024]), stF[:], r=[stF.g])
                    kb.ld(dbg_out("ftok", [128, 32 * 512], BF16), f_tok[:].rearrange("p a b -> p (a b)"), r=[f_tok.g])
                    kb.ld(dbg_out("hT", [128, 8 * NT], BF16), hT[:].rearrange("p a b -> p (a b)"), r=[hT.g])
                kb.barrier()
            if stage >= 3:
                with contextlib.ExitStack() as ph3:
                    cd = T(kb, ph3, "cd", [128, 256], BF16)
                    kb.ld(cd[:], cdft_d, w=[cd.g])
                    ring = [T(kb, ph3, "dring%d" % i, [128, 2, 8, 256], BF16) for i in range(3)]
                    pq = [T(kb, ph3, "pq%d" % i, [128, 4, 512], BF16) for i in range(2)]
                    ri = 0
                    for blk in range(8):
                        accs = [bank() for _ in range(4)]
                        for k0 in range(0, 32, 8):
                            rt = ring[ri % 3]
                            ri += 1
                            for ci, src in enumerate((dftC, dftS)):
                                kb.ld(rt[:, ci, :, :], src[k0 * 128:(k0 + 8) * 128, blk * 256:(blk + 1) * 256].rearrange("(k p) f -> p k f", p=128),
                                      w=[rt.g])
                            for kk in range(8):
                                kc = k0 + kk
                                for g in range(4):
                                    pb, pg = accs[g]
                                    last = (kc == 31)
                                    kb.mm(pb[:, :], f_tok[:, kc, g * 128:(g + 1) * 128], rt[:, :, kk, :], kc == 0, last,
                                          r=[f_tok.g, rt.g], w=[pg], inc=(last or (kk == 7 and g == 3)))
                        pqt = pq[blk % 2]
                        for g in range(4):
                            pb, pg = accs[g]
                            kb.copy("act" if g % 2 else "dve", pqt[:, g, :], pb[:, :], r=[pg], w=[pqt.g])
                        for g in range(4):
                            pb, pg = bank()
                            kb.mm(pb[:, 0:256], cd[:, 0:128], pqt[:, g, 0:256], True, False, r=[cd.g, pqt.g], w=[pg], inc=False)
                            kb.mm(pb[:, 0:256], cd[:, 128:256], pqt[:, g, 256:512], False, True, r=[cd.g, pqt.g], w=[pg])
                            kb.copy("act" if g % 2 else "dve", FT[:, g, blk * 256:(blk + 1) * 256], pb[:, 0:256], r=[pg], w=[FT.g])
                    if dbg and stage == 3:
                        kb.ld(dbg_out("FT", [128, 4 * NT], BF16), FT[:].rearrange("p a b -> p (a b)"), r=[FT.g])
                    kb.barrier()


        if stage >= 4:
            with contextlib.ExitStack() as ph4:
                wq = T(kb, ph4, "wq", [128, 8, 512], BF16)
                wload(wq, wq.g, w_in, 512, c0=OQ)
                bqs = T(kb, ph4, "bqs", [128, 4], F32)
                QS = 128.0 ** -0.5
                kb.ts("dve", bqs[:], colsT[:, C_BQ:C_BQ + 4], QS, None, ALU.mult, r=[colsT.g], w=[bqs.g])
                wrc = [T(kb, ph4, "wrc%d" % i, [128, 8, 128], BF16) for i in range(4)]
                BL = 256
                qT = T(kb, ph4, "qT", [128, 4, BL], F32)
                kT = T(kb, ph4, "kT", [128, 4, BL], F32)
                ktok = T(kb, ph4, "ktok4", [128, 2, 512], F32)
                vtok = T(kb, ph4, "vtok4", [128, 2, 1024], BF16)
                rs = T(kb, ph4, "rs", [128, 8, BL], BF16)
                lrT2 = [T(kb, ph4, "lrT4_%d" % i, [32, BL], F32) for i in range(2)]
                for i in range(2):
                    kb.memset("pool", lrT2[i][:], 1.0, w=[lrT2[i].g])
                e1 = T(kb, ph4, "e1_4", [128, 512], F32)
                Ls = [T(kb, ph4, "L4_%d" % i, [128, 512], F32) for i in range(2)]
                Eq = T(kb, ph4, "Eq", [128, 512], F32)
                Ek = T(kb, ph4, "Ek", [128, 512], F32)
                Eqs = [Eq, T(kb, ph4, "Eq2", [128, 512], F32)]
                Eks = [Ek, T(kb, ph4, "Ek2", [128, 512], F32)]
                e1s = [e1, T(kb, ph4, "e1_4b", [128, 512], F32)]
                decB = T(kb, ph4, "decB", [128, 4], F32)
                qin = [T(kb, ph4, "qin%d" % i, [128, 4, 128], BF16) for i in range(2)]
                kin = [T(kb, ph4, "kin%d" % i, [128, 4, 128], BF16) for i in range(2)]
                kst = T(kb, ph4, "kst4", [128, 512], BF16)
                ta = T(kb, ph4, "ta", [128, 512], F32)
                tb = T(kb, ph4, "tb", [128, 512], F32)
                attb = T(kb, ph4, "attb", [128, 4, 128], BF16)
                sq = [ta, tb]
                rstd = T(kb, ph4, "rstd", [128, 512], F32)
                ton = T(kb, ph4, "ton", [128, 256], F32)
                wri = 0
                wreq = [0]
                for blk in range(NT // BL - 1, -1, -1):
                    tok0 = blk * BL
                    for h in range(4):
                        pb, pg = bank()
                        for k in range(8):
                            kb.mm(pb[:, 0:BL], wq[:, k, h * 128:(h + 1) * 128], hT[:, k, tok0:tok0 + BL], k == 0, k == 7, r=[wq.g, hT.g], w=[pg])
                        kb.act(qT[:, h, :], pb[:, 0:BL], AF.Identity, r=[pg, bqs.g], w=[qT.g], bias=bqs[:, h:h + 1], scale=QS)
                        pb, pg = bank()
                        for k in range(8):
                            kb.mm(pb[:, 0:BL], wk[:, k, h * 128:(h + 1) * 128], hT[:, k, tok0:tok0 + BL], k == 0, k == 7, r=[wk.g, hT.g], w=[pg])
                        kb.act(kT[:, h, :], pb[:, 0:BL], AF.Identity, r=[pg, colsT.g], w=[kT.g], bias=col(C_BK + h))
                    for hc in range(8):
                        while wreq[0] < min(wri + 4, 8 * (NT // BL)):
                            wt2 = wrc[wreq[0] % 4]
                            wload(wt2, wt2.g, w_in, 128, c0=OR + (wreq[0] % 8) * 128, kstep=8)
                            wreq[0] += 1
                        wt = wrc[wri % 4]
                        wri += 1
                        pb, pg = bank()
                        for k in range(8):
                            kb.mm(pb[:, 0:BL], wt[:, k, :], hT[:, k, tok0:tok0 + BL], k == 0, k == 7, r=[wt.g, hT.g], w=[pg])
                        kb.act(rs[:, hc, :], pb[:, 0:BL], AF.Silu, r=[pg, colsT.g], w=[rs.g], bias=col(C_BR + hc))
                    for dirn in range(2):
                        pb, pg = bank()
                        for k in range(8):
                            kb.mm(pb[0:16, 0:BL], wlr[:, k, dirn * 16:(dirn + 1) * 16], hT[:, k, tok0:tok0 + BL], k == 0, k == 7,
                                  r=[wlr.g, hT.g], w=[pg])
                        kb.act(lrT2[dirn][0:16, :], pb[0:16, 0:BL], AF.Identity, r=[pg, colsT.g], w=[lrT2[dirn].g],
                               bias=colsT[0:16, C_BLF + dirn:C_BLF + dirn + 1])
                    for t in range(BL // 128):
                        pb, pg = bank()
                        for k in range(8):
                            kb.mm(pb[:, :], hT[:, k, tok0 + t * 128:tok0 + (t + 1) * 128], wk[:, k, :], k == 0, k == 7, r=[hT.g, wk.g], w=[pg])
                        kb.tt("dve", ktok[:, t, :], pb[:, :], bkv[:, 0:512], ALU.add, r=[pg, bkv.g], w=[ktok.g])
                        for hh in range(2):
                            pb, pg = bank()
                            for k in range(8):
                                kb.mm(pb[:, :], hT[:, k, tok0 + t * 128:tok0 + (t + 1) * 128], wv[:, k, hh * 512:(hh + 1) * 512], k == 0, k == 7,
                                      r=[hT.g, wv.g], w=[pg])
                            kb.tt("dve", vtok[:, t, hh * 512:(hh + 1) * 512], pb[:, :], bkv[:, 512 + hh * 512:1024 + hh * 512], ALU.add,
                                  r=[pg, bkv.g], w=[vtok.g])
                    for t in range(BL // 128 - 1, -1, -1):
                        gt = blk * (BL // 128) + t
                        i0 = t * 128
                        zbs = []
                        for dirn in range(2):
                            pb, pg = bank()
                            kb.mm(pb[:, :], lrT2[dirn][0:32, i0:i0 + 128], wdec_sb[0:32, dirn, :], True, True,
                                  r=[lrT2[dirn].g, wdec_sb.g], w=[pg])
                            zbs.append((pb, pg))
                        for dirn in range(2):
                            pb, pg = zbs[dirn]
                            kb.act(e1s[dirn][:], pb[:, :], AF.Exp, r=[pg], w=[e1s[dirn].g], scale=-1.0)
                        for dirn in range(2):
                            kb.act(Ls[dirn][:], e1s[dirn][:], AF.Ln, r=[e1s[dirn].g], w=[Ls[dirn].g], bias=1.0)
                        abs_ = []
                        for dirn in range(2):
                            pb, pg = bank()
                            tri = cst[:, K_TRIF:K_TRIF + 128] if dirn == 0 else cst[:, K_TRIB:K_TRIB + 128]
                            for h in range(4):
                                kb.mm(pb[:, h * 128:(h + 1) * 128], Ls[dirn][:, h * 128:(h + 1) * 128], tri, True, True,
                                      r=[Ls[dirn].g, cst.g], w=[pg], inc=(h == 3))
                            abs_.append((pb, pg))
                        pbr, pgr = bank()
                        kb.mm(pbr[:, :], cst[:, K_TRIRB:K_TRIRB + 128], Ls[1][:], True, True, r=[cst.g, Ls[1].g], w=[pgr])
                        for dirn in range(2):
                            pb, pg = abs_[dirn]
                            kb.act(Eqs[dirn][:], pb[:, :], AF.Exp, r=[pg], w=[Eqs[dirn].g])
                            kb.act(Eks[dirn][:], pb[:, :], AF.Exp, r=[pg], w=[Eks[dirn].g], scale=-1.0)
                        kb.act(e1s[0][:], pbr[:, :], AF.Exp, r=[pgr], w=[e1s[0].g])
                        kb.copy("pool", decB[:], Eqs[1][:].rearrange("p (h i) -> p h i", h=4)[:, :, 0], r=[Eqs[1].g], w=[decB.g])
                        for dirn in range(2):
                            kb.tt("pool", qin[dirn][:], qT[:, :, i0:i0 + 128], Eqs[dirn][:].rearrange("p (h i) -> p h i", h=4), ALU.mult,
                                  r=[qT.g, Eqs[dirn].g], w=[qin[dirn].g])
                            kb.tt("dve", kin[dirn][:], kT[:, :, i0:i0 + 128], Eks[dirn][:].rearrange("p (h i) -> p h i", h=4), ALU.mult,
                                  r=[kT.g, Eks[dirn].g], w=[kin[dirn].g])
                        kb.tt("pool", kst[:], ktok[:, t, :], e1s[0][:], ALU.mult, r=[ktok.g, e1s[0].g], w=[kst.g])
                        pbF, pgF = bank()
                        pbB, pgB = bank()
                        for h in range(4):
                            kb.mm(pbF[:, h * 128:(h + 1) * 128], kin[0][:, h, :], qin[0][:, h, :], True, True,
                                  r=[kin[0].g, qin[0].g], w=[pgF], inc=(h == 3))
                        for h in range(4):
                            kb.mm(pbB[:, h * 128:(h + 1) * 128], kin[1][:, h, :], qin[1][:, h, :], True, True,
                                  r=[kin[1].g, qin[1].g], w=[pgB], inc=(h == 3))
                        kb.tt("dve", ta[:], pbF[:, :], cst[:, K_MF:K_MF + 512], ALU.mult, r=[pgF, cst.g], w=[ta.g])
                        kb.tt("dve", tb[:], pbB[:, :], cst[:, K_MB:K_MB + 512], ALU.mult, r=[pgB, cst.g], w=[tb.g])
                        kb.tt("pool", attb[:].rearrange("p h i -> p (h i)"), ta[:], tb[:], ALU.add, r=[ta.g, tb.g], w=[attb.g])
                        obanks = [bank(), bank()]
                        for hp in range(2):
                            pb, pg = obanks[hp]
                            for h2 in range(2):
                                h = hp * 2 + h2
                                for c in range(2):
                                    vs = h * 256 + c * 128
                                    oc = pb[:, (h2 * 2 + c) * 128:(h2 * 2 + c + 1) * 128]
                                    kb.mm(oc, vtok[:, t, vs:vs + 128], attb[:, h, :], True, False, r=[vtok.g, attb.g], w=[pg], inc=False)
                                    kb.mm(oc, slots[:, gt, vs:vs + 128], qin[0][:, h, :], False, False, r=[slot_g[gt], qin[0].g], w=[pg], inc=False)
                                    kb.mm(oc, stBb[:, vs:vs + 128], qin[1][:, h, :], False, True, r=[stBb.g, qin[1].g], w=[pg],
                                          inc=(h2 == 1 and c == 1))
                        msb, msg = bank()
                        for hp in range(2):
                            pb, pg = obanks[hp]
                            kb.act(sq[hp][:], pb[:, :], AF.Square, r=[pg], w=[sq[hp].g])
                        for h in range(4):
                            hp, h2 = h // 2, h % 2
                            kb.mm(msb[:, h * 128:(h + 1) * 128], cst[:, K_ONES:K_ONES + 128], sq[hp][:, (h2 * 2) * 128:(h2 * 2 + 1) * 128],
                                  True, False, r=[cst.g, sq[hp].g], w=[msg], inc=False)
                            kb.mm(msb[:, h * 128:(h + 1) * 128], cst[:, K_ONES:K_ONES + 128], sq[hp][:, (h2 * 2 + 1) * 128:(h2 * 2 + 2) * 128],
                                  False, True, r=[cst.g, sq[hp].g], w=[msg], inc=(h == 3))
                        kb.act(e1[:], msb[:, :], AF.Sqrt, r=[msg], w=[e1.g], bias=RMS_EPS)
                        kb.op("dve", lambda e, a=rstd[:], b_=e1[:]: e.reciprocal(a, b_), r=[e1.g], w=[rstd.g])
                        for hp in range(2):
                            pb, pg = obanks[hp]
                            ov = pb[:, :].rearrange("p (h c i) -> p h c i", h=2, c=2)
                            rv = rstd[:].rearrange("p (h i) -> p h i", h=4)[:, hp * 2:hp * 2 + 2, :]
                            sv = slots[:, gt, :].rearrange("p (h c i) -> p h c i", h=4, c=2)
                            rsv = rs[:].rearrange("p (h c) i -> p h c i", c=2)
                            for c in range(2):
                                tv = ton[:].rearrange("p (h i) -> p h i", h=2)
                                kb.stt("dve", tv, ov[:, :, c, :], col(C_GN + c), rv, ALU.mult, ALU.mult, r=[pg, rstd.g, colsT.g], w=[ton.g])
                                kb.tt("pool", sv[:, hp * 2:hp * 2 + 2, c, :], tv, rsv[:, hp * 2:hp * 2 + 2, c, i0:i0 + 128], ALU.mult,
                                      r=[ton.g, rs.g], w=[slot_g[gt]])
                        for hp in range(2):
                            pb, pg = bank()
                            for h2 in range(2):
                                h = hp * 2 + h2
                                kb.mm(pb[:, h2 * 256:(h2 + 1) * 256], kst[:, h * 128:(h + 1) * 128], vtok[:, t, h * 256:(h + 1) * 256],
                                      True, True, r=[kst.g, vtok.g], w=[pg], inc=(h2 == 1))
                            for h2 in range(2):
                                h = hp * 2 + h2
                                kb.stt("dve", stB[:, h * 256:(h + 1) * 256], stB[:, h * 256:(h + 1) * 256], decB[:, h:h + 1],
                                       pb[:, h2 * 256:(h2 + 1) * 256], ALU.mult, ALU.add, r=[pg, decB.g], w=[stB.g])
                        kb.copy("act", stBb[:], stB[:], r=[stB.g], w=[stBb.g])
                if dbg and stage == 4:
                    kb.ld(dbg_out("og", [128, 16 * 1024], BF16), slots[:].rearrange("p a b -> p (a b)"), r=slot_g)
                    kb.ld(dbg_out("FT", [128, 4 * NT], BF16), FT[:].rearrange("p a b -> p (a b)"), r=[FT.g])
                kb.barrier()


        if stage >= 5:
            mixA.close()
            omT = T(kb, mix, "omT", [128, 4, NT], BF16)
            onesb = T(kb, mix, "onesb", [128, 128], BF16)
            kb.copy("dve", onesb[:], cst[:, K_ONE1:K_ONE1 + 128], r=[cst.g], w=[onesb.g])
            with contextlib.ExitStack() as ph5:
                wmq = T(kb, ph5, "wmq", [128, 8, 512], BF16)
                wload(wmq, wmq.g, w_in, 512, c0=OMQ)
                MS = 128.0 ** -0.5
                bms = T(kb, ph5, "bms", [128, 4], F32)
                kb.ts("dve", bms[:], colsT[:, C_BMQ:C_BMQ + 4], MS, None, ALU.mult, r=[colsT.g], w=[bms.g])
                mqT = T(kb, ph5, "mqT", [128, 4, 512], BF16)
                eT = [T(kb, ph5, "eT%d" % i, [128, 2, 512], BF16) for i in range(2)]
                rden = [T(kb, ph5, "rden%d" % i, [128, 512], F32) for i in range(2)]
                for blk in range(4):
                    tok0 = blk * 512
                    for h in range(4):
                        pb, pg = bank()
                        for k in range(8):
                            kb.mm(pb[:, :], wmq[:, k, h * 128:(h + 1) * 128], hT[:, k, tok0:tok0 + 512], k == 0, k == 7, r=[wmq.g, hT.g], w=[pg])
                        kb.act(mqT[:, h, :], pb[:, :], AF.Identity, r=[pg, bms.g], w=[mqT.g], bias=bms[:, h:h + 1], scale=MS)
                    for h in range(4):
                        et = eT[h % 2]
                        for mc in range(2):
                            pb, pg = bank()
                            kb.mm(pb[:, :], mkT[:, h, mc * 128:(mc + 1) * 128], mqT[:, h, :], True, True, r=[mkT.g, mqT.g], w=[pg])
                            kb.act(et[:, mc, :], pb[:, :], AF.Exp, r=[pg], w=[et.g])
                        pbo, pgo = bank()
                        pbd, pgd = bank()
                        for mc in range(2):
                            kb.mm(pbo[:, :], mv[:, mc, h * 128:(h + 1) * 128], et[:, mc, :], mc == 0, mc == 1, r=[mv.g, et.g], w=[pgo])
                        for mc in range(2):
                            kb.mm(pbd[:, :], onesb[:], et[:, mc, :], mc == 0, mc == 1, r=[onesb.g, et.g], w=[pgd])
                        rd = rden[h % 2]
                        kb.op("dve", lambda e, a=rd[:], b_=pbd[:, :]: e.reciprocal(a, b_), r=[pgd], w=[rd.g])
                        kb.tt("dve", omT[:, h, tok0:tok0 + 512], pbo[:, :], rd[:], ALU.mult, r=[pgo, rd.g], w=[omT.g])
                if dbg and stage == 5:
                    kb.ld(dbg_out("omT", [128, 4 * NT], BF16), omT[:].rearrange("p a b -> p (a b)"), r=[omT.g])
                kb.barrier()

        if stage >= 6:
            p7 = top.enter_context(contextlib.ExitStack())
            sprev = T(kb, p7, "sprev", [128, 32], F32)
            kb.memset("pool", sprev[:], 0.0, w=[sprev.g])
            wout = T(kb, p7, "wout", [128, 8, 1024], BF16)
            wload(wout, wout.g, w_out, 1024)
            wrt = T(kb, p7, "wrt", [128, 8, 32], F32)
            kb.ld(wrt[:], w_router.rearrange("(k p) e -> p k e", p=128), w=[wrt.g])
            bc = T(kb, p7, "bc", [128, 4, 1024], F32)
            brt = T(kb, p7, "brt", [128, 32], F32)
            bcast_load(brt[:], V_BRT, 32, brt.g)
            with contextlib.ExitStack() as pz:
                tmpb = T(kb, pz, "tmpb", [128, 2, 1024], F32)
                bcast_load(bc[:, 0, :], V_LNG, 1024, bc.g)
                bcast_load(tmpb[:, 0, :], V_LNB, 1024, tmpb.g)
                bcast_load(tmpb[:, 1, :], V_BOUT, 1024, tmpb.g)
                bcast_load(bc[:, 2, :], V_L1G, 1024, bc.g)
                bcast_load(bc[:, 3, :], V_L1B, 1024, bc.g)
                kb.ts("dve", bc[:, 0, :], bc[:, 0, :], ALPHA, None, ALU.mult, r=[bc.g], w=[bc.g])
                kb.stt("dve", bc[:, 1, :], tmpb[:, 0, :], ALPHA, tmpb[:, 1, :], ALU.mult, ALU.add, r=[tmpb.g], w=[bc.g])
                kb.barrier()
            h1g = kb.reg("h1_dram")
            for half in range(2):
                mergedT = T(kb, p7, "mergedT%d" % half, [128, 8, 1024], BF16) if half == 0 else mergedT
                with contextlib.ExitStack() as p6:
                    wsets = []
                    for i in range(2):
                        wsets.append({"g": [T(kb, p6, "wg%d_%d" % (i, j), [128, 8, 128], BF16) for j in range(3)],
                                      "bg": T(kb, p6, "wbg%d" % i, [128, 8, 128], BF16),
                                      "bf": T(kb, p6, "wbf%d" % i, [128, 4, 128], BF16),
                                      "bm": T(kb, p6, "wbm%d" % i, [128, 4, 128], BF16)})
                    sig = [T(kb, p6, "sig%d" % i, [128, 3, 512], F32) for i in range(2)]
                    t0s = [T(kb, p6, "t0s%d" % i, [128, 512], F32) for i in range(2)]
                    t1s = [T(kb, p6, "t1s%d" % i, [128, 512], F32) for i in range(2)]
                    it = 0

                    def load_chunk(c2):
                        ws2 = wsets[c2 % 2]
                        for j2 in range(3):
                            wload(ws2["g"][j2], ws2["g"][j2].g, w_in, 128, c0=OG + j2 * 1024 + c2 * 128)
                        wload(ws2["bg"], ws2["bg"].g, w_br_gla, 128, c0=c2 * 128)
                        wload(ws2["bf"], ws2["bf"].g, w_br_fnet, 128, kchunks=4, c0=c2 * 128)
                        wload(ws2["bm"], ws2["bm"].g, w_br_mem, 128, kchunks=4, c0=c2 * 128)

                    load_chunk(0)
                    for c in range(8):
                        ws = wsets[c % 2]
                        if c + 1 < 8:
                            load_chunk(c + 1)
                        for b2 in range(2):
                            tok0 = half * 1024 + b2 * 512
                            tl0 = tok0 // 128
                            sg_, t0, t1 = sig[it % 2], t0s[it % 2], t1s[it % 2]
                            it += 1
                            for j in range(3):
                                pb, pg = bank()
                                for k in range(8):
                                    kb.mm(pb[:, :], ws["g"][j][:, k, :], hT[:, k, tok0:tok0 + 512], k == 0, k == 7, r=[ws["g"][j].g, hT.g], w=[pg])
                                kb.act(sg_[:, j, :], pb[:, :], AF.Sigmoid, r=[pg, colsT.g], w=[sg_.g], bias=col(C_BG + j * 8 + c))
                            pbg, pgg = bank()
                            for hc in range(8):
                                kb.mm(pbg[:, :], ws["bg"][:, hc, :], slots[:, tl0:tl0 + 4, hc * 128:(hc + 1) * 128], hc == 0, hc == 7,
                                      r=[ws["bg"].g] + slot_g[tl0:tl0 + 4], w=[pgg])
                            pbf, pgf = bank()
                            for g in range(4):
                                kb.mm(pbf[:, :], ws["bf"][:, g, :], FT[:, g, tok0:tok0 + 512], g == 0, g == 3, r=[ws["bf"].g, FT.g], w=[pgf])
                            pbm, pgm = bank()
                            for h in range(4):
                                kb.mm(pbm[:, :], ws["bm"][:, h, :], omT[:, h, tok0:tok0 + 512], h == 0, h == 3, r=[ws["bm"].g, omT.g], w=[pgm])
                            kb.tt("dve", t0[:], pbg[:, :], sg_[:, 0, :], ALU.mult, r=[pgg, sg_.g], w=[t0.g])
                            kb.tt("dve", t1[:], pbf[:, :], sg_[:, 1, :], ALU.mult, r=[pgf, sg_.g], w=[t1.g])
                            kb.tt("dve", t0[:], t0[:], t1[:], ALU.add, r=[t1.g], w=[t0.g])
                            kb.tt("dve", t1[:], pbm[:, :], sg_[:, 2, :], ALU.mult, r=[pgm, sg_.g], w=[t1.g])
                            kb.tt("dve", mergedT[:, c, b2 * 512:(b2 + 1) * 512], t0[:], t1[:], ALU.add, r=[t0.g, t1.g], w=[mergedT.g])
                    if dbg and stage == 6 and half == 0:
                        kb.ld(dbg_out("mergedT", [128, 8 * 1024], BF16), mergedT[:].rearrange("p a b -> p (a b)"), r=[mergedT.g])
                    kb.barrier()
                if stage < 7:
                    continue
                with contextlib.ExitStack() as q7:
                    xt7 = [T(kb, q7, "xt7_%d" % i, [128, 1024], F32) for i in range(2)]
                    st7 = [T(kb, q7, "st7_%d" % i, [128, 16], F32) for i in range(2)]
                    zt7 = [T(kb, q7, "zt7_%d" % i, [128, 1024], F32) for i in range(2)]
                    sz7 = [T(kb, q7, "sz7_%d" % i, [128, 16], F32) for i in range(2)]
                    h1f = [T(kb, q7, "h1f_%d" % i, [128, 1024], F32) for i in range(2)]
                    h1b = [T(kb, q7, "h1b_%d" % i, [128, 1024], BF16) for i in range(4)]
                    h1T = T(kb, q7, "h1T", [128, 8, 128], F32)
                    lg = T(kb, q7, "lg", [128, 32], F32)
                    m8 = T(kb, q7, "m8", [128, 8], F32)
                    msk = T(kb, q7, "msk", [128, 32], F32)
                    ex4 = T(kb, q7, "ex4", [128, 8], F32)
                    posE = T(kb, q7, "posE", [128, 32], F32)
                    posS = T(kb, q7, "posS", [128, 32], F32)
                    prod4 = T(kb, q7, "prod4", [128, 4, 32], F32)
                    ovf = T(kb, q7, "ovf", [128, 32], F32)
                    prod = T(kb, q7, "prod", [128, 32], F32)
                    destf = T(kb, q7, "destf", [128, 4], F32)
                    lgs = [lg, T(kb, q7, "lg2", [128, 32], F32), T(kb, q7, "lg3", [128, 32], F32)]
                    m8s = [m8, T(kb, q7, "m8b", [128, 8], F32)]

                    obk = {}

                    def p7A0(tl):
                        obk[tl] = []
                        for h2 in range(2):
                            pb, pg = bank()
                            for c in range(8):
                                kb.mm(pb[:, :], mergedT[:, c, tl * 128:(tl + 1) * 128], wout[:, c, h2 * 512:(h2 + 1) * 512], c == 0, c == 7,
                                      r=[mergedT.g, wout.g], w=[pg])
                            obk[tl].append((pb, pg))

                    def p7A(tl):
                        lg = lgs[tl % 3]
                        gt = half * 8 + tl
                        p = tl % 2
                        xt, stt_, z, sz, hf_, hb_ = xt7[p], st7[p], zt7[p], sz7[p], h1f[p], h1b[tl % 4]
                        kb.ld(xt[:], x_own[gt * 128:(gt + 1) * 128, :], w=[xt.g])
                        ln_tile(xt, xt.g, stt_, stt_.g, None, None)
                        kb.stt("dve", z[:], xt[:], stt_[:, 0:1], bc[:, 0, :], ALU.subtract, ALU.mult, r=[xt.g, stt_.g, bc.g], w=[z.g])
                        kb.stt("dve", z[:], z[:], stt_[:, 2:3], bc[:, 1, :], ALU.mult, ALU.add, r=[stt_.g, bc.g], w=[z.g])
                        for h2 in range(2):
                            pb, pg = obk[tl][h2]
                            kb.tt("dve", z[:, h2 * 512:(h2 + 1) * 512], z[:, h2 * 512:(h2 + 1) * 512], pb[:, :], ALU.add, r=[pg], w=[z.g])
                        ln_tile(z, z.g, sz, sz.g, None, None)
                        kb.stt("dve", hf_[:], z[:], sz[:, 0:1], bc[:, 2, :], ALU.subtract, ALU.mult, r=[z.g, sz.g, bc.g], w=[hf_.g])
                        kb.stt("dve", hf_[:], hf_[:], sz[:, 2:3], bc[:, 3, :], ALU.mult, ALU.add, r=[sz.g, bc.g], w=[hf_.g])
                        kb.ld(h1_dram[gt * 128:(gt + 1) * 128, :], hf_[:], r=[hf_.g], w=[h1g])
                        kb.copy("act", hb_[:], hf_[:], r=[hf_.g], w=[hb_.g])

                    def p7A2(tl):
                        lg = lgs[tl % 3]
                        gt = half * 8 + tl
                        p = tl % 2
                        hf_ = h1f[p]
                        for h2 in range(2):
                            pb, pg = bank()
                            for c4 in range(4):
                                c = h2 * 4 + c4
                                kb.tr(pb[:, c4 * 128:(c4 + 1) * 128], hf_[:, c * 128:(c + 1) * 128], ident, r=[hf_.g, cst.g], w=[pg], inc=(c4 == 3))
                            kb.copy("act", h1T[:, h2 * 4:(h2 + 1) * 4, :].rearrange("p a b -> p (a b)"), pb[:, :], r=[pg], w=[h1T.g])
                        pb, pg = bank()
                        for k in range(8):
                            kb.mm(pb[:, 0:32], h1T[:, k, :], wrt[:, k, :], k == 0, k == 7, r=[h1T.g, wrt.g], w=[pg])
                        rbk[tl] = (pb, pg)

                    rbk = {}

                    def p7A2b(tl):
                        lg = lgs[tl % 3]
                        pb, pg = rbk[tl]
                        kb.tt("dve", lg[:], pb[:, 0:32], brt[:], ALU.add, r=[pg, brt.g], w=[lg.g])

                    def p7Be(tl):
                        lg = lgs[tl % 3]
                        m8 = m8s[tl % 2]
                        gt = half * 8 + tl
                        kb.op("dve", lambda e, a=m8[:], b_=lg[:]: e.max(a, b_), r=[lg.g], w=[m8.g])
                        kb.ts("dve", ex4[:, 4:5], m8[:, 0:1], -1.0, None, ALU.mult, r=[m8.g], w=[ex4.g])
                        kb.act(ex4[:, 0:4], m8[:, 0:4], AF.Exp, r=[m8.g, ex4.g], w=[ex4.g], bias=ex4[:, 4:5], accum_out=ex4[:, 5:6])
                        kb.op("dve", lambda e, a=ex4[:, 6:7], b_=ex4[:, 5:6]: e.reciprocal(a, b_), r=[ex4.g], w=[ex4.g])
                        kb.ts("dve", wts[:, gt * 4:gt * 4 + 4], ex4[:, 0:4], ex4[:, 6:7], None, ALU.mult, r=[ex4.g], w=[wts.g])
                        kb.ts("pool", msk[:], lg[:], m8[:, 3:4], None, ALU.is_ge, r=[lg.g, m8.g], w=[msk.g])

                    def p7Bl(tl):
                        lg = lgs[tl % 3]
                        gt = half * 8 + tl
                        pb, pg = bank()
                        kb.mm(pb[:, 0:32], cst[:, K_STRI:K_STRI + 128], msk[:], True, False, r=[cst.g, msk.g], w=[pg], inc=False)
                        kb.mm(pb[:, 0:32], cst[:, K_ONE1:K_ONE1 + 128], sprev[:], False, True, r=[cst.g, sprev.g], w=[pg])
                        kb.copy("act", posS[:], pb[:, 0:32], r=[pg], w=[posS.g])

                    def p7Bl1(tl):
                        lg = lgs[tl % 3]
                        gt = half * 8 + tl
                        kb.ts("pool", ovf[:], posS[:], float(CAP), None, ALU.is_ge, r=[posS.g], w=[ovf.g])
                        kb.tt("pool", posE[:], posS[:], cst[:, K_ECAP:K_ECAP + 32], ALU.add, r=[posS.g, cst.g], w=[posE.g])
                        kb.ts("pool", prod[:], posE[:], -1.0, float(E * CAP), ALU.mult, ALU.add, r=[posE.g], w=[prod.g])
                        kb.tt("pool", prod[:], prod[:], ovf[:], ALU.mult, r=[ovf.g], w=[prod.g])
                        kb.tt("pool", posE[:], posE[:], prod[:], ALU.add, r=[prod.g], w=[posE.g])
                        kb.tt("pool", sprev[:], sprev[:], msk[:], ALU.add, r=[msk.g], w=[sprev.g])

                    def p7C(tl):
                        gt = half * 8 + tl
                        hb_ = h1b[tl % 4]
                        lg = lgs[tl % 3]
                        m8 = m8s[tl % 2]
                        for k in range(4):
                            kb.stt("dve", prod4[:, k, :], lg[:], m8[:, k:k + 1], posE[:], ALU.is_equal, ALU.mult,
                                   r=[lg.g, m8.g, posE.g], w=[prod4.g])
                        kb.op("dve", lambda e, a=destf[:, 0:4], b_=prod4[:, :, :]: e.reduce_sum(a, b_, AX.X), r=[prod4.g], w=[destf.g])
                        kb.copy("dve", desti[:, gt * 4:gt * 4 + 4], destf[:], r=[destf.g], w=[desti_g[gt]])
                        for k in range(4):
                            kb.dma("pool", lambda e, idx=desti[:, gt * 4 + k:gt * 4 + k + 1], src=hb_[:, :]: e.indirect_dma_start(
                                out=xbuf, out_offset=bass.IndirectOffsetOnAxis(ap=idx, axis=0), in_=src, in_offset=None,
                                bounds_check=bcreg, oob_is_err=False), r=[hb_.g, desti_g[gt]], w=[xg])

                    for st_ in range(11):
                        if 0 <= st_ - 2 < 8:
                            p7Be(st_ - 2)
                        if st_ < 8:
                            p7A0(st_)
                        if 0 <= st_ - 1 < 8:
                            p7A2(st_ - 1)
                        if st_ < 8:
                            p7A(st_)
                        if 0 <= st_ - 1 < 8:
                            p7A2b(st_ - 1)
                        if 0 <= st_ - 2 < 8:
                            p7Bl(st_ - 2)
                        if 0 <= st_ - 3 < 8:
                            p7C(st_ - 3)
                        if 0 <= st_ - 2 < 8:
                            p7Bl1(st_ - 2)
                    kb.barrier()
            if dbg and stage == 7:
                kb.ld(dbg_out("wts", [128, 64]), wts[:], r=[wts.g])
                kb.ld(dbg_out("desti", [128, 64], I32), desti[:], r=desti_g)
                kb.ld(dbg_out("h1", [NT, D]), h1_dram, r=[h1g])


        if stage >= 8:
            p7.close()
            mix.close()
            with contextlib.ExitStack() as p8:
                NS = 16
                wslot = [T(kb, p8, "wslot%d" % i, [128, 8, 512], BF16) for i in range(NS)]
                xtok = [T(kb, p8, "xtok%d" % i, [128, 3, 1024], BF16) for i in range(2)]
                xT = [T(kb, p8, "xT%d" % i, [128, 8, CAP], BF16) for i in range(2)]
                actT = T(kb, p8, "actT", [128, 8, CAP], BF16)
                bdn = [T(kb, p8, "bdn%d" % i, [128, 1024], F32) for i in range(2)]
                gg = [T(kb, p8, "gg%d" % i, [128, CAP], F32) for i in range(2)]
                sg8 = [T(kb, p8, "sg8%d" % i, [128, CAP], F32) for i in range(2)]
                uu = [T(kb, p8, "uu%d" % i, [128, CAP], F32) for i in range(2)]
                yo = [T(kb, p8, "yo%d" % i, [128, 1024], F32) for i in range(2)]
                it = 0

                def load_gu(e2):
                    ps_ = []
                    for pc in range(4):
                        wsl = wslot[(e2 * 6 + pc) % NS]
                        wload(wsl, wsl.g, w_gu[e2], 512, c0=pc * 512, kstep=8)
                        ps_.append(wsl)
                    return ps_

                def load_dn(e2):
                    ps_ = []
                    for pc in range(2):
                        wsl = wslot[(e2 * 6 + 4 + pc) % NS]
                        wload(wsl, wsl.g, w_down[e2], 512, c0=pc * 512, kstep=8)
                        ps_.append(wsl)
                    return ps_

                def prep_expert(e2):
                    xk2, xt2, bd2 = xtok[e2 % 2], xT[e2 % 2], bdn[e2 % 2]
                    kb.ld(xk2[:], xbuf[e2 * CAP:(e2 + 1) * CAP, :].rearrange("(c p) d -> p c d", p=128), r=[xg], w=[xk2.g])
                    kb.ld(bd2[:], b_down[e2:e2 + 1, :].partition_broadcast(128), w=[bd2.g])
                    for sc in range(3):
                        for c in range(8):
                            kb.tr(pbt[:, c * 128:(c + 1) * 128], xk2[:, sc, c * 128:(c + 1) * 128], identb[:], r=[xk2.g, identb.g], w=[pbt_g], inc=(c == 7))
                        kb.copy("act" if sc % 2 else "dve", xt2[:, :, sc * 128:(sc + 1) * 128], pbt[:, :].rearrange("p (c i) -> p c i", c=8),
                                r=[pbt_g], w=[xt2.g])

                loaded = {0: load_gu(0) + load_dn(0), 1: load_gu(1) + load_dn(1)}
                for e_ in range(E):
                    pieces = loaded.pop(e_)
                    if e_ + 2 < E:
                        loaded[e_ + 2] = load_gu(e_ + 2)
                    xk, xt_ = xtok[e_ % 2], xT[e_ % 2]
                    bd = bdn[e_ % 2]
                    if e_ == 0:
                        prep_expert(0)
                    for j in range(8):
                        g_, s_, u_ = gg[it % 2], sg8[it % 2], uu[it % 2]
                        it += 1
                        pg_t, pg_g = bank()
                        wg_ = pieces[j // 4]
                        for k in range(8):
                            kb.mm(pg_t[:, 0:CAP], wg_[:, k, (j % 4) * 128:(j % 4 + 1) * 128], xt_[:, k, :], k == 0, k == 7, r=[wg_.g, xt_.g], w=[pg_g])
                        pu_t, pu_g = bank()
                        wu_ = pieces[2 + j // 4]
                        for k in range(8):
                            kb.mm(pu_t[:, 0:CAP], wu_[:, k, (j % 4) * 128:(j % 4 + 1) * 128], xt_[:, k, :], k == 0, k == 7, r=[wu_.g, xt_.g], w=[pu_g])
                        bgc = col(C_BGU + e_ * 16 + j)
                        buc = col(C_BGU + e_ * 16 + 8 + j)
                        kb.ts("dve", g_[:], pg_t[:, 0:CAP], bgc, 7.0, ALU.add, ALU.min, r=[pg_g, colsT.g], w=[g_.g])
                        kb.act(s_[:], g_[:], AF.Sigmoid, r=[g_.g], w=[s_.g], scale=1.702)
                        kb.act(u_[:], pu_t[:, 0:CAP], AF.Identity, r=[pu_g, colsT.g], w=[u_.g], bias=buc)
                        kb.ts("dve", u_[:], u_[:], 7.0, -7.0, ALU.min, ALU.max, r=[], w=[u_.g])
                        kb.tt("dve", g_[:], g_[:], s_[:], ALU.mult, r=[s_.g], w=[g_.g])
                        kb.stt("dve", actT[:, j, :], u_[:], 1.0, g_[:], ALU.add, ALU.mult, r=[g_.g, u_.g], w=[actT.g])
                    if e_ + 2 < E:
                        loaded[e_ + 2] = loaded[e_ + 2] + load_dn(e_ + 2)
                    if e_ + 1 < E:
                        prep_expert(e_ + 1)
                    for sc in range(3):
                        y_ = yo[sc % 2]
                        for h2 in range(2):
                            pb, pg = bank()
                            wd_ = pieces[4 + h2]
                            for j in range(8):
                                kb.mm(pb[:, :], actT[:, j, sc * 128:(sc + 1) * 128], wd_[:, j, :], j == 0, j == 7, r=[actT.g, wd_.g], w=[pg])
                            kb.tt("dve", y_[:, h2 * 512:(h2 + 1) * 512], pb[:, :], bd[:, h2 * 512:(h2 + 1) * 512], ALU.add, r=[pg, bd.g], w=[y_.g])
                        r0 = e_ * CAP + sc * 128
                        kb.ld(ybuf[r0:r0 + 128, :], y_[:], r=[y_.g], w=[yg])
                kb.barrier()
            with contextlib.ExitStack() as p9:
                l2 = T(kb, p9, "l2", [128, 2, 1024], F32)
                bcast_load(l2[:, 0, :], V_L2G, 1024, l2.g)
                bcast_load(l2[:, 1, :], V_L2B, 1024, l2.g)
                ygt = [T(kb, p9, "ygt%d" % i, [128, 4, 1024], F32) for i in range(4)]
                h1r = [T(kb, p9, "h1r%d" % i, [128, 1024], F32) for i in range(4)]
                acc = [T(kb, p9, "acc%d" % i, [128, 1024], F32) for i in range(4)]
                s9 = [T(kb, p9, "s9_%d" % i, [128, 16], F32) for i in range(4)]
                og_ = kb.reg("out")
                def combine_steps(gt):
                    p = gt % 4
                    y4, hr, ac, st9 = ygt[p], h1r[p], acc[p], s9[p]
                    for k in range(4):
                        kb.dma("pool", lambda e, idx=desti[:, gt * 4 + k:gt * 4 + k + 1], dst=y4[:, k, :]: e.indirect_dma_start(
                            out=dst, out_offset=None, in_=ybuf, in_offset=bass.IndirectOffsetOnAxis(ap=idx, axis=0),
                            bounds_check=bcreg, oob_is_err=False), r=[yg, desti_g[gt]], w=[y4.g])
                    kb.ld(hr[:], h1_dram[gt * 128:(gt + 1) * 128, :], r=[h1g], w=[hr.g])
                    yield
                    kb.stt("dve", ac[:], y4[:, 0, :], wts[:, gt * 4:gt * 4 + 1], hr[:], ALU.mult, ALU.add, r=[y4.g, wts.g, hr.g], w=[ac.g])
                    yield
                    kb.stt("dve", ac[:], hr[:], ALPHA - 1.0, ac[:], ALU.mult, ALU.add, r=[hr.g], w=[ac.g])
                    yield
                    for k in range(1, 4):
                        kb.stt("dve", ac[:], y4[:, k, :], wts[:, gt * 4 + k:gt * 4 + k + 1], ac[:], ALU.mult, ALU.add, r=[y4.g, wts.g], w=[ac.g])
                        yield
                    ln_tile(ac, ac.g, st9, st9.g, None, None)
                    yield
                    kb.stt("dve", hr[:], ac[:], st9[:, 0:1], l2[:, 0, :], ALU.subtract, ALU.mult, r=[ac.g, st9.g, l2.g], w=[hr.g])
                    yield
                    kb.stt("dve", hr[:], hr[:], st9[:, 2:3], l2[:, 1, :], ALU.mult, ALU.add, r=[st9.g, l2.g], w=[hr.g])
                    yield
                    kb.ld(out_d[gt * 128:(gt + 1) * 128, :], hr[:], r=[hr.g], w=[og_])

                for g0 in range(0, 16, 2):
                    gens = [combine_steps(g0), combine_steps(g0 + 1)]
                    while gens:
                        for g_ in list(gens):
                            try:
                                next(g_)
                            except StopIteration:
                                gens.remove(g_)
                kb.barrier()

        kb.barrier()
    return nc, dbg_d


def prep_inputs(inp):
    x = np.asarray(inp["x"], np.float32)
    mem = np.asarray(inp["mem"], np.float32)
    w_in = np.ascontiguousarray(np.asarray(inp["w_in"], np.float32)[0])
    b_in = np.asarray(inp["b_in"], np.float32)[0]
    cst = host_constants()
    cd = chan_dft()
    dfts = [dft_mats(0), dft_mats(1)]
    g1 = lambda k: np.asarray(inp[k], np.float32).reshape(-1)
    vecs = np.concatenate([b_in[OK_:OK_ + 512], b_in[OV:OV + 1024], b_in[OFN:OFN + 512], g1("b_out"),
                           g1("ln_in_g"), g1("ln_in_b"), g1("ln1_g"), g1("ln1_b"), g1("ln2_g"), g1("ln2_b"),
                           g1("b_router")]).astype(np.float32)[None, :]
    assert vecs.shape[1] == NVEC
    maps = []
    for c in range(8):
        b, hf = c // 2, c % 2
        if hf == 0:
            xo, xt = x[b, :NT], x[b, NT:]
            lf, lb, df, db = OLF, OLB, "f", "b"
        else:
            xo, xt = x[b, ::-1][:NT], x[b, ::-1][NT:]
            lf, lb, df, db = OLB, OLF, "b", "f"
        w_lr = np.concatenate([w_in[:, lf:lf + 16], w_in[:, lb:lb + 16]], axis=1)
        wdec = np.zeros((2, 32, 512), np.float32)
        for i, dd in enumerate((df, db)):
            wdec[i, :16] = np.asarray(inp["w_decay_" + dd], np.float32)[0]
            wdec[i, 16] = np.asarray(inp["b_decay_" + dd], np.float32)[0]
        colsT = np.zeros((128, NCOLS), np.float32)

        def put(c0, vec):
            v = np.asarray(vec, np.float32).reshape(-1, 128)
            colsT[:, c0:c0 + v.shape[0]] = v.T

        put(C_LNG, g1("ln_in_g")); put(C_LNB, g1("ln_in_b")); put(C_MG, g1("ln_mem_g")); put(C_MB, g1("ln_mem_b"))
        put(C_BQ, b_in[OQ:OQ + 512]); put(C_BK, b_in[OK_:OK_ + 512]); put(C_BR, b_in[OR:OR + 1024])
        put(C_BMQ, b_in[OMQ:OMQ + 512]); put(C_BG, b_in[OG:OG + 3072]); put(C_GN, g1("gla_norm_g"))
        colsT[:16, C_BLF] = b_in[lf:lf + 16]
        colsT[:16, C_BLB] = b_in[lb:lb + 16]
        put(C_BGU, np.asarray(inp["b_gu"], np.float32)[0].reshape(-1))
        maps.append({
            "x_own": np.ascontiguousarray(xo), "x_oth": np.ascontiguousarray(xt), "mem": np.ascontiguousarray(mem[b]),
            "w_in": w_in, "w_lr": np.ascontiguousarray(w_lr), "wdec": wdec, "colsT": colsT, "vecs": vecs, "cst": cst,
            "dftC": dfts[hf][0], "dftS": dfts[hf][1], "cdft": cd,
            "w_br_gla": np.asarray(inp["w_br_gla"], np.float32)[0], "w_br_fnet": np.asarray(inp["w_br_fnet"], np.float32)[0],
            "w_br_mem": np.asarray(inp["w_br_mem"], np.float32)[0], "w_mem_kv": np.asarray(inp["w_mem_kv"], np.float32)[0],
            "w_out": np.asarray(inp["w_out"], np.float32)[0], "w_router": np.asarray(inp["w_router"], np.float32)[0],
            "w_gu": np.asarray(inp["w_gu"], np.float32)[0], "w_down": np.asarray(inp["w_down"], np.float32)[0],
            "b_down": np.asarray(inp["b_down"], np.float32)[0],
        })
    return maps


def kernel(**inputs):
    nc, _ = build()
    maps = prep_inputs(inputs)
    res = run_bass_kernel_spmd(nc, maps, core_ids=list(range(8)))
    out = np.zeros((4, S, D), np.float32)
    for c in range(8):
        b, hf = c // 2, c % 2
        o = np.asarray(res.results[c]["out"], np.float32)
        if hf == 0:
            out[b, :NT] = o
        else:
            out[b, NT:] = o[::-1]
    return out
```

```python
import contextlib
import math
import numpy as np
import ml_dtypes
import concourse.bass as bass
import concourse.mybir as mybir
from concourse.bass_utils import run_bass_kernel_spmd

F32 = mybir.dt.float32
BF16 = mybir.dt.bfloat16
I32 = mybir.dt.int32
U32 = mybir.dt.uint32
AF = mybir.ActivationFunctionType
ALU = mybir.AluOpType
AX = mybir.AxisListType

D = 1024
S = 4096
NT = 2048
NTILE = 16
E = 32
CAP = 384
ALPHA = 2.0 ** 0.25
LN_EPS = 1e-5
RMS_EPS = 1e-6
OQ, OK_, OV, OR, OLF, OLB, OFN, OMQ, OG = 0, 512, 1024, 2048, 3072, 3088, 3104, 3616, 4128
C_LNG, C_LNB, C_MG, C_MB, C_BQ, C_BK, C_BR, C_BMQ, C_BG, C_GN, C_BLF, C_BLB, C_BGU = 0, 8, 16, 24, 32, 36, 40, 48, 52, 76, 78, 79, 80
NCOLS = 80 + 512
V_BK, V_BV, V_BFN, V_BOUT, V_LNG, V_LNB, V_L1G, V_L1B, V_L2G, V_L2B, V_BRT = 0, 512, 1536, 2048, 3072, 4096, 5120, 6144, 7168, 8192, 9216
NVEC = 9216 + 32
K_ID, K_TRIF, K_TRIB, K_TRIRF, K_TRIRB, K_MF, K_MB, K_ONES, K_STRI, K_ONE1, K_ECAP = 0, 128, 256, 384, 512, 640, 1152, 1664, 1792, 1920, 2048
NCST = 2048 + 32


class Reg:
    __slots__ = ("name", "w", "r", "pend")

    def __init__(self, name):
        self.name = name
        self.w = {}
        self.r = {}
        self.pend = None


class Eng:
    def __init__(self, name, sem):
        self.name = name
        self.sem = sem
        self.cnt = 0
        self.waited = {}
        self.ops = []
        self.pending = []
        self.dsems = []
        self.dval = {}
        self.di = 0


class KB:
    def __init__(self, nc, stack):
        self.nc = nc
        self.stack = stack
        self.E = {}
        for n in ("pe", "dve", "act", "pool", "sp"):
            self.E[n] = Eng(n, stack.enter_context(nc.semaphore("s_" + n)))
        for n, k in (("sp", 8), ("pool", 8), ("act", 4)):
            for i in range(k):
                s = stack.enter_context(nc.semaphore("d_%s%d" % (n, i)))
                self.E[n].dsems.append(s)
                self.E[n].dval[s] = 0
        self.nreg = 0
        self.dma_sems = set()
        for e_ in self.E.values():
            self.dma_sems.update(e_.dsems)
        self.eobj = {"pe": nc.tensor, "dve": nc.vector, "act": nc.scalar, "pool": nc.gpsimd, "sp": nc.sync}

    def reg(self, name=None):
        self.nreg += 1
        return Reg(name or ("r%d" % self.nreg))

    def _waits(self, E, r, w, skip_dma_w=False):
        waits = {}

        def need(sem, val):
            if E.waited.get(sem, 0) < val and waits.get(sem, 0) < val:
                waits[sem] = val

        for g in r:
            if g.pend is not None and g.pend != E.name:
                raise RuntimeError("region %s has pending updates from %s" % (g.name, g.pend))
            for sem, val in g.w.items():
                need(sem, val)
        for g in w:
            if g.pend is not None and g.pend != E.name:
                raise RuntimeError("region %s has pending updates from %s" % (g.name, g.pend))
            for sem, val in g.w.items():
                if skip_dma_w and sem in self.dma_sems:
                    continue
                need(sem, val)
            for sem, val in g.r.items():
                need(sem, val)
        if E.name == "pe":
            waits.pop(E.sem, None)
        for sem, val in waits.items():
            E.waited[sem] = val
        return list(waits.items())

    def op(self, en, fn, r=(), w=(), inc=True):
        E = self.E[en]
        wl = self._waits(E, r, w)
        if inc:
            E.cnt += 1
            tok = (E.sem, E.cnt)
            for rr, ww in E.pending + [(r, w)]:
                for g in ww:
                    g.w = {tok[0]: tok[1]}
                    g.r = {}
                    g.pend = None
                for g in rr:
                    if g not in ww:
                        g.r[E.sem] = E.cnt
                        g.pend = None
            E.pending = []
        else:
            E.pending.append((tuple(r), tuple(w)))
            for g in list(r) + list(w):
                g.pend = E.name
        self._emit(E, wl, fn, 1 if inc else 0, None)

    def _emit(self, E, wl, fn, inc, ds):
        eng = self.eobj[E.name]
        for sem, val in wl:
            eng.wait_ge(sem, val)
        if fn is None:
            return
        ins = fn(eng)
        if ds is not None:
            ins.then_inc(ds, 16)
        elif inc:
            ins.then_inc(E.sem, 1)

    def dma(self, en, fn, r=(), w=()):
        E = self.E[en]
        wl = self._waits(E, r, w, skip_dma_w=True)
        ds = E.dsems[E.di % len(E.dsems)]
        E.di += 1
        prev = E.dval[ds]
        if prev > 0 and E.waited.get(ds, 0) < prev:
            E.waited[ds] = prev
            wl.append((ds, prev))
        E.dval[ds] = prev + 16
        tok = (ds, prev + 16)
        for g in w:
            keep = {sm: v for sm, v in g.w.items() if sm in self.dma_sems}
            keep[ds] = prev + 16
            g.w = keep
            g.r = {}
        for g in r:
            if g not in w:
                g.r[ds] = prev + 16
        self._emit(E, wl, fn, 16, ds)

    def barrier(self):
        toks = []
        for E in self.E.values():
            if E.cnt > 0:
                toks.append((E.sem, E.cnt))
            for ds, v in E.dval.items():
                if v > 0:
                    toks.append((ds, v))
        for E in self.E.values():
            if E.pending:
                raise RuntimeError("pending at barrier on " + E.name)
            wl = []
            for sem, val in toks:
                if sem is E.sem and E.name == "pe":
                    continue
                if E.waited.get(sem, 0) < val:
                    E.waited[sem] = val
                    wl.append((sem, val))
            if wl:
                self._emit(E, wl, None, 0, None)

    def replay(self, en, eng):
        E = self.E[en]
        for wl, fn, inc, ds in E.ops:
            for sem, val in wl:
                eng.wait_ge(sem, val)
            if fn is None:
                continue
            ins = fn(eng)
            if ds is not None:
                ins.then_inc(ds, 16)
            elif inc:
                ins.then_inc(E.sem, 1)

    def mm(self, out, lhsT, rhs, start, stop, r=(), w=(), inc=None):
        if inc is None:
            inc = stop
        self.op("pe", lambda e: e.matmul(out, lhsT, rhs, start=start, stop=stop), r, w, inc)

    def tr(self, out, in_, ident, r=(), w=(), inc=True):
        self.op("pe", lambda e: e.transpose(out, in_, ident), r, w, inc)

    def act(self, out, in_, func, r=(), w=(), bias=0.0, scale=1.0, accum_out=None, en="act"):
        if accum_out is None:
            self.op(en, lambda e: e.activation(out, in_, func, bias=bias, scale=scale), r, w)
        else:
            self.op(en, lambda e: e.activation(out, in_, func, bias=bias, scale=scale, accum_out=accum_out), r, w)

    def tt(self, en, out, in0, in1, op, r=(), w=()):
        self.op(en, lambda e: e.tensor_tensor(out, in0, in1, op), r, w)

    def ts(self, en, out, in0, s1, s2, op0, op1=None, r=(), w=(), accum_out=None):
        if op1 is None:
            self.op(en, lambda e: e.tensor_scalar(out, in0, s1, None, op0), r, w)
        elif accum_out is not None:
            self.op(en, lambda e: e.tensor_scalar(out, in0, s1, s2, op0, op1, accum_out), r, w)
        else:
            self.op(en, lambda e: e.tensor_scalar(out, in0, s1, s2, op0, op1), r, w)

    def stt(self, en, out, in0, scalar, in1, op0, op1, r=(), w=()):
        self.op(en, lambda e: e.scalar_tensor_tensor(out, in0, scalar, in1, op0, op1), r, w)

    def copy(self, en, out, in_, r=(), w=()):
        if en == "act":
            self.op(en, lambda e: e.copy(out, in_), r, w)
        else:
            self.op(en, lambda e: e.tensor_copy(out, in_), r, w)

    def memset(self, en, ap, val, w=()):
        self.op(en, lambda e: e.memset(ap, val), (), w)

    def ld(self, out, in_, r=(), w=(), en="sp"):
        self.dma(en, lambda e: e.dma_start(out=out, in_=in_), r, w)


class T:
    def __init__(self, kb, stack, name, shape, dt):
        self.g = kb.reg(name)
        self.t = stack.enter_context(kb.nc.sbuf_tensor("sb_%s_%d" % (name, kb.nreg), list(shape), dt))

    def __getitem__(self, k):
        return self.t[k]


def host_constants():
    j = np.arange(128)[:, None]
    i = np.arange(128)[None, :]
    c = np.zeros((128, NCST), np.float32)
    c[:, K_ID:K_ID + 128] = (j == i)
    c[:, K_TRIF:K_TRIF + 128] = (j <= i) * (-1.0 / 16)
    c[:, K_TRIB:K_TRIB + 128] = (j >= i) * (-1.0 / 16)
    c[:, K_TRIRF:K_TRIRF + 128] = (j > i) * (-1.0 / 16)
    c[:, K_TRIRB:K_TRIRB + 128] = (j < i) * (-1.0 / 16)
    c[:, K_MF:K_MF + 512] = np.tile((j <= i).astype(np.float32), (1, 4))
    c[:, K_MB:K_MB + 512] = np.tile((j >= i).astype(np.float32), (1, 4))
    c[:, K_ONES:K_ONES + 128] = 1.0 / 256
    c[:, K_STRI:K_STRI + 128] = (j < i)
    c[:, K_ONE1:K_ONE1 + 128] = 1.0
    c[:, K_ECAP:K_ECAP + 32] = (np.arange(32) * CAP)[None, :]
    return c


def dft_mats(hf):
    tau = np.arange(S, dtype=np.int64)
    sg = tau if hf == 0 else (S - 1 - tau)
    prod = (sg[:, None] * sg[None, :NT]) % S
    th = prod.astype(np.float64) * (2 * np.pi / S)
    sc = 1.0 / math.sqrt(S)
    return ((np.cos(th) * sc).astype(ml_dtypes.bfloat16), (np.sin(th) * sc).astype(ml_dtypes.bfloat16))


def chan_dft():
    c = np.arange(128, dtype=np.int64)
    ph = ((c[:, None] * c[None, :]) % 128).astype(np.float64) * (2 * np.pi / 128)
    sc = 1.0 / math.sqrt(128)
    return np.concatenate([np.cos(ph) * sc, -np.sin(ph) * sc], axis=1).astype(ml_dtypes.bfloat16)


def build(stage=99, dbg=False):
    nc = bass.Bass("TRN2", target_bir_lowering=False)
    dr = lambda name, shape, dt, kind="ExternalInput": nc.dram_tensor(name, list(shape), dt, kind=kind).ap()
    x_own = dr("x_own", [NT, D], F32)
    x_oth = dr("x_oth", [NT, D], F32)
    mem = dr("mem", [256, D], F32)
    w_in = dr("w_in", [D, 7200], F32)
    w_lr = dr("w_lr", [D, 32], F32)
    wdec = dr("wdec", [2, 32, 512], F32)
    colsT_d = dr("colsT", [128, NCOLS], F32)
    vecs_d = dr("vecs", [1, NVEC], F32)
    cst_d = dr("cst", [128, NCST], F32)
    dftC = dr("dftC", [S, NT], BF16)
    dftS = dr("dftS", [S, NT], BF16)
    cdft_d = dr("cdft", [128, 256], BF16)
    w_br_gla = dr("w_br_gla", [1024, D], F32)
    w_br_fnet = dr("w_br_fnet", [512, D], F32)
    w_br_mem = dr("w_br_mem", [512, D], F32)
    w_mem_kv = dr("w_mem_kv", [D, 1024], F32)
    w_out = dr("w_out", [D, D], F32)
    w_router = dr("w_router", [D, E], F32)
    if stage >= 8:
        w_gu = dr("w_gu", [E, D, 2048], F32)
        w_down = dr("w_down", [E, D, D], F32)
        b_down = dr("b_down", [E, D], F32)
    out_d = dr("out", [NT, D], F32, "ExternalOutput")
    dbg_d = {}

    def dbg_out(name, shape, dt=F32):
        dbg_d[name] = dr("dbg_" + name, shape, dt, "ExternalOutput")
        return dbg_d[name]

    xbuf = dr("xbuf", [E * CAP + 1, D], BF16, "Internal")
    ybuf = dr("ybuf", [E * CAP + 1, D], F32, "Internal")
    h1_dram = dr("h1_dram", [NT, D], F32, "Internal")

    with contextlib.ExitStack() as top:
        kb = KB(nc, top)
        bcreg = nc.gpsimd.alloc_register("bcreg")
        nc.gpsimd.reg_mov(bcreg, E * CAP)
        banks = []
        for i in range(7):
            t = top.enter_context(nc.psum_tensor("pb%d" % i, [128, 512], F32))
            banks.append((t, kb.reg("pb%d" % i)))
        pbt = top.enter_context(nc.psum_tensor("pbt", [128, 1024], BF16))
        pbt_g = kb.reg("pbt")
        bstate = {"i": 0}

        def bank():
            b = banks[bstate["i"] % 7]
            bstate["i"] += 1
            return b

        cst = T(kb, top, "cst", [128, NCST], F32)
        colsT = T(kb, top, "colsT", [128, NCOLS], F32)
        identb = T(kb, top, "identb", [128, 128], BF16)
        wts = T(kb, top, "wts", [128, 64], F32)
        desti = T(kb, top, "desti", [128, 64], I32)
        desti_g = [kb.reg("desti%d" % i) for i in range(16)]
        kb.ld(cst[:], cst_d, w=[cst.g])
        kb.ld(colsT[:], colsT_d, w=[colsT.g])
        kb.copy("dve", identb[:], cst[:, K_ID:K_ID + 128], r=[cst.g], w=[identb.g])
        ident = cst[:, K_ID:K_ID + 128]

        def col(c0, n=1):
            return colsT[:, c0:c0 + n]

        def bcast_load(tile_ap, off, n, g):
            kb.ld(tile_ap, vecs_d[:, off:off + n].partition_broadcast(128), w=[g])

        def ln_tile(xt, xg, stats, sg, out_bf, og):
            kb.op("dve", lambda e: e.bn_stats(stats[:, 4:10], xt[:, 0:512]), r=[xg], w=[sg])
            kb.op("dve", lambda e: e.bn_stats(stats[:, 10:16], xt[:, 512:1024]), r=[xg], w=[sg])
            kb.op("dve", lambda e: e.bn_aggr(stats[:, 0:2], stats[:, 4:16]), r=[sg], w=[sg])
            kb.act(stats[:, 3:4], stats[:, 1:2], AF.Sqrt, r=[sg], w=[sg], bias=LN_EPS)
            kb.op("dve", lambda e: e.reciprocal(stats[:, 2:3], stats[:, 3:4]), r=[sg], w=[sg])
            if out_bf is not None:
                kb.ts("dve", out_bf, xt[:, :], stats[:, 0:1], stats[:, 2:3], ALU.subtract, ALU.mult, r=[xg, sg], w=[og])

        def transpose_to_fm(src_bf, sg_, dstT, dg, tok0, gcol, bcol):
            for c in range(8):
                kb.tr(pbt[:, c * 128:(c + 1) * 128], src_bf[:, c * 128:(c + 1) * 128], identb[:],
                      r=[sg_, identb.g], w=[pbt_g], inc=(c == 7))
            for c in range(8):
                if c % 2 == 0:
                    kb.act(dstT[:, c, tok0:tok0 + 128], pbt[:, c * 128:(c + 1) * 128], AF.Identity,
                           r=[pbt_g, colsT.g], w=[dg], bias=col(bcol + c), scale=col(gcol + c))
                else:
                    kb.ts("dve", dstT[:, c, tok0:tok0 + 128], pbt[:, c * 128:(c + 1) * 128], col(gcol + c), col(bcol + c),
                          ALU.mult, ALU.add, r=[pbt_g, colsT.g], w=[dg])

        def wload(tile, g, src, ncols, kchunks=8, c0=0, r0=0, kstep=8):
            for k0 in range(0, kchunks, kstep):
                k1 = min(kchunks, k0 + kstep)
                srcv = src[r0 + k0 * 128:r0 + k1 * 128, c0:c0 + ncols].rearrange("(k p) f -> p k f", p=128)
                kb.ld(tile[:, k0:k1, 0:ncols], srcv, w=[g], en="pool")

        mkT = T(kb, top, "mkT", [128, 4, 256], BF16)
        mv = T(kb, top, "mv", [128, 2, 512], BF16)
        xg = kb.reg("xbuf")
        yg = kb.reg("ybuf")
        with contextlib.ExitStack() as ph:
            ztf = T(kb, ph, "ztf", [1, 1024], F32)
            kb.memset("pool", ztf[:], 0.0, w=[ztf.g])
            kb.ld(ybuf[E * CAP:E * CAP + 1, :], ztf[:], r=[ztf.g], w=[yg])
            wkv = T(kb, ph, "wkv", [128, 8, 1024], BF16)
            wload(wkv, wkv.g, w_mem_kv, 1024)
            memT = T(kb, ph, "memT", [128, 8, 256], BF16)
            for mt in range(2):
                xt = T(kb, ph, "memx%d" % mt, [128, 1024], F32)
                st = T(kb, ph, "memst%d" % mt, [128, 16], F32)
                xb = T(kb, ph, "memxb%d" % mt, [128, 1024], BF16)
                kb.ld(xt[:], mem[mt * 128:(mt + 1) * 128, :], w=[xt.g])
                ln_tile(xt, xt.g, st, st.g, xb[:], xb.g)
                transpose_to_fm(xb, xb.g, memT, memT.g, mt * 128, C_MG, C_MB)
            for h in range(4):
                pb, pg = bank()
                for k in range(8):
                    kb.mm(pb[:, 0:256], wkv[:, k, h * 128:(h + 1) * 128], memT[:, k, :], k == 0, k == 7,
                          r=[wkv.g, memT.g], w=[pg])
                kb.copy("dve", mkT[:, h, :], pb[:, 0:256], r=[pg], w=[mkT.g])
            for mt in range(2):
                pb, pg = bank()
                for k in range(8):
                    kb.mm(pb[:, :], memT[:, k, mt * 128:(mt + 1) * 128], wkv[:, k, 512:1024], k == 0, k == 7,
                          r=[wkv.g, memT.g], w=[pg])
                kb.copy("dve", mv[:, mt, :], pb[:, :], r=[pg], w=[mv.g])
            kb.barrier()
        if dbg and stage == 0:
            o1 = dbg_out("mkT", [128, 4 * 256], BF16)
            kb.ld(o1, mkT[:].rearrange("p a b -> p (a b)"), r=[mkT.g])
            o2 = dbg_out("mv", [128, 2 * 512], BF16)
            kb.ld(o2, mv[:].rearrange("p a b -> p (a b)"), r=[mv.g])


        mix = top.enter_context(contextlib.ExitStack())
        hT = T(kb, mix, "hT", [128, 8, NT], BF16)
        slots = T(kb, mix, "slots", [128, 16, 1024], BF16)
        slot_g = [kb.reg("slot%d" % i) for i in range(17)]
        FT = T(kb, mix, "FT", [128, 4, NT], BF16)
        mixA = top.enter_context(contextlib.ExitStack())
        stB = T(kb, mixA, "stB", [128, 1024], F32)
        stBb = T(kb, mixA, "stBb", [128, 1024], BF16)
        wk = T(kb, mixA, "wk", [128, 8, 512], BF16)
        wv = T(kb, mixA, "wv", [128, 8, 1024], BF16)
        wlr = T(kb, mixA, "wlr", [128, 8, 32], BF16)
        wdec_sb = T(kb, mixA, "wdec_sb", [32, 2, 512], F32)
        bkv = T(kb, mixA, "bkv", [128, 2048], F32)
        wload(wk, wk.g, w_in, 512, c0=OK_)
        wload(wv, wv.g, w_in, 1024, c0=OV)
        wload(wlr, wlr.g, w_lr, 32)
        kb.ld(wdec_sb[:, 0, :], wdec[0], w=[wdec_sb.g])
        kb.ld(wdec_sb[:, 1, :], wdec[1], w=[wdec_sb.g])
        bcast_load(bkv[:, 0:2048], V_BK, 2048, bkv.g)
        kb.memset("pool", stB[:], 0.0, w=[stB.g])

        def state_pass(ph, xsrc, own, dirn, f_tok, wfn, tmp, hTd):
            st = tmp["st"]
            hist = []
            qcnt = [0]
            blocks = list(range(4)) if own else list(range(3, -1, -1))
            hregs = [kb.reg("hblk%d" % i) for i in range(4)]
            fcnt = [0]
            zcnt = [0]

            def front_tile(blk, t):
                p = fcnt[0] % 2
                fcnt[0] += 1
                xt, stt_, xb = tmp["x"][p], tmp["stat"][p], tmp["xb"][p]
                r0 = blk * 512 + t * 128
                kb.ld(xt[:], xsrc[r0:r0 + 128, :], w=[xt.g])
                if own:
                    for _ in range(6):
                        zi = zcnt[0]
                        zcnt[0] += 1
                        kb.ld(xbuf[zi * 128:(zi + 1) * 128, :], slots[:, 0, :], r=[slot_g[0]], w=[xg], en="pool")
                ln_tile(xt, xt.g, stt_, stt_.g, xb[:], xb.g)
                transpose_to_fm(xb, xb.g, hTd, hregs[blk], r0, C_LNG, C_LNB)

            for t in range(4):
                front_tile(blocks[0], t)
            for bi, blk in enumerate(blocks):
                nxt_blk = blocks[bi + 1] if bi + 1 < 4 else None
                ftl = [0]
                hv = lambda k, a, b_, blk=blk: hTd[:, k, blk * 512 + a: blk * 512 + b_]
                hg = hregs[blk]
                lrT = tmp["lrT"]
                pb, pg = bank()
                for k in range(8):
                    kb.mm(pb[0:16, :], wlr[:, k, dirn * 16:(dirn + 1) * 16], hv(k, 0, 512), k == 0, k == 7,
                          r=[wlr.g, hg], w=[pg])
                kb.act(lrT[0:16, :], pb[0:16, :], AF.Identity, r=[pg, colsT.g], w=[lrT.g],
                       bias=colsT[0:16, C_BLF + dirn:C_BLF + dirn + 1])
                tiles = range(4) if own else range(3, -1, -1)

                def stageA(t, q, blk=blk, hv=hv, hg=hg):
                    p = t % 2
                    gt = blk * 4 + t
                    ktok, e1, L, kst, dec = (tmp[n][p] for n in ("ktok", "e1", "L", "kst", "dec"))
                    vtok = tmp["vtok"][q % 3]
                    pb, pg = bank()
                    for k in range(8):
                        kb.mm(pb[:, :], hv(k, t * 128, (t + 1) * 128), wk[:, k, :], k == 0, k == 7, r=[hg, wk.g], w=[pg])
                    kb.tt("dve", ktok[:], pb[:, :], bkv[:, 0:512], ALU.add, r=[pg, bkv.g], w=[ktok.g])
                    for hh in range(2):
                        pb, pg = bank()
                        for k in range(8):
                            kb.mm(pb[:, :], hv(k, t * 128, (t + 1) * 128), wv[:, k, hh * 512:(hh + 1) * 512], k == 0, k == 7,
                                  r=[hg, wv.g], w=[pg])
                        kb.tt("dve", vtok[:, hh * 512:(hh + 1) * 512], pb[:, :], bkv[:, 512 + hh * 512:1024 + hh * 512], ALU.add,
                              r=[pg, bkv.g], w=[vtok.g])
                    pb, pg = bank()
                    for k in range(8):
                        kb.mm(pb[:, :], hv(k, t * 128, (t + 1) * 128), wfn[:, k, :], k == 0, k == 7, r=[hg, wfn.g], w=[pg])
                    fidx = gt if own else 16 + gt
                    kb.tt("dve", f_tok[:, fidx, :], pb[:, :], bkv[:, 1536:2048], ALU.add, r=[pg, bkv.g], w=[f_tok.g])
                    pb, pg = bank()
                    kb.mm(pb[:, :], lrT[0:32, t * 128:(t + 1) * 128], wdec_sb[0:32, dirn, :], True, True,
                          r=[lrT.g, wdec_sb.g], w=[pg])
                    kb.act(e1[:], pb[:, :], AF.Exp, r=[pg], w=[e1.g], scale=-1.0)
                    kb.act(L[:], e1[:], AF.Ln, r=[e1.g], w=[L.g], bias=1.0)

                def stageB1(t, q, blk=blk):
                    p = t % 2
                    ktok, e1, L, kst, dec = (tmp[n][p] for n in ("ktok", "e1", "L", "kst", "dec"))
                    pb, pg = bank()
                    tri = cst[:, K_TRIRF:K_TRIRF + 128] if dirn == 0 else cst[:, K_TRIRB:K_TRIRB + 128]
                    kb.mm(pb[:, :], tri, L[:], True, True, r=[cst.g, L.g], w=[pg])
                    kb.act(e1[:], pb[:, :], AF.Exp, r=[pg], w=[e1.g])
                    kb.tt("pool", kst[:], ktok[:], e1[:], ALU.mult, r=[ktok.g, e1.g], w=[kst.g])
                    pb, pg = bank()
                    for h in range(4):
                        kb.mm(pb[:, h:h + 1], L[:, h * 128:(h + 1) * 128], cst[:, K_TRIF + 127:K_TRIF + 128], True, True,
                              r=[L.g, cst.g], w=[pg], inc=(h == 3))
                    kb.act(dec[:], pb[:, 0:4], AF.Exp, r=[pg], w=[dec.g])

                def stageB2(t, q, blk=blk):
                    p = t % 2
                    gt = blk * 4 + t
                    kst, dec = tmp["kst"][p], tmp["dec"][p]
                    vtok = tmp["vtok"][q % 3]
                    for hp in range(2):
                        pb, pg = bank()
                        for h2 in range(2):
                            h = hp * 2 + h2
                            kb.mm(pb[:, h2 * 256:(h2 + 1) * 256], kst[:, h * 128:(h + 1) * 128], vtok[:, h * 256:(h + 1) * 256],
                                  True, True, r=[kst.g, vtok.g], w=[pg], inc=(h2 == 1))
                        for h2 in range(2):
                            h = hp * 2 + h2
                            kb.stt("dve", st[:, h * 256:(h + 1) * 256], st[:, h * 256:(h + 1) * 256], dec[:, h:h + 1],
                                   pb[:, h2 * 256:(h2 + 1) * 256], ALU.mult, ALU.add, r=[pg, dec.g], w=[st.g])
                    if own and gt < 15:
                        kb.copy("act", slots[:, gt + 1, :], st[:], r=[st.g], w=[slot_g[gt + 1]])

                for t in tiles:
                    q = qcnt[0]
                    qcnt[0] += 1
                    stageA(t, q)
                    if nxt_blk is not None:
                        front_tile(nxt_blk, ftl[0])
                        ftl[0] += 1
                    if len(hist) >= 2:
                        hist[-2][1]()
                    if len(hist) >= 1:
                        hist[-1][0]()
                    hist.append(((lambda t=t, q=q, f=stageB1: f(t, q)), (lambda t=t, q=q, f=stageB2: f(t, q))))
            if len(hist) >= 2:
                hist[-2][1]()
            hist[-1][0]()
            hist[-1][1]()

        with contextlib.ExitStack() as ph:
            f_tok = T(kb, ph, "f_tok", [128, 32, 512], BF16)
            with contextlib.ExitStack() as ph2:
                wfn = T(kb, ph2, "wfn", [128, 8, 512], BF16)
                wload(wfn, wfn.g, w_in, 512, c0=OFN)
                tmp = {"st": stB}
                class V:
                    def __init__(self, ap, name):
                        self.ap = ap
                        self.g = kb.reg(name)

                    def __getitem__(self, k):
                        return self.ap[k]
                hTo = V(slots[:].rearrange("p a b -> p (a b)").rearrange("p (k f) -> p k f", k=8), "hTo")
                tmp["xb"] = [V(FT[:, 2, i * 1024:(i + 1) * 1024], "xb%d" % i) for i in range(2)]
                tmp["vtok"] = [V(FT[:, 3, i * 1024:(i + 1) * 1024], "vtok%d" % i) for i in range(2)] + [V(FT[:, 1, 0:1024], "vtok2")]
                tmp["lrT"] = T(kb, ph2, "lrT", [32, 512], F32)
                kb.memset("pool", tmp["lrT"][:], 1.0, w=[tmp["lrT"].g])
                x1 = T(kb, ph2, "sp_x", [128, 1024], F32)
                tmp["x"] = [x1, V(FT[:, 0, :].bitcast(F32), "sp_x2")]
                for n, shp, dt in (("stat", [128, 16], F32),
                                   ("ktok", [128, 512], F32), ("e1", [128, 512], F32),
                                   ("L", [128, 512], F32), ("kst", [128, 512], BF16), ("dec", [128, 4], F32)):
                    tmp[n] = [T(kb, ph2, "sp_%s%d" % (n, i), shp, dt) for i in range(2)]
                state_pass(ph2, x_oth, False, 1, f_tok, wfn, tmp, hTo)
                kb.copy("act", stBb[:], stB[:], r=[stB.g], w=[stBb.g])
                kb.barrier()
                kb.memset("pool", slots[:, 0, :], 0.0, w=[slot_g[0]])
                kb.ld(xbuf[E * CAP:E * CAP + 1, :], slots[0:1, 0, :], r=[slot_g[0]], w=[xg])
                stF = T(kb, ph2, "stF", [128, 1024], F32)
                kb.memset("pool", stF[:], 0.0, w=[stF.g])
                tmp["st"] = stF
                state_pass(ph2, x_own, True, 0, f_tok, wfn, tmp, hT)
                if dbg and stage == 2:
                    kb.ld(dbg_out("stB", [128, 1024]), stB[:], r=[stB.g])
                    kb.ld(dbg_out("stF", [128, 1024]), stF[:], r=[stF.g])
                    kb.ld(dbg_out("ftok", [128, 32 * 512], BF16), f_tok[:].rearrange("p a b -> p (a b)"), r=[f_tok.g])
                    kb.ld(dbg_out("hT", [128, 8 * NT], BF16), hT[:].rearrange("p a b -> p (a b)"), r=[hT.g])
                kb.barrier()
            if stage >= 3:
                with contextlib.ExitStack() as ph3:
                    cd = T(kb, ph3, "cd", [128, 256], BF16)
                    kb.ld(cd[:], cdft_d, w=[cd.g])
                    ring = [T(kb, ph3, "dring%d" % i, [128, 2, 8, 256], BF16) for i in range(3)]
                    pq = [T(kb, ph3, "pq%d" % i, [128, 4, 512], BF16) for i in range(2)]
                    ri = 0
                    for blk in range(8):
                        accs = [bank() for _ in range(4)]
                        for k0 in range(0, 32, 8):
                            rt = ring[ri % 3]
                            ri += 1
                            for ci, src in enumerate((dftC, dftS)):
                                kb.ld(rt[:, ci, :, :], src[k0 * 128:(k0 + 8) * 128, blk * 256:(blk + 1) * 256].rearrange("(k p) f -> p k f", p=128),
                                      w=[rt.g])
                            for kk in range(8):
                                kc = k0 + kk
                                for g in range(4):
                                    pb, pg = accs[g]
                                    last = (kc == 31)
                                    kb.mm(pb[:, :], f_tok[:, kc, g * 128:(g + 1) * 128], rt[:, :, kk, :], kc == 0, last,
                                          r=[f_tok.g, rt.g], w=[pg], inc=(last or (kk == 7 and g == 3)))
                        pqt = pq[blk % 2]
                        for g in range(4):
                            pb, pg = accs[g]
                            kb.copy("act" if g % 2 else "dve", pqt[:, g, :], pb[:, :], r=[pg], w=[pqt.g])
                        for g in range(4):
                            pb, pg = bank()
                            kb.mm(pb[:, 0:256], cd[:, 0:128], pqt[:, g, 0:256], True, False, r=[cd.g, pqt.g], w=[pg], inc=False)
                            kb.mm(pb[:, 0:256], cd[:, 128:256], pqt[:, g, 256:512], False, True, r=[cd.g, pqt.g], w=[pg])
                            kb.copy("act" if g % 2 else "dve", FT[:, g, blk * 256:(blk + 1) * 256], pb[:, 0:256], r=[pg], w=[FT.g])
                    if dbg and stage == 3:
                        kb.ld(dbg_out("FT", [128, 4 * NT], BF16), FT[:].rearrange("p a b -> p (a b)"), r=[FT.g])
                    kb.barrier()


        if stage >= 4:
            with contextlib.ExitStack() as ph4:
                wq = T(kb, ph4, "wq", [128, 8, 512], BF16)
                wload(wq, wq.g, w_in, 512, c0=OQ)
                bqs = T(kb, ph4, "bqs", [128, 4], F32)
                QS = 128.0 ** -0.5
                kb.ts("dve", bqs[:], colsT[:, C_BQ:C_BQ + 4], QS, None, ALU.mult, r=[colsT.g], w=[bqs.g])
                wrc = [T(kb, ph4, "wrc%d" % i, [128, 8, 128], BF16) for i in range(4)]
                BL = 256
                qT = T(kb, ph4, "qT", [128, 4, BL], F32)
                kT = T(kb, ph4, "kT", [128, 4, BL], F32)
                ktok = T(kb, ph4, "ktok4", [128, 2, 512], F32)
                vtok = T(kb, ph4, "vtok4", [128, 2, 1024], BF16)
                rs = T(kb, ph4, "rs", [128, 8, BL], BF16)
                lrT2 = [T(kb, ph4, "lrT4_%d" % i, [32, BL], F32) for i in range(2)]
                for i in range(2):
                    kb.memset("pool", lrT2[i][:], 1.0, w=[lrT2[i].g])
                e1 = T(kb, ph4, "e1_4", [128, 512], F32)
                Ls = [T(kb, ph4, "L4_%d" % i, [128, 512], F32) for i in range(2)]
                Eq = T(kb, ph4, "Eq", [128, 512], F32)
                Ek = T(kb, ph4, "Ek", [128, 512], F32)
                Eqs = [Eq, T(kb, ph4, "Eq2", [128, 512], F32)]
                Eks = [Ek, T(kb, ph4, "Ek2", [128, 512], F32)]
                e1s = [e1, T(kb, ph4, "e1_4b", [128, 512], F32)]
                decB = T(kb, ph4, "decB", [128, 4], F32)
                qin = [T(kb, ph4, "qin%d" % i, [128, 4, 128], BF16) for i in range(2)]
                kin = [T(kb, ph4, "kin%d" % i, [128, 4, 128], BF16) for i in range(2)]
                kst = T(kb, ph4, "kst4", [128, 512], BF16)
                ta = T(kb, ph4, "ta", [128, 512], F32)
                tb = T(kb, ph4, "tb", [128, 512], F32)
                attb = T(kb, ph4, "attb", [128, 4, 128], BF16)
                sq = [ta, tb]
                rstd = T(kb, ph4, "rstd", [128, 512], F32)
                ton = T(kb, ph4, "ton", [128, 256], F32)
                wri = 0
                wreq = [0]
                for blk in range(NT // BL - 1, -1, -1):
                    tok0 = blk * BL
                    for h in range(4):
                        pb, pg = bank()
                        for k in range(8):
                            kb.mm(pb[:, 0:BL], wq[:, k, h * 128:(h + 1) * 128], hT[:, k, tok0:tok0 + BL], k == 0, k == 7, r=[wq.g, hT.g], w=[pg])
                        kb.act(qT[:, h, :], pb[:, 0:BL], AF.Identity, r=[pg, bqs.g], w=[qT.g], bias=bqs[:, h:h + 1], scale=QS)
                        pb, pg = bank()
                        for k in range(8):
                            kb.mm(pb[:, 0:BL], wk[:, k, h * 128:(h + 1) * 128], hT[:, k, tok0:tok0 + BL], k == 0, k == 7, r=[wk.g, hT.g], w=[pg])
                        kb.act(kT[:, h, :], pb[:, 0:BL], AF.Identity, r=[pg, colsT.g], w=[kT.g], bias=col(C_BK + h))
                    for hc in range(8):
                        while wreq[0] < min(wri + 4, 8 * (NT // BL)):
                            wt2 = wrc[wreq[0] % 4]
                            wload(wt2, wt2.g, w_in, 128, c0=OR + (wreq[0] % 8) * 128, kstep=8)
                            wreq[0] += 1
                        wt = wrc[wri % 4]
                        wri += 1
                        pb, pg = bank()
                        for k in range(8):
                            kb.mm(pb[:, 0:BL], wt[:, k, :], hT[:, k, tok0:tok0 + BL], k == 0, k == 7, r=[wt.g, hT.g], w=[pg])
                        kb.act(rs[:, hc, :], pb[:, 0:BL], AF.Silu, r=[pg, colsT.g], w=[rs.g], bias=col(C_BR + hc))
                    for dirn in range(2):
                        pb, pg = bank()
                        for k in range(8):
                            kb.mm(pb[0:16, 0:BL], wlr[:, k, dirn * 16:(dirn + 1) * 16], hT[:, k, tok0:tok0 + BL], k == 0, k == 7,
                                  r=[wlr.g, hT.g], w=[pg])
                        kb.act(lrT2[dirn][0:16, :], pb[0:16, 0:BL], AF.Identity, r=[pg, colsT.g], w=[lrT2[dirn].g],
                               bias=colsT[0:16, C_BLF + dirn:C_BLF + dirn + 1])
                    for t in range(BL // 128):
                        pb, pg = bank()
                        for k in range(8):
                            kb.mm(pb[:, :], hT[:, k, tok0 + t * 128:tok0 + (t + 1) * 128], wk[:, k, :], k == 0, k == 7, r=[hT.g, wk.g], w=[pg])
                        kb.tt("dve", ktok[:, t, :], pb[:, :], bkv[:, 0:512], ALU.add, r=[pg, bkv.g], w=[ktok.g])
                        for hh in range(2):
                            pb, pg = bank()
                            for k in range(8):
                                kb.mm(pb[:, :], hT[:, k, tok0 + t * 128:tok0 + (t + 1) * 128], wv[:, k, hh * 512:(hh + 1) * 512], k == 0, k == 7,
                                      r=[hT.g, wv.g], w=[pg])
                            kb.tt("dve", vtok[:, t, hh * 512:(hh + 1) * 512], pb[:, :], bkv[:, 512 + hh * 512:1024 + hh * 512], ALU.add,
                                  r=[pg, bkv.g], w=[vtok.g])
                    for t in range(BL // 128 - 1, -1, -1):
                        gt = blk * (BL // 128) + t
                        i0 = t * 128
                        zbs = []
                        for dirn in range(2):
                            pb, pg = bank()
                            kb.mm(pb[:, :], lrT2[dirn][0:32, i0:i0 + 128], wdec_sb[0:32, dirn, :], True, True,
                                  r=[lrT2[dirn].g, wdec_sb.g], w=[pg])
                            zbs.append((pb, pg))
                        for dirn in range(2):
                            pb, pg = zbs[dirn]
                            kb.act(e1s[dirn][:], pb[:, :], AF.Exp, r=[pg], w=[e1s[dirn].g], scale=-1.0)
                        for dirn in range(2):
                            kb.act(Ls[dirn][:], e1s[dirn][:], AF.Ln, r=[e1s[dirn].g], w=[Ls[dirn].g], bias=1.0)
                        abs_ = []
                        for dirn in range(2):
                            pb, pg = bank()
                            tri = cst[:, K_TRIF:K_TRIF + 128] if dirn == 0 else cst[:, K_TRIB:K_TRIB + 128]
                            for h in range(4):
                                kb.mm(pb[:, h * 128:(h + 1) * 128], Ls[dirn][:, h * 128:(h + 1) * 128], tri, True, True,
                                      r=[Ls[dirn].g, cst.g], w=[pg], inc=(h == 3))
                            abs_.append((pb, pg))
                        pbr, pgr = bank()
                        kb.mm(pbr[:, :], cst[:, K_TRIRB:K_TRIRB + 128], Ls[1][:], True, True, r=[cst.g, Ls[1].g], w=[pgr])
                        for dirn in range(2):
                            pb, pg = abs_[dirn]
                            kb.act(Eqs[dirn][:], pb[:, :], AF.Exp, r=[pg], w=[Eqs[dirn].g])
                            kb.act(Eks[dirn][:], pb[:, :], AF.Exp, r=[pg], w=[Eks[dirn].g], scale=-1.0)
                        kb.act(e1s[0][:], pbr[:, :], AF.Exp, r=[pgr], w=[e1s[0].g])
                        kb.copy("pool", decB[:], Eqs[1][:].rearrange("p (h i) -> p h i", h=4)[:, :, 0], r=[Eqs[1].g], w=[decB.g])
                        for dirn in range(2):
                            kb.tt("pool", qin[dirn][:], qT[:, :, i0:i0 + 128], Eqs[dirn][:].rearrange("p (h i) -> p h i", h=4), ALU.mult,
                                  r=[qT.g, Eqs[dirn].g], w=[qin[dirn].g])
                            kb.tt("dve", kin[dirn][:], kT[:, :, i0:i0 + 128], Eks[dirn][:].rearrange("p (h i) -> p h i", h=4), ALU.mult,
                                  r=[kT.g, Eks[dirn].g], w=[kin[dirn].g])
                        kb.tt("pool", kst[:], ktok[:, t, :], e1s[0][:], ALU.mult, r=[ktok.g, e1s[0].g], w=[kst.g])
                        pbF, pgF = bank()
                        pbB, pgB = bank()
                        for h in range(4):
                            kb.mm(pbF[:, h * 128:(h + 1) * 128], kin[0][:, h, :], qin[0][:, h, :], True, True,
                                  r=[kin[0].g, qin[0].g], w=[pgF], inc=(h == 3))
                        for h in range(4):
                            kb.mm(pbB[:, h * 128:(h + 1) * 128], kin[1][:, h, :], qin[1][:, h, :], True, True,
                                  r=[kin[1].g, qin[1].g], w=[pgB], inc=(h == 3))
                        kb.tt("dve", ta[:], pbF[:, :], cst[:, K_MF:K_MF + 512], ALU.mult, r=[pgF, cst.g], w=[ta.g])
                        kb.tt("dve", tb[:], pbB[:, :], cst[:, K_MB:K_MB + 512], ALU.mult, r=[pgB, cst.g], w=[tb.g])
                        kb.tt("pool", attb[:].rearrange("p h i -> p (h i)"), ta[:], tb[:], ALU.add, r=[ta.g, tb.g], w=[attb.g])
                        obanks = [bank(), bank()]
                        for hp in range(2):
                            pb, pg = obanks[hp]
                            for h2 in range(2):
                                h = hp * 2 + h2
                                for c in range(2):
                                    vs = h * 256 + c * 128
                                    oc = pb[:, (h2 * 2 + c) * 128:(h2 * 2 + c + 1) * 128]
                                    kb.mm(oc, vtok[:, t, vs:vs + 128], attb[:, h, :], True, False, r=[vtok.g, attb.g], w=[pg], inc=False)
                                    kb.mm(oc, slots[:, gt, vs:vs + 128], qin[0][:, h, :], False, False, r=[slot_g[gt], qin[0].g], w=[pg], inc=False)
                                    kb.mm(oc, stBb[:, vs:vs + 128], qin[1][:, h, :], False, True, r=[stBb.g, qin[1].g], w=[pg],
                                          inc=(h2 == 1 and c == 1))
                        msb, msg = bank()
                        for hp in range(2):
                            pb, pg = obanks[hp]
                            kb.act(sq[hp][:], pb[:, :], AF.Square, r=[pg], w=[sq[hp].g])
                        for h in range(4):
                            hp, h2 = h // 2, h % 2
                            kb.mm(msb[:, h * 128:(h + 1) * 128], cst[:, K_ONES:K_ONES + 128], sq[hp][:, (h2 * 2) * 128:(h2 * 2 + 1) * 128],
                                  True, False, r=[cst.g, sq[hp].g], w=[msg], inc=False)
                            kb.mm(msb[:, h * 128:(h + 1) * 128], cst[:, K_ONES:K_ONES + 128], sq[hp][:, (h2 * 2 + 1) * 128:(h2 * 2 + 2) * 128],
                                  False, True, r=[cst.g, sq[hp].g], w=[msg], inc=(h == 3))
                        kb.act(e1[:], msb[:, :], AF.Sqrt, r=[msg], w=[e1.g], bias=RMS_EPS)
                        kb.op("dve", lambda e, a=rstd[:], b_=e1[:]: e.reciprocal(a, b_), r=[e1.g], w=[rstd.g])
                        for hp in range(2):
                            pb, pg = obanks[hp]
                            ov = pb[:, :].rearrange("p (h c i) -> p h c i", h=2, c=2)
                            rv = rstd[:].rearrange("p (h i) -> p h i", h=4)[:, hp * 2:hp * 2 + 2, :]
                            sv = slots[:, gt, :].rearrange("p (h c i) -> p h c i", h=4, c=2)
                            rsv = rs[:].rearrange("p (h c) i -> p h c i", c=2)
                            for c in range(2):
                                tv = ton[:].rearrange("p (h i) -> p h i", h=2)
                                kb.stt("dve", tv, ov[:, :, c, :], col(C_GN + c), rv, ALU.mult, ALU.mult, r=[pg, rstd.g, colsT.g], w=[ton.g])
                                kb.tt("pool", sv[:, hp * 2:hp * 2 + 2, c, :], tv, rsv[:, hp * 2:hp * 2 + 2, c, i0:i0 + 128], ALU.mult,
                                      r=[ton.g, rs.g], w=[slot_g[gt]])
                        for hp in range(2):
                            pb, pg = bank()
                            for h2 in range(2):
                                h = hp * 2 + h2
                                kb.mm(pb[:, h2 * 256:(h2 + 1) * 256], kst[:, h * 128:(h + 1) * 128], vtok[:, t, h * 256:(h + 1) * 256],
                                      True, True, r=[kst.g, vtok.g], w=[pg], inc=(h2 == 1))
                            for h2 in range(2):
                                h = hp * 2 + h2
                                kb.stt("dve", stB[:, h * 256:(h + 1) * 256], stB[:, h * 256:(h + 1) * 256], decB[:, h:h + 1],
                                       pb[:, h2 * 256:(h2 + 1) * 256], ALU.mult, ALU.add, r=[pg, decB.g], w=[stB.g])
                        kb.copy("act", stBb[:], stB[:], r=[stB.g], w=[stBb.g])
                if dbg and stage == 4:
                    kb.ld(dbg_out("og", [128, 16 * 1024], BF16), slots[:].rearrange("p a b -> p (a b)"), r=slot_g)
                    kb.ld(dbg_out("FT", [128, 4 * NT], BF16), FT[:].rearrange("p a b -> p (a b)"), r=[FT.g])
                kb.barrier()


        if stage >= 5:
            mixA.close()
            omT = T(kb, mix, "omT", [128, 4, NT], BF16)
            onesb = T(kb, mix, "onesb", [128, 128], BF16)
            kb.copy("dve", onesb[:], cst[:, K_ONE1:K_ONE1 + 128], r=[cst.g], w=[onesb.g])
            with contextlib.ExitStack() as ph5:
                wmq = T(kb, ph5, "wmq", [128, 8, 512], BF16)
                wload(wmq, wmq.g, w_in, 512, c0=OMQ)
                MS = 128.0 ** -0.5
                bms = T(kb, ph5, "bms", [128, 4], F32)
                kb.ts("dve", bms[:], colsT[:, C_BMQ:C_BMQ + 4], MS, None, ALU.mult, r=[colsT.g], w=[bms.g])
                mqT = T(kb, ph5, "mqT", [128, 4, 512], BF16)
                eT = [T(kb, ph5, "eT%d" % i, [128, 2, 512], BF16) for i in range(2)]
                rden = [T(kb, ph5, "rden%d" % i, [128, 512], F32) for i in range(2)]
                for blk in range(4):
                    tok0 = blk * 512
                    for h in range(4):
                        pb, pg = bank()
                        for k in range(8):
                            kb.mm(pb[:, :], wmq[:, k, h * 128:(h + 1) * 128], hT[:, k, tok0:tok0 + 512], k == 0, k == 7, r=[wmq.g, hT.g], w=[pg])
                        kb.act(mqT[:, h, :], pb[:, :], AF.Identity, r=[pg, bms.g], w=[mqT.g], bias=bms[:, h:h + 1], scale=MS)
                    for h in range(4):
                        et = eT[h % 2]
                        for mc in range(2):
                            pb, pg = bank()
                            kb.mm(pb[:, :], mkT[:, h, mc * 128:(mc + 1) * 128], mqT[:, h, :], True, True, r=[mkT.g, mqT.g], w=[pg])
                            kb.act(et[:, mc, :], pb[:, :], AF.Exp, r=[pg], w=[et.g])
                        pbo, pgo = bank()
                        pbd, pgd = bank()
                        for mc in range(2):
                            kb.mm(pbo[:, :], mv[:, mc, h * 128:(h + 1) * 128], et[:, mc, :], mc == 0, mc == 1, r=[mv.g, et.g], w=[pgo])
                        for mc in range(2):
                            kb.mm(pbd[:, :], onesb[:], et[:, mc, :], mc == 0, mc == 1, r=[onesb.g, et.g], w=[pgd])
                        rd = rden[h % 2]
                        kb.op("dve", lambda e, a=rd[:], b_=pbd[:, :]: e.reciprocal(a, b_), r=[pgd], w=[rd.g])
                        kb.tt("dve", omT[:, h, tok0:tok0 + 512], pbo[:, :], rd[:], ALU.mult, r=[pgo, rd.g], w=[omT.g])
                if dbg and stage == 5:
                    kb.ld(dbg_out("omT", [128, 4 * NT], BF16), omT[:].rearrange("p a b -> p (a b)"), r=[omT.g])
                kb.barrier()

        if stage >= 6:
            p7 = top.enter_context(contextlib.ExitStack())
            sprev = T(kb, p7, "sprev", [128, 32], F32)
            kb.memset("pool", sprev[:], 0.0, w=[sprev.g])
            wout = T(kb, p7, "wout", [128, 8, 1024], BF16)
            wload(wout, wout.g, w_out, 1024)
            wrt = T(kb, p7, "wrt", [128, 8, 32], F32)
            kb.ld(wrt[:], w_router.rearrange("(k p) e -> p k e", p=128), w=[wrt.g])
            bc = T(kb, p7, "bc", [128, 4, 1024], F32)
            brt = T(kb, p7, "brt", [128, 32], F32)
            bcast_load(brt[:], V_BRT, 32, brt.g)
            with contextlib.ExitStack() as pz:
                tmpb = T(kb, pz, "tmpb", [128, 2, 1024], F32)
                bcast_load(bc[:, 0, :], V_LNG, 1024, bc.g)
                bcast_load(tmpb[:, 0, :], V_LNB, 1024, tmpb.g)
                bcast_load(tmpb[:, 1, :], V_BOUT, 1024, tmpb.g)
                bcast_load(bc[:, 2, :], V_L1G, 1024, bc.g)
                bcast_load(bc[:, 3, :], V_L1B, 1024, bc.g)
                kb.ts("dve", bc[:, 0, :], bc[:, 0, :], ALPHA, None, ALU.mult, r=[bc.g], w=[bc.g])
                kb.stt("dve", bc[:, 1, :], tmpb[:, 0, :], ALPHA, tmpb[:, 1, :], ALU.mult, ALU.add, r=[tmpb.g], w=[bc.g])
                kb.barrier()
            h1g = kb.reg("h1_dram")
            for half in range(2):
                mergedT = T(kb, p7, "mergedT%d" % half, [128, 8, 1024], BF16) if half == 0 else mergedT
                with contextlib.ExitStack() as p6:
                    wsets = []
                    for i in range(2):
                        wsets.append({"g": [T(kb, p6, "wg%d_%d" % (i, j), [128, 8, 128], BF16) for j in range(3)],
                                      "bg": T(kb, p6, "wbg%d" % i, [128, 8, 128], BF16),
                                      "bf": T(kb, p6, "wbf%d" % i, [128, 4, 128], BF16),
                                      "bm": T(kb, p6, "wbm%d" % i, [128, 4, 128], BF16)})
                    sig = [T(kb, p6, "sig%d" % i, [128, 3, 512], F32) for i in range(2)]
                    t0s = [T(kb, p6, "t0s%d" % i, [128, 512], F32) for i in range(2)]
                    t1s = [T(kb, p6, "t1s%d" % i, [128, 512], F32) for i in range(2)]
                    it = 0

                    def load_chunk(c2):
                        ws2 = wsets[c2 % 2]
                        for j2 in range(3):
                            wload(ws2["g"][j2], ws2["g"][j2].g, w_in, 128, c0=OG + j2 * 1024 + c2 * 128)
                        wload(ws2["bg"], ws2["bg"].g, w_br_gla, 128, c0=c2 * 128)
                        wload(ws2["bf"], ws2["bf"].g, w_br_fnet, 128, kchunks=4, c0=c2 * 128)
                        wload(ws2["bm"], ws2["bm"].g, w_br_mem, 128, kchunks=4, c0=c2 * 128)

                    load_chunk(0)
                    for c in range(8):
                        ws = wsets[c % 2]
                        if c + 1 < 8:
                            load_chunk(c + 1)
                        for b2 in range(2):
                            tok0 = half * 1024 + b2 * 512
                            tl0 = tok0 // 128
                            sg_, t0, t1 = sig[it % 2], t0s[it % 2], t1s[it % 2]
                            it += 1
                            for j in range(3):
                                pb, pg = bank()
                                for k in range(8):
                                    kb.mm(pb[:, :], ws["g"][j][:, k, :], hT[:, k, tok0:tok0 + 512], k == 0, k == 7, r=[ws["g"][j].g, hT.g], w=[pg])
                                kb.act(sg_[:, j, :], pb[:, :], AF.Sigmoid, r=[pg, colsT.g], w=[sg_.g], bias=col(C_BG + j * 8 + c))
                            pbg, pgg = bank()
                            for hc in range(8):
                                kb.mm(pbg[:, :], ws["bg"][:, hc, :], slots[:, tl0:tl0 + 4, hc * 128:(hc + 1) * 128], hc == 0, hc == 7,
                                      r=[ws["bg"].g] + slot_g[tl0:tl0 + 4], w=[pgg])
                            pbf, pgf = bank()
                            for g in range(4):
                                kb.mm(pbf[:, :], ws["bf"][:, g, :], FT[:, g, tok0:tok0 + 512], g == 0, g == 3, r=[ws["bf"].g, FT.g], w=[pgf])
                            pbm, pgm = bank()
                            for h in range(4):
                                kb.mm(pbm[:, :], ws["bm"][:, h, :], omT[:, h, tok0:tok0 + 512], h == 0, h == 3, r=[ws["bm"].g, omT.g], w=[pgm])
                            kb.tt("dve", t0[:], pbg[:, :], sg_[:, 0, :], ALU.mult, r=[pgg, sg_.g], w=[t0.g])
                            kb.tt("dve", t1[:], pbf[:, :], sg_[:, 1, :], ALU.mult, r=[pgf, sg_.g], w=[t1.g])
                            kb.tt("dve", t0[:], t0[:], t1[:], ALU.add, r=[t1.g], w=[t0.g])
                            kb.tt("dve", t1[:], pbm[:, :], sg_[:, 2, :], ALU.mult, r=[pgm, sg_.g], w=[t1.g])
                            kb.tt("dve", mergedT[:, c, b2 * 512:(b2 + 1) * 512], t0[:], t1[:], ALU.add, r=[t0.g, t1.g], w=[mergedT.g])
                    if dbg and stage == 6 and half == 0:
                        kb.ld(dbg_out("mergedT", [128, 8 * 1024], BF16), mergedT[:].rearrange("p a b -> p (a b)"), r=[mergedT.g])
                    kb.barrier()
                if stage < 7:
                    continue
                with contextlib.ExitStack() as q7:
                    xt7 = [T(kb, q7, "xt7_%d" % i, [128, 1024], F32) for i in range(2)]
                    st7 = [T(kb, q7, "st7_%d" % i, [128, 16], F32) for i in range(2)]
                    zt7 = [T(kb, q7, "zt7_%d" % i, [128, 1024], F32) for i in range(2)]
                    sz7 = [T(kb, q7, "sz7_%d" % i, [128, 16], F32) for i in range(2)]
                    h1f = [T(kb, q7, "h1f_%d" % i, [128, 1024], F32) for i in range(2)]
                    h1b = [T(kb, q7, "h1b_%d" % i, [128, 1024], BF16) for i in range(4)]
                    h1T = T(kb, q7, "h1T", [128, 8, 128], F32)
                    lg = T(kb, q7, "lg", [128, 32], F32)
                    m8 = T(kb, q7, "m8", [128, 8], F32)
                    msk = T(kb, q7, "msk", [128, 32], F32)
                    ex4 = T(kb, q7, "ex4", [128, 8], F32)
                    posE = T(kb, q7, "posE", [128, 32], F32)
                    posS = T(kb, q7, "posS", [128, 32], F32)
                    prod4 = T(kb, q7, "prod4", [128, 4, 32], F32)
                    ovf = T(kb, q7, "ovf", [128, 32], F32)
                    prod = T(kb, q7, "prod", [128, 32], F32)
                    destf = T(kb, q7, "destf", [128, 4], F32)
                    lgs = [lg, T(kb, q7, "lg2", [128, 32], F32), T(kb, q7, "lg3", [128, 32], F32)]
                    m8s = [m8, T(kb, q7, "m8b", [128, 8], F32)]

                    obk = {}

                    def p7A0(tl):
                        obk[tl] = []
                        for h2 in range(2):
                            pb, pg = bank()
                            for c in range(8):
                                kb.mm(pb[:, :], mergedT[:, c, tl * 128:(tl + 1) * 128], wout[:, c, h2 * 512:(h2 + 1) * 512], c == 0, c == 7,
                                      r=[mergedT.g, wout.g], w=[pg])
                            obk[tl].append((pb, pg))

                    def p7A(tl):
                        lg = lgs[tl % 3]
                        gt = half * 8 + tl
                        p = tl % 2
                        xt, stt_, z, sz, hf_, hb_ = xt7[p], st7[p], zt7[p], sz7[p], h1f[p], h1b[tl % 4]
                        kb.ld(xt[:], x_own[gt * 128:(gt + 1) * 128, :], w=[xt.g])
                        ln_tile(xt, xt.g, stt_, stt_.g, None, None)
                        kb.stt("dve", z[:], xt[:], stt_[:, 0:1], bc[:, 0, :], ALU.subtract, ALU.mult, r=[xt.g, stt_.g, bc.g], w=[z.g])
                        kb.stt("dve", z[:], z[:], stt_[:, 2:3], bc[:, 1, :], ALU.mult, ALU.add, r=[stt_.g, bc.g], w=[z.g])
                        for h2 in range(2):
                            pb, pg = obk[tl][h2]
                            kb.tt("dve", z[:, h2 * 512:(h2 + 1) * 512], z[:, h2 * 512:(h2 + 1) * 512], pb[:, :], ALU.add, r=[pg], w=[z.g])
                        ln_tile(z, z.g, sz, sz.g, None, None)
                        kb.stt("dve", hf_[:], z[:], sz[:, 0:1], bc[:, 2, :], ALU.subtract, ALU.mult, r=[z.g, sz.g, bc.g], w=[hf_.g])
                        kb.stt("dve", hf_[:], hf_[:], sz[:, 2:3], bc[:, 3, :], ALU.mult, ALU.add, r=[sz.g, bc.g], w=[hf_.g])
                        kb.ld(h1_dram[gt * 128:(gt + 1) * 128, :], hf_[:], r=[hf_.g], w=[h1g])
                        kb.copy("act", hb_[:], hf_[:], r=[hf_.g], w=[hb_.g])

                    def p7A2(tl):
                        lg = lgs[tl % 3]
                        gt = half * 8 + tl
                        p = tl % 2
                        hf_ = h1f[p]
                        for h2 in range(2):
                            pb, pg = bank()
                            for c4 in range(4):
                                c = h2 * 4 + c4
                                kb.tr(pb[:, c4 * 128:(c4 + 1) * 128], hf_[:, c * 128:(c + 1) * 128], ident, r=[hf_.g, cst.g], w=[pg], inc=(c4 == 3))
                            kb.copy("act", h1T[:, h2 * 4:(h2 + 1) * 4, :].rearrange("p a b -> p (a b)"), pb[:, :], r=[pg], w=[h1T.g])
                        pb, pg = bank()
                        for k in range(8):
                            kb.mm(pb[:, 0:32], h1T[:, k, :], wrt[:, k, :], k == 0, k == 7, r=[h1T.g, wrt.g], w=[pg])
                        rbk[tl] = (pb, pg)

                    rbk = {}

                    def p7A2b(tl):
                        lg = lgs[tl % 3]
                        pb, pg = rbk[tl]
                        kb.tt("dve", lg[:], pb[:, 0:32], brt[:], ALU.add, r=[pg, brt.g], w=[lg.g])

                    def p7Be(tl):
                        lg = lgs[tl % 3]
                        m8 = m8s[tl % 2]
                        gt = half * 8 + tl
                        kb.op("dve", lambda e, a=m8[:], b_=lg[:]: e.max(a, b_), r=[lg.g], w=[m8.g])
                        kb.ts("dve", ex4[:, 4:5], m8[:, 0:1], -1.0, None, ALU.mult, r=[m8.g], w=[ex4.g])
                        kb.act(ex4[:, 0:4], m8[:, 0:4], AF.Exp, r=[m8.g, ex4.g], w=[ex4.g], bias=ex4[:, 4:5], accum_out=ex4[:, 5:6])
                        kb.op("dve", lambda e, a=ex4[:, 6:7], b_=ex4[:, 5:6]: e.reciprocal(a, b_), r=[ex4.g], w=[ex4.g])
                        kb.ts("dve", wts[:, gt * 4:gt * 4 + 4], ex4[:, 0:4], ex4[:, 6:7], None, ALU.mult, r=[ex4.g], w=[wts.g])
                        kb.ts("pool", msk[:], lg[:], m8[:, 3:4], None, ALU.is_ge, r=[lg.g, m8.g], w=[msk.g])

                    def p7Bl(tl):
                        lg = lgs[tl % 3]
                        gt = half * 8 + tl
                        pb, pg = bank()
                        kb.mm(pb[:, 0:32], cst[:, K_STRI:K_STRI + 128], msk[:], True, False, r=[cst.g, msk.g], w=[pg], inc=False)
                        kb.mm(pb[:, 0:32], cst[:, K_ONE1:K_ONE1 + 128], sprev[:], False, True, r=[cst.g, sprev.g], w=[pg])
                        kb.copy("act", posS[:], pb[:, 0:32], r=[pg], w=[posS.g])

                    def p7Bl1(tl):
                        lg = lgs[tl % 3]
                        gt = half * 8 + tl
                        kb.ts("pool", ovf[:], posS[:], float(CAP), None, ALU.is_ge, r=[posS.g], w=[ovf.g])
                        kb.tt("pool", posE[:], posS[:], cst[:, K_ECAP:K_ECAP + 32], ALU.add, r=[posS.g, cst.g], w=[posE.g])
                        kb.ts("pool", prod[:], posE[:], -1.0, float(E * CAP), ALU.mult, ALU.add, r=[posE.g], w=[prod.g])
                        kb.tt("pool", prod[:], prod[:], ovf[:], ALU.mult, r=[ovf.g], w=[prod.g])
                        kb.tt("pool", posE[:], posE[:], prod[:], ALU.add, r=[prod.g], w=[posE.g])
                        kb.tt("pool", sprev[:], sprev[:], msk[:], ALU.add, r=[msk.g], w=[sprev.g])

                    def p7C(tl):
                        gt = half * 8 + tl
                        hb_ = h1b[tl % 4]
                        lg = lgs[tl % 3]
                        m8 = m8s[tl % 2]
                        for k in range(4):
                            kb.stt("dve", prod4[:, k, :], lg[:], m8[:, k:k + 1], posE[:], ALU.is_equal, ALU.mult,
                                   r=[lg.g, m8.g, posE.g], w=[prod4.g])
                        kb.op("dve", lambda e, a=destf[:, 0:4], b_=prod4[:, :, :]: e.reduce_sum(a, b_, AX.X), r=[prod4.g], w=[destf.g])
                        kb.copy("dve", desti[:, gt * 4:gt * 4 + 4], destf[:], r=[destf.g], w=[desti_g[gt]])
                        for k in range(4):
                            kb.dma("pool", lambda e, idx=desti[:, gt * 4 + k:gt * 4 + k + 1], src=hb_[:, :]: e.indirect_dma_start(
                                out=xbuf, out_offset=bass.IndirectOffsetOnAxis(ap=idx, axis=0), in_=src, in_offset=None,
                                bounds_check=bcreg, oob_is_err=False), r=[hb_.g, desti_g[gt]], w=[xg])

                    for st_ in range(11):
                        if 0 <= st_ - 2 < 8:
                            p7Be(st_ - 2)
                        if st_ < 8:
                            p7A0(st_)
                        if 0 <= st_ - 1 < 8:
                            p7A2(st_ - 1)
                        if st_ < 8:
                            p7A(st_)
                        if 0 <= st_ - 1 < 8:
                            p7A2b(st_ - 1)
                        if 0 <= st_ - 2 < 8:
                            p7Bl(st_ - 2)
                        if 0 <= st_ - 3 < 8:
                            p7C(st_ - 3)
                        if 0 <= st_ - 2 < 8:
                            p7Bl1(st_ - 2)
                    kb.barrier()
            if dbg and stage == 7:
                kb.ld(dbg_out("wts", [128, 64]), wts[:], r=[wts.g])
                kb.ld(dbg_out("desti", [128, 64], I32), desti[:], r=desti_g)
                kb.ld(dbg_out("h1", [NT, D]), h1_dram, r=[h1g])


        if stage >= 8:
            p7.close()
            mix.close()
            with contextlib.ExitStack() as p8:
                NS = 16
                wslot = [T(kb, p8, "wslot%d" % i, [128, 8, 512], BF16) for i in range(NS)]
                xtok = [T(kb, p8, "xtok%d" % i, [128, 3, 1024], BF16) for i in range(2)]
                xT = [T(kb, p8, "xT%d" % i, [128, 8, CAP], BF16) for i in range(2)]
                actT = T(kb, p8, "actT", [128, 8, CAP], BF16)
                bdn = [T(kb, p8, "bdn%d" % i, [128, 1024], F32) for i in range(2)]
                gg = [T(kb, p8, "gg%d" % i, [128, CAP], F32) for i in range(2)]
                sg8 = [T(kb, p8, "sg8%d" % i, [128, CAP], F32) for i in range(2)]
                uu = [T(kb, p8, "uu%d" % i, [128, CAP], F32) for i in range(2)]
                yo = [T(kb, p8, "yo%d" % i, [128, 1024], F32) for i in range(2)]
                it = 0

                def load_gu(e2):
                    ps_ = []
                    for pc in range(4):
                        wsl = wslot[(e2 * 6 + pc) % NS]
                        wload(wsl, wsl.g, w_gu[e2], 512, c0=pc * 512, kstep=8)
                        ps_.append(wsl)
                    return ps_

                def load_dn(e2):
                    ps_ = []
                    for pc in range(2):
                        wsl = wslot[(e2 * 6 + 4 + pc) % NS]
                        wload(wsl, wsl.g, w_down[e2], 512, c0=pc * 512, kstep=8)
                        ps_.append(wsl)
                    return ps_

                def prep_expert(e2):
                    xk2, xt2, bd2 = xtok[e2 % 2], xT[e2 % 2], bdn[e2 % 2]
                    kb.ld(xk2[:], xbuf[e2 * CAP:(e2 + 1) * CAP, :].rearrange("(c p) d -> p c d", p=128), r=[xg], w=[xk2.g])
                    kb.ld(bd2[:], b_down[e2:e2 + 1, :].partition_broadcast(128), w=[bd2.g])
                    for sc in range(3):
                        for c in range(8):
                            kb.tr(pbt[:, c * 128:(c + 1) * 128], xk2[:, sc, c * 128:(c + 1) * 128], identb[:], r=[xk2.g, identb.g], w=[pbt_g], inc=(c == 7))
                        kb.copy("act" if sc % 2 else "dve", xt2[:, :, sc * 128:(sc + 1) * 128], pbt[:, :].rearrange("p (c i) -> p c i", c=8),
                                r=[pbt_g], w=[xt2.g])

                loaded = {0: load_gu(0) + load_dn(0), 1: load_gu(1) + load_dn(1)}
                for e_ in range(E):
                    pieces = loaded.pop(e_)
                    if e_ + 2 < E:
                        loaded[e_ + 2] = load_gu(e_ + 2)
                    xk, xt_ = xtok[e_ % 2], xT[e_ % 2]
                    bd = bdn[e_ % 2]
                    if e_ == 0:
                        prep_expert(0)
                    for j in range(8):
                        g_, s_, u_ = gg[it % 2], sg8[it % 2], uu[it % 2]
                        it += 1
                        pg_t, pg_g = bank()
                        wg_ = pieces[j // 4]
                        for k in range(8):
                            kb.mm(pg_t[:, 0:CAP], wg_[:, k, (j % 4) * 128:(j % 4 + 1) * 128], xt_[:, k, :], k == 0, k == 7, r=[wg_.g, xt_.g], w=[pg_g])
                        pu_t, pu_g = bank()
                        wu_ = pieces[2 + j // 4]
                        for k in range(8):
                            kb.mm(pu_t[:, 0:CAP], wu_[:, k, (j % 4) * 128:(j % 4 + 1) * 128], xt_[:, k, :], k == 0, k == 7, r=[wu_.g, xt_.g], w=[pu_g])
                        bgc = col(C_BGU + e_ * 16 + j)
                        buc = col(C_BGU + e_ * 16 + 8 + j)
                        kb.ts("dve", g_[:], pg_t[:, 0:CAP], bgc, 7.0, ALU.add, ALU.min, r=[pg_g, colsT.g], w=[g_.g])
                        kb.act(s_[:], g_[:], AF.Sigmoid, r=[g_.g], w=[s_.g], scale=1.702)
                        kb.act(u_[:], pu_t[:, 0:CAP], AF.Identity, r=[pu_g, colsT.g], w=[u_.g], bias=buc)
                        kb.ts("dve", u_[:], u_[:], 7.0, -7.0, ALU.min, ALU.max, r=[], w=[u_.g])
                        kb.tt("dve", g_[:], g_[:], s_[:], ALU.mult, r=[s_.g], w=[g_.g])
                        kb.stt("dve", actT[:, j, :], u_[:], 1.0, g_[:], ALU.add, ALU.mult, r=[g_.g, u_.g], w=[actT.g])
                    if e_ + 2 < E:
                        loaded[e_ + 2] = loaded[e_ + 2] + load_dn(e_ + 2)
                    if e_ + 1 < E:
                        prep_expert(e_ + 1)
                    for sc in range(3):
                        y_ = yo[sc % 2]
                        for h2 in range(2):
                            pb, pg = bank()
                            wd_ = pieces[4 + h2]
                            for j in range(8):
                                kb.mm(pb[:, :], actT[:, j, sc * 128:(sc + 1) * 128], wd_[:, j, :], j == 0, j == 7, r=[actT.g, wd_.g], w=[pg])
                            kb.tt("dve", y_[:, h2 * 512:(h2 + 1) * 512], pb[:, :], bd[:, h2 * 512:(h2 + 1) * 512], ALU.add, r=[pg, bd.g], w=[y_.g])
                        r0 = e_ * CAP + sc * 128
                        kb.ld(ybuf[r0:r0 + 128, :], y_[:], r=[y_.g], w=[yg])
                kb.barrier()
            with contextlib.ExitStack() as p9:
                l2 = T(kb, p9, "l2", [128, 2, 1024], F32)
                bcast_load(l2[:, 0, :], V_L2G, 1024, l2.g)
                bcast_load(l2[:, 1, :], V_L2B, 1024, l2.g)
                ygt = [T(kb, p9, "ygt%d" % i, [128, 4, 1024], F32) for i in range(4)]
                h1r = [T(kb, p9, "h1r%d" % i, [128, 1024], F32) for i in range(4)]
                acc = [T(kb, p9, "acc%d" % i, [128, 1024], F32) for i in range(4)]
                s9 = [T(kb, p9, "s9_%d" % i, [128, 16], F32) for i in range(4)]
                og_ = kb.reg("out")
                def combine_steps(gt):
                    p = gt % 4
                    y4, hr, ac, st9 = ygt[p], h1r[p], acc[p], s9[p]
                    for k in range(4):
                        kb.dma("pool", lambda e, idx=desti[:, gt * 4 + k:gt * 4 + k + 1], dst=y4[:, k, :]: e.indirect_dma_start(
                            out=dst, out_offset=None, in_=ybuf, in_offset=bass.IndirectOffsetOnAxis(ap=idx, axis=0),
                            bounds_check=bcreg, oob_is_err=False), r=[yg, desti_g[gt]], w=[y4.g])
                    kb.ld(hr[:], h1_dram[gt * 128:(gt + 1) * 128, :], r=[h1g], w=[hr.g])
                    yield
                    kb.stt("dve", ac[:], y4[:, 0, :], wts[:, gt * 4:gt * 4 + 1], hr[:], ALU.mult, ALU.add, r=[y4.g, wts.g, hr.g], w=[ac.g])
                    yield
                    kb.stt("dve", ac[:], hr[:], ALPHA - 1.0, ac[:], ALU.mult, ALU.add, r=[hr.g], w=[ac.g])
                    yield
                    for k in range(1, 4):
                        kb.stt("dve", ac[:], y4[:, k, :], wts[:, gt * 4 + k:gt * 4 + k + 1], ac[:], ALU.mult, ALU.add, r=[y4.g, wts.g], w=[ac.g])
                        yield
                    ln_tile(ac, ac.g, st9, st9.g, None, None)
                    yield
                    kb.stt("dve", hr[:], ac[:], st9[:, 0:1], l2[:, 0, :], ALU.subtract, ALU.mult, r=[ac.g, st9.g, l2.g], w=[hr.g])
                    yield
                    kb.stt("dve", hr[:], hr[:], st9[:, 2:3], l2[:, 1, :], ALU.mult, ALU.add, r=[st9.g, l2.g], w=[hr.g])
                    yield
                    kb.ld(out_d[gt * 128:(gt + 1) * 128, :], hr[:], r=[hr.g], w=[og_])

                for g0 in range(0, 16, 2):
                    gens = [combine_steps(g0), combine_steps(g0 + 1)]
                    while gens:
                        for g_ in list(gens):
                            try:
                                next(g_)
                            except StopIteration:
                                gens.remove(g_)
                kb.barrier()

        kb.barrier()
    return nc, dbg_d


def prep_inputs(inp):
    x = np.asarray(inp["x"], np.float32)
    mem = np.asarray(inp["mem"], np.float32)
    w_in = np.ascontiguousarray(np.asarray(inp["w_in"], np.float32)[0])
    b_in = np.asarray(inp["b_in"], np.float32)[0]
    cst = host_constants()
    cd = chan_dft()
    dfts = [dft_mats(0), dft_mats(1)]
    g1 = lambda k: np.asarray(inp[k], np.float32).reshape(-1)
    vecs = np.concatenate([b_in[OK_:OK_ + 512], b_in[OV:OV + 1024], b_in[OFN:OFN + 512], g1("b_out"),
                           g1("ln_in_g"), g1("ln_in_b"), g1("ln1_g"), g1("ln1_b"), g1("ln2_g"), g1("ln2_b"),
                           g1("b_router")]).astype(np.float32)[None, :]
    assert vecs.shape[1] == NVEC
    maps = []
    for c in range(8):
        b, hf = c // 2, c % 2
        if hf == 0:
            xo, xt = x[b, :NT], x[b, NT:]
            lf, lb, df, db = OLF, OLB, "f", "b"
        else:
            xo, xt = x[b, ::-1][:NT], x[b, ::-1][NT:]
            lf, lb, df, db = OLB, OLF, "b", "f"
        w_lr = np.concatenate([w_in[:, lf:lf + 16], w_in[:, lb:lb + 16]], axis=1)
        wdec = np.zeros((2, 32, 512), np.float32)
        for i, dd in enumerate((df, db)):
            wdec[i, :16] = np.asarray(inp["w_decay_" + dd], np.float32)[0]
            wdec[i, 16] = np.asarray(inp["b_decay_" + dd], np.float32)[0]
        colsT = np.zeros((128, NCOLS), np.float32)

        def put(c0, vec):
            v = np.asarray(vec, np.float32).reshape(-1, 128)
            colsT[:, c0:c0 + v.shape[0]] = v.T

        put(C_LNG, g1("ln_in_g")); put(C_LNB, g1("ln_in_b")); put(C_MG, g1("ln_mem_g")); put(C_MB, g1("ln_mem_b"))
        put(C_BQ, b_in[OQ:OQ + 512]); put(C_BK, b_in[OK_:OK_ + 512]); put(C_BR, b_in[OR:OR + 1024])
        put(C_BMQ, b_in[OMQ:OMQ + 512]); put(C_BG, b_in[OG:OG + 3072]); put(C_GN, g1("gla_norm_g"))
        colsT[:16, C_BLF] = b_in[lf:lf + 16]
        colsT[:16, C_BLB] = b_in[lb:lb + 16]
        put(C_BGU, np.asarray(inp["b_gu"], np.float32)[0].reshape(-1))
        maps.append({
            "x_own": np.ascontiguousarray(xo), "x_oth": np.ascontiguousarray(xt), "mem": np.ascontiguousarray(mem[b]),
            "w_in": w_in, "w_lr": np.ascontiguousarray(w_lr), "wdec": wdec, "colsT": colsT, "vecs": vecs, "cst": cst,
            "dftC": dfts[hf][0], "dftS": dfts[hf][1], "cdft": cd,
            "w_br_gla": np.asarray(inp["w_br_gla"], np.float32)[0], "w_br_fnet": np.asarray(inp["w_br_fnet"], np.float32)[0],
            "w_br_mem": np.asarray(inp["w_br_mem"], np.float32)[0], "w_mem_kv": np.asarray(inp["w_mem_kv"], np.float32)[0],
            "w_out": np.asarray(inp["w_out"], np.float32)[0], "w_router": np.asarray(inp["w_router"], np.float32)[0],
            "w_gu": np.asarray(inp["w_gu"], np.float32)[0], "w_down": np.asarray(inp["w_down"], np.float32)[0],
            "b_down": np.asarray(inp["b_down"], np.float32)[0],
        })
    return maps


def kernel(**inputs):
    nc, _ = build()
    maps = prep_inputs(inputs)
    res = run_bass_kernel_spmd(nc, maps, core_ids=list(range(8)))
    out = np.zeros((4, S, D), np.float32)
    for c in range(8):
        b, hf = c // 2, c % 2
        o = np.asarray(res.results[c]["out"], np.float32)
        if hf == 0:
            out[b, :NT] = o
        else:
            out[b, NT:] = o[::-1]
    return out
```
